# Optimizing a Trainium2 kernel written in Bass

```python
import jax, jax.numpy as jnp
from jax import lax
import numpy as np

D_MODEL = 1024
BATCH = 8
SEQ = 2048
DEPTH = 4

CHUNK = 64
N_MIXERS = 2
N_RET_LAYERS = (DEPTH + 1) // 2
N_SB_LAYERS = DEPTH // 2
RET_HEADS = D_MODEL // 256
RET_QK_DIM = D_MODEL // RET_HEADS
RET_V_DIM = 2 * RET_QK_DIM
RET_IN_DIM = 2 * RET_HEADS * RET_QK_DIM + 2 * RET_HEADS * RET_V_DIM
ROPE_BASE = 10000.0
GN_EPS = 1e-6
SB_HEADS = 16
SB_HEAD_DIM = D_MODEL // SB_HEADS
Q_BLOCK = 128
N_EXPERTS = 32
TOP_K = 4
D_EXPERT = D_MODEL
SWIGLU_LIMIT = 7.0
SWIGLU_ALPHA = 1.702
MOE_BLOCK = 128
D_PLE = 256
DN_ALPHA = float((2 * DEPTH) ** 0.25)
DN_BETA = float((8 * DEPTH) ** -0.25)
LN_EPS = 1e-5

kernel_name = 'hybrid_retention_stickbreaking_moe_encoder'


def layer_norm(x, g, b):
    xf = x.astype(jnp.float32)
    mu = jnp.mean(xf, axis=-1, keepdims=True)
    var = jnp.mean(jnp.square(xf - mu), axis=-1, keepdims=True)
    return ((xf - mu) * lax.rsqrt(var + LN_EPS)).astype(x.dtype) * g + b


def rotary(x, pos):
    half = x.shape[-1] // 2
    inv_freq = 1.0 / (ROPE_BASE ** (jnp.arange(half, dtype=jnp.float32) / half))
    ang = pos.astype(jnp.float32)[:, None] * inv_freq[None, :]
    cos = jnp.cos(ang)[None, :, None, :].astype(x.dtype)
    sin = jnp.sin(ang)[None, :, None, :].astype(x.dtype)
    x1, x2 = x[..., :half], x[..., half:]
    return jnp.concatenate([x1 * cos - x2 * sin, x1 * sin + x2 * cos], axis=-1)


def retention_mixer(x, w_in, w_out):
    B, S, _ = x.shape
    H, dk, dv = RET_HEADS, RET_QK_DIM, RET_V_DIM
    proj = x @ w_in
    q, k, v, g = jnp.split(proj, [H * dk, 2 * H * dk, 2 * H * dk + H * dv], axis=-1)
    pos = jnp.arange(S)
    q = rotary(q.reshape(B, S, H, dk), pos)
    k = rotary(k.reshape(B, S, H, dk), pos) * (dk ** -0.5)
    v = v.reshape(B, S, H, dv)
    n_chunks = S // CHUNK
    log_gamma = jnp.log(1.0 - 2.0 ** (-5.0 - jnp.arange(H, dtype=jnp.float32)))
    idx = jnp.arange(CHUNK, dtype=jnp.float32)
    inner_decay = jnp.exp(log_gamma[:, None, None] * jnp.abs(idx[:, None] - idx[None, :]))
    query_decay = jnp.exp(log_gamma[:, None] * (idx + 1.0))
    key_decay = jnp.exp(log_gamma[:, None] * (CHUNK - 1.0 - idx))
    chunk_decay = jnp.exp(log_gamma * CHUNK)

    def to_chunks(t):
        return t.reshape(B, n_chunks, CHUNK, H, t.shape[-1]).transpose(1, 0, 3, 2, 4)

    def step(state, inp):
        qi, ki, vi = inp
        scores = jnp.einsum('bhid,bhjd->bhij', qi, ki) * inner_decay[None]
        inner = jnp.einsum('bhij,bhje->bhie', scores, vi)
        cross = jnp.einsum('bhid,bhde->bhie', qi, state) * query_decay[None, :, :, None]
        new_state = state * chunk_decay[None, :, None, None] + jnp.einsum(
            'bhjd,bhje->bhde', ki * key_decay[None, :, :, None], vi)
        return new_state, (inner + cross).astype(jnp.float32)

    state0 = jnp.zeros((B, H, dk, dv), jnp.float32)
    _, out = lax.scan(step, state0, (to_chunks(q), to_chunks(k), to_chunks(v)))
    out = out.transpose(1, 0, 3, 2, 4).reshape(B, S, H, dv)
    mu = jnp.mean(out, axis=-1, keepdims=True)
    var = jnp.mean(jnp.square(out - mu), axis=-1, keepdims=True)
    normed = ((out - mu) * lax.rsqrt(var + GN_EPS)).reshape(B, S, H * dv).astype(g.dtype)
    return (jax.nn.silu(g) * normed) @ w_out


def stick_breaking_mixer(x, w_in, w_out):
    B, S, _ = x.shape
    H, d = SB_HEADS, SB_HEAD_DIM
    q, k, v = jnp.split(x @ w_in, 3, axis=-1)
    q = q.reshape(B, S, H, d)
    k = k.reshape(B, S, H, d)
    v = v.reshape(B, S, H, d)
    outs = []
    for blk in range(S // Q_BLOCK):
        q0 = blk * Q_BLOCK
        kv_end = q0 + Q_BLOCK
        qb, kb, vb = q[:, q0:kv_end], k[:, :kv_end], v[:, :kv_end]
        z = jnp.einsum('bqhd,bkhd->bhqk', qb, kb).astype(jnp.float32) * (d ** -0.5)
        t_idx = q0 + jnp.arange(Q_BLOCK)[:, None]
        s_idx = jnp.arange(kv_end)[None, :]
        past = s_idx < t_idx
        log_not = jnp.where(past, -jax.nn.softplus(z), 0.0)
        later = lax.cumsum(log_not, axis=3, reverse=True) - log_not
        a = jnp.where(past, jnp.exp(jax.nn.log_sigmoid(z) + later), 0.0)
        outs.append(jnp.einsum('bhqk,bkhd->bqhd', a.astype(vb.dtype), vb))
    o = jnp.concatenate(outs, axis=1).reshape(B, S, H * d)
    return o @ w_out


def moe_channel_mixer(x, router_w, router_b, w_gate_up, b_gate_up, w_down, b_down):
    B, S, D = x.shape
    h = x.reshape(B * S, D)
    T = h.shape[0]
    logits = (h @ router_w + router_b).astype(jnp.float32)
    top_logits, top_idx = lax.top_k(logits, TOP_K)
    gates = jax.nn.softmax(top_logits, axis=-1)
    flat_e = top_idx.reshape(-1)
    flat_tok = jnp.repeat(jnp.arange(T, dtype=jnp.int32), TOP_K)
    flat_gate = gates.reshape(-1)
    order = jnp.argsort(flat_e)
    sorted_e = flat_e[order]
    counts = jnp.bincount(flat_e, length=N_EXPERTS)
    padded = (counts + MOE_BLOCK - 1) // MOE_BLOCK * MOE_BLOCK
    pad_ends = jnp.cumsum(padded)
    pad_starts = pad_ends - padded
    starts = jnp.cumsum(counts) - counts
    dest = pad_starts[sorted_e] + jnp.arange(T * TOP_K) - starts[sorted_e]
    n_blocks = -(-(T * TOP_K + N_EXPERTS * (MOE_BLOCK - 1)) // MOE_BLOCK)
    n_rows = n_blocks * MOE_BLOCK
    row_tok = jnp.full((n_rows,), T, jnp.int32).at[dest].set(flat_tok[order])
    row_gate = jnp.zeros((n_rows,), jnp.float32).at[dest].set(flat_gate[order])
    block_start = jnp.arange(n_blocks) * MOE_BLOCK
    block_expert = jnp.minimum(jnp.searchsorted(pad_ends, block_start, side='right'), N_EXPERTS - 1)
    h_pad = jnp.concatenate([h, jnp.zeros((1, D), h.dtype)], axis=0)
    xs = h_pad[row_tok].reshape(n_blocks, MOE_BLOCK, D)

    def expert_block(args):
        xb, e = args
        gu = xb @ w_gate_up[e] + b_gate_up[e]
        gate = jnp.minimum(gu[:, 0::2], SWIGLU_LIMIT)
        up = jnp.clip(gu[:, 1::2], -SWIGLU_LIMIT, SWIGLU_LIMIT)
        act = (up + 1.0) * (gate * jax.nn.sigmoid(gate * SWIGLU_ALPHA))
        return act @ w_down[e] + b_down[e]

    ys = lax.map(expert_block, (xs, block_expert))
    ys = ys.reshape(n_rows, D) * row_gate[:, None].astype(ys.dtype)
    out = jnp.zeros((T + 1, D), ys.dtype).at[row_tok].add(ys)[:T]
    return out.reshape(B, S, D)


def setup_inputs(seed: int = 0) -> dict:
    key = jax.random.key(seed)
    ks = jax.random.split(key, 20)
    f32 = jnp.float32
    D, F, E = D_MODEL, D_EXPERT, N_EXPERTS
    nrm = lambda k, shape: jax.random.normal(k, shape, f32)
    x = nrm(ks[0], (BATCH, SEQ, D))
    p = nrm(ks[1], (DEPTH, BATCH, SEQ, D_PLE))
    h_qk, h_v = RET_HEADS * RET_QK_DIM, RET_HEADS * RET_V_DIM
    ret_col_scale = jnp.concatenate([jnp.ones((2 * h_qk,), f32), jnp.full((h_v,), DN_BETA, f32), jnp.ones((h_v,), f32)])
    ret_w_in = nrm(ks[2], (N_RET_LAYERS, D, RET_IN_DIM)) * (D ** -0.5) * ret_col_scale
    ret_w_out = nrm(ks[3], (N_RET_LAYERS, h_v, D)) * (h_v ** -0.5) * DN_BETA
    sb_col_scale = jnp.concatenate([jnp.ones((2 * D,), f32), jnp.full((D,), DN_BETA, f32)])
    sb_w_in = nrm(ks[4], (N_SB_LAYERS, D, 3 * D)) * (D ** -0.5) * sb_col_scale
    sb_w_out = nrm(ks[5], (N_SB_LAYERS, D, D)) * (D ** -0.5) * DN_BETA
    ln1_g = 1.0 + 0.02 * nrm(ks[6], (DEPTH, D))
    ln1_b = 0.02 * nrm(ks[7], (DEPTH, D))
    router_w = nrm(ks[8], (DEPTH, D, E)) * (D ** -0.5)
    router_b = 0.01 * nrm(ks[9], (DEPTH, E))
    w_gate_up = nrm(ks[10], (DEPTH, E, D, 2 * F)) * (D ** -0.5)
    b_gate_up = 0.02 * nrm(ks[11], (DEPTH, E, 2 * F))
    w_down = nrm(ks[12], (DEPTH, E, F, D)) * (F ** -0.5) * DN_BETA
    b_down = 0.02 * nrm(ks[13], (DEPTH, E, D))
    ln2_g = 1.0 + 0.02 * nrm(ks[14], (DEPTH, D))
    ln2_b = 0.02 * nrm(ks[15], (DEPTH, D))
    ple_w = nrm(ks[16], (DEPTH, D_PLE, D)) * (D_PLE ** -0.5) * DN_BETA
    ple_gate_w = nrm(ks[17], (DEPTH, D, D)) * (D ** -0.5)
    ple_gate_b = 0.02 * nrm(ks[18], (DEPTH, D))
    return {'x': x, 'p': p, 'ret_w_in': ret_w_in, 'ret_w_out': ret_w_out,
            'sb_w_in': sb_w_in, 'sb_w_out': sb_w_out, 'ln1_g': ln1_g, 'ln1_b': ln1_b,
            'router_w': router_w, 'router_b': router_b, 'w_gate_up': w_gate_up,
            'b_gate_up': b_gate_up, 'w_down': w_down, 'b_down': b_down,
            'ln2_g': ln2_g, 'ln2_b': ln2_b, 'ple_w': ple_w, 'ple_gate_w': ple_gate_w,
            'ple_gate_b': ple_gate_b}


def reference(x, p, ret_w_in, ret_w_out, sb_w_in, sb_w_out, ln1_g, ln1_b, router_w, router_b,
              w_gate_up, b_gate_up, w_down, b_down, ln2_g, ln2_b, ple_w, ple_gate_w, ple_gate_b):
    for i in range(DEPTH):
        j = i // N_MIXERS
        if i % N_MIXERS == 0:
            mix = retention_mixer(x, ret_w_in[j], ret_w_out[j])
        else:
            mix = stick_breaking_mixer(x, sb_w_in[j], sb_w_out[j])
        x = layer_norm(DN_ALPHA * x + mix, ln1_g[i], ln1_b[i])
        ffn = moe_channel_mixer(x, router_w[i], router_b[i], w_gate_up[i], b_gate_up[i],
                                w_down[i], b_down[i])
        x = layer_norm(DN_ALPHA * x + ffn, ln2_g[i], ln2_b[i])
        ple_gate = jax.nn.sigmoid(x @ ple_gate_w[i] + ple_gate_b[i])
        x = x + ple_gate * (p[i] @ ple_w[i])
    return x
```

```python
import math
from contextlib import ExitStack

import ml_dtypes
import numpy as np

import concourse.bass as bass
import concourse.mybir as mybir
from concourse.bass_utils import run_bass_kernel_spmd

F32 = mybir.dt.float32
BF16 = mybir.dt.bfloat16
ALU = mybir.AluOpType
AF = mybir.ActivationFunctionType
AX = mybir.AxisListType

S_TOK = 2048
D = 1024
NT = 16
NCH = 8
DEPTH = 4
NE = 32
DN_ALPHA = float((2 * DEPTH) ** 0.25)
LN_EPS = 1e-5
GN_EPS = 1e-6
SEG = 30000


class _Op:
    __slots__ = ("eng", "fn", "deps", "dma", "sig", "sigval", "idx")


class Sched:
    ENGS = ("pe", "act", "dve", "pool", "sp")

    def __init__(self, nc):
        self.nc = nc
        self.streams = {e: [] for e in self.ENGS}
        self.last_w = {}
        self.readers = {}
        self.dma_cnt = {}
        self.dma_last = {}
        self.barrier_ops = []
        self.barrier_seen = {e: True for e in self.ENGS}

    def barrier(self):
        ops = []
        for e in self.ENGS:
            for op in reversed(self.streams[e]):
                if op.dma is None:
                    ops.append(op)
                    break
        ops.extend(self.dma_last.values())
        self.barrier_ops = ops
        self.barrier_seen = {e: False for e in self.ENGS}
        self.last_w = {}
        self.readers = {}

    def add(self, eng, fn, reads=(), writes=(), dma=None):
        op = _Op()
        op.eng, op.fn, op.dma = eng, fn, None
        op.sig = False
        op.sigval = None
        ps_r = [k for k in reads if isinstance(k, tuple) and k[0] == "ps"]
        if ps_r:
            reads = [k for k in reads if not (isinstance(k, tuple) and k[0] == "ps")]
            writes = list(writes) + ps_r
        deps = {}
        for k in reads:
            w = self.last_w.get(k)
            if w is not None:
                deps[w] = True
        for k in writes:
            w = self.last_w.get(k)
            if w is not None:
                deps[w] = True
            for r in self.readers.get(k, ()):
                if r not in deps:
                    deps[r] = False
        if not self.barrier_seen[eng]:
            self.barrier_seen[eng] = True
            for b in self.barrier_ops:
                deps[b] = True
        for k in reads:
            self.readers.setdefault(k, []).append(op)
        for k in writes:
            self.last_w[k] = op
            self.readers[k] = []
        pruned = []
        latest = {}
        for d, strong in deps.items():
            if d is op:
                continue
            if d.dma is not None:
                pruned.append(d)
                continue
            if d.eng == eng and (eng == "pe" or not strong):
                continue
            o = latest.get(d.eng)
            if o is None or o.idx < d.idx:
                latest[d.eng] = d
        pruned.extend(latest.values())
        op.deps = pruned
        op.idx = len(self.streams[eng])
        if dma is not None:
            c = self.dma_cnt.get(dma, 0) + 1
            self.dma_cnt[dma] = c
            op.dma = (dma, 16 * c)
            self.dma_last[dma] = op
        self.streams[eng].append(op)
        return op

    def emit(self):
        nc = self.nc
        for e in self.ENGS:
            for op in self.streams[e]:
                for d in op.deps:
                    if d.dma is None:
                        d.sig = True
        nsegs = {}
        for e in self.ENGS:
            c = 0
            for op in self.streams[e]:
                if op.sig and op.dma is None:
                    op.sigval = (e, c // SEG, c % SEG + 1)
                    c += 1
            nsegs[e] = (c + SEG - 1) // SEG
        with ExitStack() as es:
            sems = {}
            for e in self.ENGS:
                for s in range(nsegs[e]):
                    sems[(e, s)] = es.enter_context(nc.semaphore(f"s_{e}_{s}"))
            dsems = {}
            for g in self.dma_cnt:
                dsems[g] = es.enter_context(nc.semaphore(f"d_{g}"))
            self.n_sems = len(sems) + len(dsems)
            block = es.enter_context(nc.Block())

            def run(ename, eng):
                waited = {}
                for op in self.streams[ename]:
                    need = {}
                    for d in op.deps:
                        if d.dma is not None:
                            key, val = ("d", d.dma[0]), d.dma[1]
                        else:
                            key, val = (d.sigval[0], d.sigval[1]), d.sigval[2]
                        if need.get(key, 0) < val:
                            need[key] = val
                    for key, val in need.items():
                        if waited.get(key, 0) >= val:
                            continue
                        waited[key] = val
                        sem = dsems[key[1]] if key[0] == "d" else sems[key]
                        eng.wait_ge(sem, val)
                    ins = op.fn(eng)
                    if op.dma is not None:
                        ins.then_inc(dsems[op.dma[0]], 16)
                    elif op.sig:
                        ins.then_inc(sems[(op.sigval[0], op.sigval[1])], 1)

            block.tensor(lambda eng: run("pe", eng))
            block.scalar(lambda eng: run("act", eng))
            block.vector(lambda eng: run("dve", eng))
            block.gpsimd(lambda eng: run("pool", eng))
            block.sync(lambda eng: run("sp", eng))


def _consts():
    c = {}
    c["identf"] = np.eye(128, dtype=np.float32)
    c["identb"] = np.eye(128, dtype=np.float32).astype(ml_dtypes.bfloat16)
    c["ones"] = np.ones((1, 128), dtype=np.float32)
    half = 128
    inv_freq = (1.0 / (10000.0 ** (np.arange(half, dtype=np.float32) / np.float32(half)))).astype(np.float32)
    ang = (np.arange(S_TOK, dtype=np.float32)[None, :] * inv_freq[:, None]).astype(np.float32)
    c["cos"] = np.cos(ang).astype(np.float32)
    c["sin"] = np.sin(ang).astype(np.float32)
    H = 4
    lg = np.log(1.0 - 2.0 ** (-5.0 - np.arange(H, dtype=np.float64)))
    sl = np.arange(128)[:, None].astype(np.float64)
    tl = np.arange(256)[None, :].astype(np.float64)
    masks = np.zeros((H, 3, 128, 256), dtype=np.float32)
    for h in range(H):
        masks[h, 0] = np.exp(lg[h] * (tl - sl)) / 16.0
        for m in range(2):
            s_abs = 128 * m + sl
            dec = np.exp(lg[h] * np.abs(tl - s_abs))
            ok = (np.floor(s_abs / 64) <= np.floor(tl / 64))
            masks[h, 1 + m] = dec * ok / 16.0
    c["rmask"] = np.ascontiguousarray(masks.transpose(2, 0, 1, 3))
    c["rgam"] = np.exp(lg)
    t = np.arange(128)[:, None]
    s = np.arange(128)[None, :]
    c["sbmask"] = np.where(s < t, 0.0, -30000.0).astype(np.float32)
    return c


class Builder:
    def __init__(self, layers, n_exp=NE, stages=("mix", "moe", "ple")):
        self.layers = list(layers)
        self.n_exp = n_exp
        self.stages = stages
        self.nc = bass.Bass("TRN2", target_bir_lowering=False)
        self.consts = _consts()

    def dram_in(self, name, shape, dt=F32):
        return self.nc.dram_tensor(name, list(shape), dt, kind="ExternalInput").ap()

    def build(self):
        nc = self.nc
        L = len(self.layers)
        d = {}
        d["x"] = self.dram_in("x", [S_TOK, D])
        d["pT"] = self.dram_in("pT", [DEPTH, 256, S_TOK])
        d["ret_w_in"] = self.dram_in("ret_w_in", [2, D, 6144])
        d["ret_w_out"] = self.dram_in("ret_w_out", [2, 2048, D])
        d["sb_w_in"] = self.dram_in("sb_w_in", [2, D, 3 * D])
        d["sb_w_out"] = self.dram_in("sb_w_out", [2, D, D])
        d["lnp"] = self.dram_in("lnp", [DEPTH, 4, D])
        d["router_w"] = self.dram_in("router_w", [DEPTH, D, NE])
        d["router_b"] = self.dram_in("router_b", [DEPTH, NE])
        big = "moe" in self.stages
        d["w_gate_up"] = self.dram_in("w_gate_up", [DEPTH, NE, D, 2 * D] if big else [1, 1, D, 2 * D])
        d["bgu"] = self.dram_in("bgu", [DEPTH, 128, NE * 16])
        d["w_down"] = self.dram_in("w_down", [DEPTH, NE, D, D] if big else [1, 1, D, D])
        d["b_down"] = self.dram_in("b_down", [DEPTH, NE, D])
        d["ple_w"] = self.dram_in("ple_w", [DEPTH, 256, D])
        d["ple_gate_w"] = self.dram_in("ple_gate_w", [DEPTH, D, D])
        d["ple_gate_b"] = self.dram_in("ple_gate_b", [DEPTH, D])
        d["identf"] = self.dram_in("identf", [128, 128])
        d["identb"] = self.dram_in("identb", [128, 128], BF16)
        d["ones"] = self.dram_in("ones", [1, 128])
        d["cos"] = self.dram_in("cos", [128, S_TOK])
        d["sin"] = self.dram_in("sin", [128, S_TOK])
        d["rmask"] = self.dram_in("rmask", [128, 4, 3, 256])
        d["sbmask"] = self.dram_in("sbmask", [128, 128])
        d["y"] = nc.dram_tensor("y", [S_TOK, D], F32, kind="ExternalOutput").ap()
        self.d = d

        AW = 27648
        with ExitStack() as es:
            sb = lambda n, s, dt: es.enter_context(nc.sbuf_tensor(n, s, dt))
            self.X = sb("X", [128, NT, D], F32)
            self.XT = sb("XT", [128, NCH, S_TOK], BF16)
            self.IDF = sb("IDF", [128, 128], F32)
            self.IDB = sb("IDB", [128, 128], BF16)
            self.ONES = sb("ONES", [1, 128], F32)
            self.GALL = sb("GALL", [128, NT, NE], F32)
            self.ARENA = sb("ARENA", [128, AW], F32)
            self.AW = AW
            self.PS = [es.enter_context(nc.psum_tensor(f"ps{i}", [128, 512], F32)) for i in range(8)]
            self.S = Sched(nc)
            self.uid = 0
            self.program()
            self.S.emit()
        return nc

    def arena_reset(self):
        self.aptr = 0

    def alloc(self, shape, dt=F32, parts=128):
        n = int(np.prod(shape))
        words = n if dt == F32 else (n + 1) // 2
        words = (words + 7) // 8 * 8
        a = self.ARENA[0:parts, self.aptr:self.aptr + words]
        self.aptr += words
        assert self.aptr <= self.AW, f"arena overflow {self.aptr} > {self.AW}"
        if dt != F32:
            a = a.bitcast(dt)[:, 0:n]
        else:
            a = a[:, 0:n]
        if len(shape) == 2:
            a = a.rearrange("p (a b) -> p a b", a=shape[0])
        elif len(shape) == 3:
            a = a.rearrange("p (a b c) -> p a b c", a=shape[0], b=shape[1])
        return a

    def key(self, base):
        self.uid += 1
        return (base, self.uid)

    def program(self):
        S, d = self.S, self.d
        X = self.X
        S.add("sp", lambda e: e.dma_start(out=self.IDF[:], in_=d["identf"]), writes=["IDF"], dma="c0")
        S.add("sp", lambda e: e.dma_start(out=self.IDB[:], in_=d["identb"]), writes=["IDB"], dma="c1")
        S.add("sp", lambda e: e.dma_start(out=self.ONES[:], in_=d["ones"]), writes=["ONES"], dma="c2")
        xin = d["x"].rearrange("(t p) f -> p t f", p=128)
        for q in range(4):
            S.add("sp", lambda e, q=q: e.dma_start(out=X[:, 4 * q:4 * q + 4, :], in_=xin[:, 4 * q:4 * q + 4, :]),
                  writes=[("X", t) for t in range(4 * q, 4 * q + 4)], dma=f"xin{q}")
        for l in self.layers:
            if "mix" in self.stages:
                self.arena_reset()
                S.barrier()
                self.make_xt(l, scale=DN_ALPHA, router=False)
                if l % 2 == 0:
                    self.retention(l)
                else:
                    self.stickbreak(l)
                self.layernorm(l, 0)
            if "moe" in self.stages:
                self.arena_reset()
                S.barrier()
                self.moe(l)
                self.layernorm(l, 1, self.ln_bufs)
            if "ple" in self.stages:
                self.arena_reset()
                S.barrier()
                self.ple(l)
        yout = d["y"].rearrange("(t p) f -> p t f", p=128)
        for q in range(4):
            S.add("sp", lambda e, q=q: e.dma_start(out=yout[:, 4 * q:4 * q + 4, :], in_=X[:, 4 * q:4 * q + 4, :]),
                  reads=[("X", t) for t in range(4 * q, 4 * q + 4)], writes=[("yout", q)], dma=f"yout{q}")
        S.add("sp", lambda e: e.nop(), reads=[("yout", q) for q in range(4)])

    def make_xt(self, l, scale=None, router=False):
        S, d, X, XT, PS = self.S, self.d, self.X, self.XT, self.PS
        if router:
            RW = self.alloc([NCH, NE])
            RB = self.alloc([NE], parts=1)
            XT32 = [self.alloc([NCH, 128]) for _ in range(2)]
            LG = [self.alloc([NE]) for _ in range(2)]
            EXm = [self.alloc([NE]) for _ in range(2)]
            MSK = [self.alloc([NE]) for _ in range(2)]
            SM = [self.alloc([16]) for _ in range(2)]
            kRW, kRB = self.key("RW"), self.key("RB")
            S.add("sp", lambda e: e.dma_start(out=RW, in_=d["router_w"][l].rearrange("(c p) n -> p c n", p=128)),
                  writes=[kRW], dma="rw")
            S.add("sp", lambda e: e.dma_start(out=RB, in_=d["router_b"][l:l + 1, :]), writes=[kRB], dma="rb")
        for t in range(NT):
            b = t % 2
            for h in range(2):
                pb = PS[2 * b + h]
                for j in range(4):
                    c = 4 * h + j
                    S.add("pe", lambda e, pb=pb, j=j, c=c, t=t: e.transpose(pb[:, j * 128:(j + 1) * 128], X[:, t, c * 128:(c + 1) * 128], self.IDF[:]),
                          reads=[("X", t), "IDF"], writes=[("ps", 2 * b + h)])
                S.add("act", lambda e, pb=pb, h=h, t=t: e.activation(out=XT[:, 4 * h:4 * h + 4, t * 128:(t + 1) * 128],
                                                                      in_=pb[:].rearrange("p (a b) -> p a b", a=4), func=AF.Copy),
                      reads=[("ps", 2 * b + h)], writes=[("XT", t)])
                if router:
                    S.add("dve", lambda e, pb=pb, h=h, b=b: e.tensor_copy(out=XT32[b][:, 4 * h:4 * h + 4, :],
                                                                           in_=pb[:].rearrange("p (a b) -> p a b", a=4)),
                          reads=[("ps", 2 * b + h)], writes=[("XT32", b, h)])
            if scale is not None:
                S.add("pool", lambda e, t=t: e.tensor_scalar(out=X[:, t, :], in0=X[:, t, :], scalar1=float(scale), scalar2=None, op0=ALU.mult),
                      reads=[("X", t)], writes=[("X", t)])
            if router:
                pl = PS[4 + b]
                for c in range(NCH):
                    S.add("pe", lambda e, pl=pl, c=c, b=b: e.matmul(pl[:, 0:NE], XT32[b][:, c, :], RW[:, c, :], start=(c == 0), stop=False),
                          reads=[("XT32", b, c // 4), kRW], writes=[("ps", 4 + b)])
                S.add("pe", lambda e, pl=pl: e.matmul(pl[:, 0:NE], self.ONES[0:1, :], RB[0:1, :], start=False, stop=True),
                      reads=["ONES", kRB], writes=[("ps", 4 + b)])
                lg, ex, mk, sm = LG[b], EXm[b], MSK[b], SM[b]
                kl = ("rt", b)
                S.add("dve", lambda e, pl=pl, lg=lg: e.tensor_copy(out=lg, in_=pl[:, 0:NE]), reads=[("ps", 4 + b)], writes=[(kl, "lg")])
                S.add("dve", lambda e, lg=lg, sm=sm: e.max(out=sm[:, 0:8], in_=lg), reads=[(kl, "lg")], writes=[(kl, "top")])
                S.add("dve", lambda e, lg=lg, sm=sm, mk=mk: e.tensor_scalar(out=mk, in0=lg, scalar1=sm[:, 3:4], scalar2=None, op0=ALU.is_ge),
                      reads=[(kl, "lg"), (kl, "top")], writes=[(kl, "mk")])
                S.add("dve", lambda e, sm=sm: e.tensor_scalar(out=sm[:, 8:9], in0=sm[:, 0:1], scalar1=-1.0, scalar2=None, op0=ALU.mult),
                      reads=[(kl, "top")], writes=[(kl, "nm")])
                S.add("act", lambda e, lg=lg, ex=ex, sm=sm: e.activation(out=ex, in_=lg, func=AF.Exp, bias=sm[:, 8:9], scale=1.0),
                      reads=[(kl, "lg"), (kl, "nm")], writes=[(kl, "ex")])
                S.add("dve", lambda e, ex=ex, mk=mk: e.tensor_tensor(out=ex, in0=ex, in1=mk, op=ALU.mult),
                      reads=[(kl, "ex"), (kl, "mk")], writes=[(kl, "ex")])
                S.add("dve", lambda e, ex=ex, sm=sm: e.reduce_sum(out=sm[:, 9:10], in_=ex, axis=AX.X),
                      reads=[(kl, "ex")], writes=[(kl, "ss")])
                S.add("dve", lambda e, sm=sm: e.reciprocal(out=sm[:, 10:11], in_=sm[:, 9:10]), reads=[(kl, "ss")], writes=[(kl, "rs")])
                S.add("dve", lambda e, ex=ex, sm=sm, t=t: e.tensor_scalar(out=self.GALL[:, t, :], in0=ex, scalar1=sm[:, 10:11], scalar2=None, op0=ALU.mult),
                      reads=[(kl, "ex"), (kl, "rs")], writes=[("G", t)])
                pg = PS[6 + b]
                S.add("pe", lambda e, pg=pg, t=t: e.transpose(pg[0:NE, 0:128], self.GALL[:, t, :], self.IDF[:]),
                      reads=[("G", t), "IDF"], writes=[("ps", 6 + b)])
                S.add("act", lambda e, pg=pg, t=t: e.activation(out=self.GT[0:NE, t * 128:(t + 1) * 128], in_=pg[0:NE, 0:128], func=AF.Copy),
                      reads=[("ps", 6 + b)], writes=[("GT", t)])

    def ln_alloc(self):
        return (self.alloc([D]), self.alloc([D]), [self.alloc([16]) for _ in range(2)])

    def layernorm(self, l, which, bufs=None):
        S, d, X = self.S, self.d, self.X
        G, B, ST = bufs if bufs is not None else self.ln_alloc()
        kG, kB = self.key("lng"), self.key("lnb")
        S.add("sp", lambda e: e.dma_start(out=G, in_=d["lnp"][l, 2 * which:2 * which + 1, :].to_broadcast([128, D])), writes=[kG], dma="lng")
        S.add("sp", lambda e: e.dma_start(out=B, in_=d["lnp"][l, 2 * which + 1:2 * which + 2, :].to_broadcast([128, D])), writes=[kB], dma="lnb")
        for t in range(NT):
            st = ST[t % 2]
            ks = ("lnst", t % 2)
            xt = X[:, t, :]
            S.add("dve", lambda e, st=st, xt=xt: e.bn_stats(out=st[:, 0:6], in_=xt[:, 0:512]), reads=[("X", t)], writes=[(ks, 0)])
            S.add("dve", lambda e, st=st, xt=xt: e.bn_stats(out=st[:, 6:12], in_=xt[:, 512:1024]), reads=[("X", t)], writes=[(ks, 1)])
            S.add("dve", lambda e, st=st: e.bn_aggr(out=st[:, 12:14], in_=st[:, 0:12]),
                  reads=[(ks, 0), (ks, 1)], writes=[(ks, 2)])
            S.add("dve", lambda e, st=st: e.tensor_scalar(out=st[:, 14:15], in0=st[:, 13:14], scalar1=float(LN_EPS), scalar2=None, op0=ALU.add),
                  reads=[(ks, 2)], writes=[(ks, 3)])
            S.add("act", lambda e, st=st: e.activation(out=st[:, 14:15], in_=st[:, 14:15], func=AF.Sqrt), reads=[(ks, 3)], writes=[(ks, 3)])
            S.add("dve", lambda e, st=st: e.reciprocal(out=st[:, 15:16], in_=st[:, 14:15]), reads=[(ks, 3)], writes=[(ks, 4)])
            S.add("dve", lambda e, st=st, xt=xt: e.tensor_scalar(out=xt, in0=xt, scalar1=st[:, 12:13], scalar2=st[:, 15:16], op0=ALU.subtract, op1=ALU.mult),
                  reads=[("X", t), (ks, 2), (ks, 4)], writes=[("X", t)])
            S.add("pool", lambda e, xt=xt: e.tensor_tensor(out=xt, in0=xt, in1=G, op=ALU.mult), reads=[("X", t), kG], writes=[("X", t)])
            S.add("pool", lambda e, xt=xt: e.tensor_tensor(out=xt, in0=xt, in1=B, op=ALU.add), reads=[("X", t), kB], writes=[("X", t)])

    def moe_bias(self, l, GT, BD, BGU):
        S, d, X, PS = self.S, self.d, self.X, self.PS
        kBD, kBGU = self.key("BD"), self.key("BGU")
        self.moe_keys = (kBD, kBGU)
        S.add("sp", lambda e: e.dma_start(out=BD, in_=d["b_down"][l]), writes=[kBD], dma="bd")
        S.add("sp", lambda e: e.dma_start(out=BGU, in_=d["bgu"][l]), writes=[kBGU], dma="bgu")
        for t in range(NT):
            for h in range(2):
                pb = PS[6 + h]
                S.add("pe", lambda e, pb=pb, t=t, h=h: e.matmul(pb[:], GT[0:NE, t * 128:(t + 1) * 128], BD[0:NE, h * 512:(h + 1) * 512], start=True, stop=True),
                      reads=[("GT", t), kBD], writes=[("ps", 6 + h)])
                S.add("dve", lambda e, pb=pb, t=t, h=h: e.tensor_tensor(out=X[:, t, h * 512:(h + 1) * 512], in0=pb[:], in1=X[:, t, h * 512:(h + 1) * 512], op=ALU.add),
                      reads=[("ps", 6 + h), ("X", t)], writes=[("X", t)])

    def moe(self, l):
        S, d, X, XT, PS = self.S, self.d, self.X, self.XT, self.PS
        self.GT = self.alloc([S_TOK], parts=32)
        GT = self.GT
        BD = self.alloc([D], parts=32)
        BGU = self.alloc([NE * 16])
        self.ln_bufs = self.ln_alloc()
        mark = self.aptr
        self.make_xt(l, scale=DN_ALPHA, router=True)
        self.moe_bias(l, GT, BD, BGU)
        S.barrier()
        self.aptr = mark
        kBD, kBGU = self.moe_keys
        ACTT = self.alloc([NCH, S_TOK], BF16)
        NSLOT = 4
        WGU = [self.alloc([NCH, 256], BF16) for _ in range(NSLOT)]
        WD = self.alloc([NCH, D], BF16)
        NTMP = 2
        TG = [self.alloc([512]) for _ in range(NTMP)]
        TU = [self.alloc([512]) for _ in range(NTMP)]
        TS = [self.alloc([512]) for _ in range(NTMP)]
        wgu_src = d["w_gate_up"]
        wd_src = d["w_down"]
        unit = 0
        ycnt = 0
        n_chunks = self.n_exp * 8

        def dma_wgu(g):
            if g >= n_chunks:
                return
            slot = g % NSLOT
            wsrc = wgu_src[l, g // 8].rearrange("(c p) f -> p c f", p=128)
            j = g % 8
            S.add("pool", lambda e, slot=slot, j=j, wsrc=wsrc: e.dma_start(out=WGU[slot], in_=wsrc[:, :, 256 * j:256 * (j + 1)]),
                  writes=[("WGU", slot)], dma=f"wgu{slot}")

        def dma_wd(ei):
            src = wd_src[l, ei].rearrange("(c p) f -> p c f", p=128)
            for q in range(2):
                S.add("pool", lambda e, q=q, src=src: e.dma_start(out=WD[:, 4 * q:4 * q + 4, :], in_=src[:, 4 * q:4 * q + 4, :]),
                      writes=[("WD", q)], dma=f"wd{q}")

        for g in range(NSLOT - 1):
            dma_wgu(g)
        for ei in range(self.n_exp):
            e_ = ei
            for j in range(8):
                dma_wgu(ei * 8 + j + NSLOT - 1)
                if j == 1:
                    dma_wd(ei)
                slot = (ei * 8 + j) % NSLOT
                W = WGU[slot]
                for c in range(4):
                    pr = unit % 3
                    pg_, pu_ = PS[2 * pr], PS[2 * pr + 1]
                    tb = unit % NTMP
                    unit += 1
                    tok = slice(c * 512, (c + 1) * 512)
                    for k in range(NCH):
                        S.add("pe", lambda e, pg_=pg_, W=W, k=k, tok=tok: e.matmul(pg_[:], W[:, k, 0:256:2], XT[:, k, tok], start=(k == 0), stop=(k == 7)),
                              reads=[("WGU", slot)] + [("XT", tt) for tt in range(4 * c, 4 * c + 4)], writes=[("ps", 2 * pr)])
                    for k in range(NCH):
                        S.add("pe", lambda e, pu_=pu_, W=W, k=k, tok=tok: e.matmul(pu_[:], W[:, k, 1:256:2], XT[:, k, tok], start=(k == 0), stop=(k == 7)),
                              reads=[("WGU", slot)] + [("XT", tt) for tt in range(4 * c, 4 * c + 4)], writes=[("ps", 2 * pr + 1)])
                    bg = BGU[:, e_ * 16 + 2 * j:e_ * 16 + 2 * j + 1]
                    bu = BGU[:, e_ * 16 + 2 * j + 1:e_ * 16 + 2 * j + 2]
                    tg, tu, ts = TG[tb], TU[tb], TS[tb]
                    S.add("dve", lambda e, tg=tg, pg_=pg_, bg=bg: e.tensor_scalar(out=tg, in0=pg_[:], scalar1=bg, scalar2=7.0, op0=ALU.add, op1=ALU.min),
                          reads=[("ps", 2 * pr), kBGU], writes=[("TG", tb)])
                    S.add("act", lambda e, tu=tu, pu_=pu_, bu=bu: e.activation(out=tu, in_=pu_[:], func=AF.Identity, bias=bu, scale=1.0),
                          reads=[("ps", 2 * pr + 1), kBGU], writes=[("TU", tb)])
                    S.add("act", lambda e, ts=ts, tg=tg: e.activation(out=ts, in_=tg, func=AF.Sigmoid, scale=1.702),
                          reads=[("TG", tb)], writes=[("TS", tb)])
                    S.add("pool", lambda e, tu=tu: e.tensor_scalar(out=tu, in0=tu, scalar1=7.0, scalar2=-7.0, op0=ALU.min, op1=ALU.max),
                          reads=[("TU", tb)], writes=[("TU", tb)])
                    S.add("pool", lambda e, tg=tg, ts=ts: e.tensor_tensor(out=tg, in0=tg, in1=ts, op=ALU.mult),
                          reads=[("TG", tb), ("TS", tb)], writes=[("TG", tb)])
                    S.add("dve", lambda e, tu=tu, tg=tg, j=j, tok=tok: e.scalar_tensor_tensor(out=ACTT[:, j, tok], in0=tu, scalar=1.0, in1=tg, op0=ALU.add, op1=ALU.mult),
                          reads=[("TU", tb), ("TG", tb)], writes=[("ACTT", j, c)])
            for t in range(NT):
                for h in range(2):
                    pi = 6 + (ycnt % 2)
                    ycnt += 1
                    py = PS[pi]
                    for k in range(NCH):
                        S.add("pe", lambda e, py=py, k=k, t=t, h=h: e.matmul(py[:], ACTT[:, k, t * 128:(t + 1) * 128], WD[:, k, h * 512:(h + 1) * 512], start=(k == 0), stop=(k == 7)),
                              reads=[("ACTT", k, t // 4), ("WD", k // 4)], writes=[("ps", pi)])
                    S.add("dve", lambda e, py=py, t=t, h=h, e_=e_: e.scalar_tensor_tensor(out=X[:, t, h * 512:(h + 1) * 512], in0=py[:], scalar=self.GALL[:, t, e_:e_ + 1],
                                                                                          in1=X[:, t, h * 512:(h + 1) * 512], op0=ALU.mult, op1=ALU.add),
                          reads=[("ps", pi), ("G", t), ("X", t)], writes=[("X", t)])

    def ple(self, l):
        S, d, X, XT, PS = self.S, self.d, self.X, self.XT, self.PS
        self.make_xt(l, scale=None, router=False)
        WG = self.alloc([NCH, D], BF16)
        WP = self.alloc([2, D], BF16)
        PT_ = self.alloc([2, S_TOK], BF16)
        BG = self.alloc([D], parts=1)
        SG = [self.alloc([D]) for _ in range(2)]
        kWG, kWP, kPT, kBG = self.key("WG"), self.key("WP"), self.key("PT"), self.key("BG")
        S.add("pool", lambda e: e.dma_start(out=WG, in_=d["ple_gate_w"][l].rearrange("(c p) f -> p c f", p=128)), writes=[kWG], dma="pwg")
        S.add("pool", lambda e: e.dma_start(out=WP, in_=d["ple_w"][l].rearrange("(c p) f -> p c f", p=128)), writes=[kWP], dma="pwp")
        S.add("pool", lambda e: e.dma_start(out=PT_, in_=d["pT"][l].rearrange("(c p) s -> p c s", p=128)), writes=[kPT], dma="ppt")
        S.add("sp", lambda e: e.dma_start(out=BG, in_=d["ple_gate_b"][l:l + 1, :]), writes=[kBG], dma="pbg")
        for t in range(NT):
            sg = SG[t % 2]
            for h in range(2):
                pg = PS[4 + h]
                pp = PS[6 + h]
                cols = slice(h * 512, (h + 1) * 512)
                for k in range(NCH):
                    S.add("pe", lambda e, pg=pg, k=k, t=t, cols=cols: e.matmul(pg[:], XT[:, k, t * 128:(t + 1) * 128], WG[:, k, cols], start=(k == 0), stop=False),
                          reads=[("XT", t), kWG], writes=[("ps", 4 + h)])
                S.add("pe", lambda e, pg=pg, cols=cols: e.matmul(pg[:], self.ONES[0:1, :], BG[0:1, cols], start=False, stop=True),
                      reads=["ONES", kBG], writes=[("ps", 4 + h)])
                for k in range(2):
                    S.add("pe", lambda e, pp=pp, k=k, t=t, cols=cols: e.matmul(pp[:], PT_[:, k, t * 128:(t + 1) * 128], WP[:, k, cols], start=(k == 0), stop=(k == 1)),
                          reads=[kPT, kWP], writes=[("ps", 6 + h)])
                S.add("act", lambda e, sg=sg, pg=pg, cols=cols: e.activation(out=sg[:, cols], in_=pg[:], func=AF.Sigmoid),
                      reads=[("ps", 4 + h)], writes=[("SG", t % 2, h)])
                S.add("dve", lambda e, sg=sg, pp=pp, cols=cols: e.tensor_tensor(out=sg[:, cols], in0=pp[:], in1=sg[:, cols], op=ALU.mult),
                      reads=[("ps", 6 + h), ("SG", t % 2, h)], writes=[("SG", t % 2, h)])
                S.add("pool", lambda e, sg=sg, t=t, cols=cols: e.tensor_tensor(out=X[:, t, cols], in0=X[:, t, cols], in1=sg[:, cols], op=ALU.add),
                      reads=[("SG", t % 2, h), ("X", t)], writes=[("X", t)])

    def retention(self, l):
        S, d, X, XT, PS = self.S, self.d, self.X, self.XT, self.PS
        jl = l // 2
        w_in = d["ret_w_in"][jl].rearrange("(c p) f -> p c f", p=128)
        w_out = d["ret_w_out"][jl]
        gam = self.consts["rgam"]
        COS = self.alloc([S_TOK])
        SIN = self.alloc([S_TOK])
        QT = self.alloc([2, S_TOK], BF16)
        KT = self.alloc([2, S_TOK], BF16)
        V = self.alloc([NT, 512], BF16)
        WC = self.alloc([NCH, 512], BF16)
        WO = self.alloc([4, D], BF16)
        MASK = self.alloc([4, 3, 256])
        kC, kSn, kM = self.key("cos"), self.key("sin"), self.key("rmask")
        S.add("sp", lambda e: e.dma_start(out=COS, in_=d["cos"]), writes=[kC], dma="cos")
        S.add("sp", lambda e: e.dma_start(out=SIN, in_=d["sin"]), writes=[kSn], dma="sin")
        S.add("sp", lambda e: e.dma_start(out=MASK, in_=d["rmask"]), writes=[kM], dma="rmask")
        mark = self.aptr
        PSB = [p[:].bitcast(BF16) for p in PS]
        for h in range(4):
            if h > 0:
                S.barrier()
            self.aptr = mark
            WA = self.alloc([NCH, 512], BF16)
            WB = self.alloc([NCH, 512], BF16)
            T1 = [self.alloc([512]) for _ in range(2)]
            T2 = [self.alloc([512]) for _ in range(2)]
            kWA, kWB, kWC, kWO = self.key("WA"), self.key("WB"), self.key("WC"), self.key("WO")
            S.add("pool", lambda e, h=h, WA=WA: e.dma_start(out=WA[:, :, 0:256], in_=w_in[:, :, h * 256:(h + 1) * 256]), writes=[(kWA, 0)], dma="rwa0")
            S.add("pool", lambda e, h=h, WA=WA: e.dma_start(out=WA[:, :, 256:512], in_=w_in[:, :, 1024 + h * 256:1024 + (h + 1) * 256]), writes=[(kWA, 1)], dma="rwa1")
            S.add("pool", lambda e, h=h, WB=WB: e.dma_start(out=WB, in_=w_in[:, :, 2048 + h * 512:2048 + (h + 1) * 512]), writes=[kWB], dma="rwb")
            S.add("pool", lambda e, h=h: e.dma_start(out=WC, in_=w_in[:, :, 4096 + h * 512:4096 + (h + 1) * 512]), writes=[kWC], dma="rwc")
            S.add("pool", lambda e, h=h: e.dma_start(out=WO, in_=w_out[h * 512:(h + 1) * 512, :].rearrange("(c p) f -> p c f", p=128)), writes=[kWO], dma="rwo")
            u = 0
            for qk in range(2):
                DST = QT if qk == 0 else KT
                for c in range(4):
                    tok = slice(c * 512, (c + 1) * 512)
                    p1, p2 = PS[2 * (u % 2)], PS[2 * (u % 2) + 1]
                    tb = u % 2
                    u += 1
                    for a, pp in ((0, p1), (1, p2)):
                        for kk in range(NCH):
                            S.add("pe", lambda e, pp=pp, kk=kk, a=a, qk=qk, tok=tok, WA=WA: e.matmul(pp[:], WA[:, kk, qk * 256 + a * 128:qk * 256 + (a + 1) * 128], XT[:, kk, tok],
                                                                                              start=(kk == 0), stop=(kk == 7)),
                                  reads=[(kWA, qk)] + [("XT", tt) for tt in range(4 * c, 4 * c + 4)], writes=[("ps", 2 * tb + a)])
                    t1, t2 = T1[tb], T2[tb]
                    k1, k2 = ("T1", tb), ("T2", tb)
                    S.add("dve", lambda e, t1=t1, p1=p1, tok=tok: e.tensor_tensor(out=t1, in0=p1[:], in1=COS[:, tok], op=ALU.mult), reads=[("ps", 2 * tb), kC], writes=[k1])
                    S.add("dve", lambda e, t2=t2, p2=p2, tok=tok: e.tensor_tensor(out=t2, in0=p2[:], in1=SIN[:, tok], op=ALU.mult), reads=[("ps", 2 * tb + 1), kSn], writes=[k2])
                    S.add("pool", lambda e, t1=t1, t2=t2, DST=DST, tok=tok: e.tensor_tensor(out=DST[:, 0, tok], in0=t1, in1=t2, op=ALU.subtract), reads=[k1, k2], writes=[("QK", qk, c, 0)])
                    S.add("dve", lambda e, t1=t1, p1=p1, tok=tok: e.tensor_tensor(out=t1, in0=p1[:], in1=SIN[:, tok], op=ALU.mult), reads=[("ps", 2 * tb), kSn], writes=[k1])
                    S.add("dve", lambda e, t2=t2, p2=p2, tok=tok: e.tensor_tensor(out=t2, in0=p2[:], in1=COS[:, tok], op=ALU.mult), reads=[("ps", 2 * tb + 1), kC], writes=[k2])
                    S.add("pool", lambda e, t1=t1, t2=t2, DST=DST, tok=tok: e.tensor_tensor(out=DST[:, 1, tok], in0=t1, in1=t2, op=ALU.add), reads=[k1, k2], writes=[("QK", qk, c, 1)])
            for t in range(NT):
                pv = PS[4 + t % 2]
                for kk in range(NCH):
                    S.add("pe", lambda e, pv=pv, kk=kk, t=t, WB=WB: e.matmul(pv[:], XT[:, kk, t * 128:(t + 1) * 128], WB[:, kk, :], start=(kk == 0), stop=(kk == 7)),
                          reads=[kWB, ("XT", t)], writes=[("ps", 4 + t % 2)])
                S.add("act", lambda e, pv=pv, t=t: e.activation(out=V[:, t, :], in_=pv[:], func=AF.Copy), reads=[("ps", 4 + t % 2)], writes=[("V", t)])
            S.barrier()
            self.aptr = mark
            PT = self.alloc([NT, 256], BF16)
            RB_ = [self.alloc([512]) for _ in range(2)]
            SG = [self.alloc([512]) for _ in range(2)]
            Y = [self.alloc([512], BF16) for _ in range(2)]
            YT = [self.alloc([4, 128], BF16) for _ in range(2)]
            ST = [self.alloc([16]) for _ in range(2)]
            sc = 0
            for c in range(8):
                q0 = 256 * c
                nk = 2 * c + 2
                for ks in range(nk):
                    pi = sc % 3
                    sc += 1
                    pss = PS[pi]
                    for a in range(2):
                        S.add("pe", lambda e, pss=pss, a=a, ks=ks, q0=q0: e.matmul(pss[:, 0:256], KT[:, a, ks * 128:(ks + 1) * 128], QT[:, a, q0:q0 + 256], start=(a == 0), stop=(a == 1)),
                              reads=[], writes=[("ps", pi)])
                    if ks >= 2 * c:
                        S.add("dve", lambda e, pss=pss, ks=ks, c=c, h=h: e.tensor_tensor(out=PT[:, ks, :], in0=pss[:, 0:256], in1=MASK[:, h, 1 + ks - 2 * c, :], op=ALU.mult),
                              reads=[("ps", pi), kM], writes=[("PT", ks)])
                    else:
                        off = q0 - 128 * ks
                        gv = float(gam[h] ** off)
                        S.add("dve", lambda e, pss=pss, ks=ks, gv=gv, h=h: e.scalar_tensor_tensor(out=PT[:, ks, :], in0=pss[:, 0:256], scalar=gv, in1=MASK[:, h, 0, :], op0=ALU.mult, op1=ALU.mult),
                              reads=[("ps", pi), kM], writes=[("PT", ks)])
                for qi in range(2):
                    qt = 2 * c + qi
                    b = qt % 2
                    po, pg, ptr_, px0, px1 = PS[3], PS[4], PSB[5], PS[6], PS[7]
                    for ks in range(qt + 1):
                        S.add("pe", lambda e, po=po, ks=ks, qi=qi, qt=qt: e.matmul(po[:], PT[:, ks, qi * 128:(qi + 1) * 128], V[:, ks, :], start=(ks == 0), stop=(ks == qt)),
                              reads=[("PT", ks), ("V", ks)], writes=[("ps", 3)])
                    for kk in range(NCH):
                        S.add("pe", lambda e, pg=pg, kk=kk, qt=qt: e.matmul(pg[:], XT[:, kk, qt * 128:(qt + 1) * 128], WC[:, kk, :], start=(kk == 0), stop=(kk == 7)),
                              reads=[kWC, ("XT", qt)], writes=[("ps", 4)])
                    st, rb, sg, y, yt = ST[b], RB_[b], SG[b], Y[b], YT[b]
                    ks_ = ("gn", b)
                    S.add("dve", lambda e, st=st, po=po: e.bn_stats(out=st[:, 0:6], in_=po[:]), reads=[("ps", 3)], writes=[(ks_, 0)])
                    S.add("dve", lambda e, st=st: e.bn_aggr(out=st[:, 6:8], in_=st[:, 0:6]), reads=[(ks_, 0)], writes=[(ks_, 1)])
                    S.add("dve", lambda e, st=st: e.tensor_scalar(out=st[:, 8:9], in0=st[:, 7:8], scalar1=float(GN_EPS), scalar2=None, op0=ALU.add), reads=[(ks_, 1)], writes=[(ks_, 2)])
                    S.add("act", lambda e, st=st: e.activation(out=st[:, 8:9], in_=st[:, 8:9], func=AF.Sqrt), reads=[(ks_, 2)], writes=[(ks_, 2)])
                    S.add("dve", lambda e, st=st: e.reciprocal(out=st[:, 9:10], in_=st[:, 8:9]), reads=[(ks_, 2)], writes=[(ks_, 3)])
                    S.add("dve", lambda e, st=st, po=po, rb=rb: e.tensor_scalar(out=rb, in0=po[:], scalar1=st[:, 6:7], scalar2=st[:, 9:10], op0=ALU.subtract, op1=ALU.mult),
                          reads=[("ps", 3), (ks_, 1), (ks_, 3)], writes=[("RB", b)])
                    S.add("act", lambda e, sg=sg, pg=pg: e.activation(out=sg, in_=pg[:], func=AF.Silu), reads=[("ps", 4)], writes=[("SGr", b)])
                    S.add("pool", lambda e, y=y, rb=rb, sg=sg: e.tensor_tensor(out=y, in0=rb, in1=sg, op=ALU.mult), reads=[("RB", b), ("SGr", b)], writes=[("Y", b)])
                    for a in range(4):
                        S.add("pe", lambda e, ptr_=ptr_, a=a, y=y: e.transpose(ptr_[:, a * 128:(a + 1) * 128], y[:, a * 128:(a + 1) * 128], self.IDB[:]),
                              reads=[("Y", b), "IDB"], writes=[("ps", 5)])
                    S.add("act", lambda e, ptr_=ptr_, yt=yt: e.activation(out=yt, in_=ptr_[:, 0:512].rearrange("p (a b) -> p a b", a=4), func=AF.Copy), reads=[("ps", 5)], writes=[("YT", b)])
                    for hh, px in ((0, px0), (1, px1)):
                        for a in range(4):
                            S.add("pe", lambda e, px=px, a=a, yt=yt, hh=hh: e.matmul(px[:], yt[:, a, :], WO[:, a, hh * 512:(hh + 1) * 512], start=(a == 0), stop=(a == 3)),
                                  reads=[("YT", b), kWO], writes=[("ps", 6 + hh)])
                        S.add("dve", lambda e, px=px, qt=qt, hh=hh: e.tensor_tensor(out=X[:, qt, hh * 512:(hh + 1) * 512], in0=px[:], in1=X[:, qt, hh * 512:(hh + 1) * 512], op=ALU.add),
                              reads=[("ps", 6 + hh), ("X", qt)], writes=[("X", qt)])
        S.barrier()
        self.arena_reset()

    def stickbreak(self, l):
        S, d, X, XT, PS = self.S, self.d, self.X, self.XT, self.PS
        jl = l // 2
        w_in = d["sb_w_in"][jl].rearrange("(c p) f -> p c f", p=128)
        w_out = d["sb_w_out"][jl]
        PSB = [p[:].bitcast(BF16) for p in PS]
        SBM = self.alloc([128])
        kSBM = self.key("sbm")
        S.add("sp", lambda e: e.dma_start(out=SBM, in_=d["sbmask"]), writes=[kSBM], dma="sbm")
        WQ = [self.alloc([NCH, 128], BF16) for _ in range(2)]
        WK = [self.alloc([NCH, 128], BF16) for _ in range(2)]
        WV = [self.alloc([NCH, 128], BF16) for _ in range(2)]
        WOp = [self.alloc([D], BF16) for _ in range(2)]
        QT = self.alloc([S_TOK], BF16)
        KT = self.alloc([S_TOK], BF16)
        V = self.alloc([NT, 128], BF16)
        Z = [self.alloc([S_TOK]) for _ in range(2)]
        B1 = [self.alloc([S_TOK]) for _ in range(2)]
        B2 = [self.alloc([S_TOK]) for _ in range(2)]
        A = [self.alloc([S_TOK], BF16) for _ in range(2)]
        AT = [self.alloc([NT, 128], BF16) for _ in range(2)]
        SM = [self.alloc([8]) for _ in range(2)]
        OS = [self.alloc([128], BF16) for _ in range(2)]
        OT = [self.alloc([128], BF16) for _ in range(2)]
        for s_ in range(2):
            S.add("pool", lambda e, s_=s_: e.memset(B2[s_][:, 0:1], 0.0), writes=[("B2", s_)])

        def load_w(m):
            wb = m % 2
            S.add("pool", lambda e, m=m, wb=wb: e.dma_start(out=WQ[wb], in_=w_in[:, :, m * 128:(m + 1) * 128]), writes=[("WQ", wb)], dma=f"swq{wb}")
            S.add("pool", lambda e, m=m, wb=wb: e.dma_start(out=WK[wb], in_=w_in[:, :, 1024 + m * 128:1024 + (m + 1) * 128]), writes=[("WK", wb)], dma=f"swk{wb}")
            S.add("pool", lambda e, m=m, wb=wb: e.dma_start(out=WV[wb], in_=w_in[:, :, 2048 + m * 128:2048 + (m + 1) * 128]), writes=[("WV", wb)], dma=f"swv{wb}")
            S.add("pool", lambda e, m=m, wb=wb: e.dma_start(out=WOp[wb], in_=w_out[m * 128:(m + 1) * 128, :]), writes=[("WOp", wb)], dma=f"swo{wb}")

        load_w(0)
        unit = 0
        for m in range(8):
            wb = m % 2
            if m + 1 < 8:
                load_w(m + 1)
            for c in range(4):
                tok = slice(c * 512, (c + 1) * 512)
                for which, W_, DST, scl in ((0, WQ[wb], QT, 0.125), (1, WK[wb], KT, 1.0)):
                    pp = PS[which]
                    for kk in range(NCH):
                        S.add("pe", lambda e, pp=pp, kk=kk, W_=W_, tok=tok: e.matmul(pp[:], W_[:, kk, :], XT[:, kk, tok], start=(kk == 0), stop=(kk == 7)),
                              reads=[("WQ" if which == 0 else "WK", wb)] + [("XT", tt) for tt in range(4 * c, 4 * c + 4)], writes=[("ps", which)])
                    S.add("act", lambda e, pp=pp, DST=DST, tok=tok, scl=scl: e.activation(out=DST[:, tok], in_=pp[:], func=AF.Copy, scale=float(scl)),
                          reads=[("ps", which)], writes=[("QTKT", which, c)])
            for t in range(NT):
                pv = PS[2 + t % 2]
                for kk in range(NCH):
                    S.add("pe", lambda e, pv=pv, kk=kk, t=t, wb=wb: e.matmul(pv[:, 0:128], XT[:, kk, t * 128:(t + 1) * 128], WV[wb][:, kk, :], start=(kk == 0), stop=(kk == 7)),
                          reads=[("WV", wb), ("XT", t)], writes=[("ps", 2 + t % 2)])
                S.add("dve", lambda e, pv=pv, t=t: e.tensor_copy(out=V[:, t, :], in_=pv[:, 0:128]), reads=[("ps", 2 + t % 2)], writes=[("V", t)])
            for qt in range(NT):
                n = 128 * (qt + 1)
                ob = qt % 2
                for hh in range(2):
                    sb_ = unit % 2
                    unit += 1
                    z, b1, b2, a_, at, sm = Z[sb_], B1[sb_], B2[sb_], A[sb_], AT[sb_], SM[sb_]
                    hp = slice(64 * hh, 64 * hh + 64)
                    nkb = (n + 511) // 512
                    for kb in range(nkb):
                        w = min(512, n - 512 * kb)
                        pz = PS[kb % 2]
                        S.add("pe", lambda e, pz=pz, w=w, kb=kb, hp=hp, qt=qt: e.matmul(pz[:, 0:w], QT[hp, qt * 128:(qt + 1) * 128], KT[hp, 512 * kb:512 * kb + w], start=True, stop=True),
                              reads=[("QTKT", 0, qt // 4)] + [("QTKT", 1, cc) for cc in range(kb, kb + 1)], writes=[("ps", kb % 2)])
                        last = (kb == nkb - 1)
                        wc = w - 128 if last else w
                        if wc > 0:
                            S.add("act", lambda e, pz=pz, z=z, kb=kb, wc=wc: e.activation(out=z[:, 512 * kb:512 * kb + wc], in_=pz[:, 0:wc], func=AF.Copy),
                                  reads=[("ps", kb % 2)], writes=[("Z", sb_, kb)])
                        if last:
                            S.add("dve", lambda e, pz=pz, z=z, kb=kb, w=w: e.tensor_tensor(out=z[:, 512 * kb + w - 128:512 * kb + w], in0=pz[:, w - 128:w], in1=SBM, op=ALU.add),
                                  reads=[("ps", kb % 2), kSBM], writes=[("Zd", sb_)])
                    zk = [("Z", sb_, kb) for kb in range(nkb)] + [("Zd", sb_)]
                    S.add("act", lambda e, z=z, b1=b1, n=n: e.activation(out=b1[:, 0:n], in_=z[:, 0:n], func=AF.Exp), reads=zk, writes=[("B1", sb_)])
                    S.add("act", lambda e, b1=b1, n=n, sm=sm: e.activation(out=b1[:, 0:n], in_=b1[:, 0:n], func=AF.Ln, bias=1.0, scale=1.0, accum_out=sm[:, 0:1]),
                          reads=[("B1", sb_)], writes=[("B1", sb_), ("TT", sb_)])
                    S.add("dve", lambda e, sm=sm: e.tensor_scalar(out=sm[:, 1:2], in0=sm[:, 0:1], scalar1=-1.0, scalar2=None, op0=ALU.mult), reads=[("TT", sb_)], writes=[("NTT", sb_)])
                    S.add("dve", lambda e, b1=b1, b2=b2, n=n: e.tensor_tensor_scan(out=b2[:, 1:n], data0=b1[:, 0:n - 1], data1=b1[:, 0:n - 1], initial=0.0, op0=ALU.add, op1=ALU.max),
                          reads=[("B1", sb_)], writes=[("B2", sb_)])
                    S.add("pool", lambda e, z=z, b2=b2, n=n: e.tensor_tensor(out=z[:, 0:n], in0=z[:, 0:n], in1=b2[:, 0:n], op=ALU.add), reads=zk + [("B2", sb_)], writes=[("Zw", sb_)] + zk)
                    S.add("act", lambda e, z=z, a_=a_, n=n, sm=sm: e.activation(out=a_[:, 0:n], in_=z[:, 0:n], func=AF.Exp, bias=sm[:, 1:2], scale=1.0),
                          reads=[("Zw", sb_), ("NTT", sb_)] + zk, writes=[("A", sb_)])
                    for g in range((qt + 8) // 8):
                        nb = min(8, qt + 1 - 8 * g)
                        ptb = PSB[2 + g % 2]
                        for i in range(nb):
                            kb2 = 8 * g + i
                            S.add("pe", lambda e, ptb=ptb, i=i, kb2=kb2, a_=a_: e.transpose(ptb[:, i * 128:(i + 1) * 128], a_[:, kb2 * 128:(kb2 + 1) * 128], self.IDB[:]),
                                  reads=[("A", sb_), "IDB"], writes=[("ps", 2 + g % 2)])
                        S.add("dve", lambda e, ptb=ptb, at=at, g=g, nb=nb: e.tensor_copy(out=at[:, 8 * g:8 * g + nb, :], in_=ptb[:, 0:nb * 128].rearrange("p (a b) -> p a b", a=nb)),
                              reads=[("ps", 2 + g % 2)], writes=[("AT", sb_, g)])
                    po = PS[4 + hh]
                    for kb2 in range(qt + 1):
                        S.add("pe", lambda e, po=po, kb2=kb2, at=at, hp=hp, qt=qt: e.matmul(po[:, 0:64], at[:, kb2, :], V[:, kb2, hp], start=(kb2 == 0), stop=(kb2 == qt)),
                              reads=[("AT", sb_, kb2 // 8), ("V", kb2)], writes=[("ps", 4 + hh)])
                    S.add("act", lambda e, po=po, hp=hp, ob=ob: e.activation(out=OS[ob][:, hp], in_=po[:, 0:64], func=AF.Copy), reads=[("ps", 4 + hh)], writes=[("OS", ob, hh)])
                ptb = PSB[3]
                S.add("pe", lambda e, ptb=ptb, ob=ob: e.transpose(ptb[:, 0:128], OS[ob], self.IDB[:]), reads=[("OS", ob, 0), ("OS", ob, 1), "IDB"], writes=[("ps", 3)])
                S.add("dve", lambda e, ptb=ptb, ob=ob: e.tensor_copy(out=OT[ob], in_=ptb[:, 0:128]), reads=[("ps", 3)], writes=[("OT", ob)])
                for hf in range(2):
                    px = PS[6 + hf]
                    S.add("pe", lambda e, px=px, ob=ob, hf=hf, wb=wb: e.matmul(px[:], OT[ob], WOp[wb][:, hf * 512:(hf + 1) * 512], start=True, stop=True),
                          reads=[("OT", ob), ("WOp", wb)], writes=[("ps", 6 + hf)])
                    S.add("dve", lambda e, px=px, qt=qt, hf=hf: e.tensor_tensor(out=X[:, qt, hf * 512:(hf + 1) * 512], in0=px[:], in1=X[:, qt, hf * 512:(hf + 1) * 512], op=ALU.add),
                          reads=[("ps", 6 + hf), ("X", qt)], writes=[("X", qt)])
        S.barrier()
        self.arena_reset()


def _host_inputs(inp, b, consts, big=True):
    f = lambda a: np.ascontiguousarray(np.asarray(a, dtype=np.float32))
    m = {}
    m["x"] = f(inp["x"][b])
    m["pT"] = f(np.transpose(np.asarray(inp["p"])[:, b], (0, 2, 1)))
    for k in ("ret_w_in", "ret_w_out", "sb_w_in", "sb_w_out", "router_w", "router_b", "w_gate_up", "w_down",
              "b_down", "ple_w", "ple_gate_w", "ple_gate_b"):
        if not big and k in ("w_gate_up", "w_down"):
            m[k] = f(np.asarray(inp[k])[0:1, 0:1])
        else:
            m[k] = f(inp[k])
    m["lnp"] = f(np.stack([inp["ln1_g"], inp["ln1_b"], inp["ln2_g"], inp["ln2_b"]], axis=1))
    bgu = np.asarray(inp["b_gate_up"], dtype=np.float32).reshape(DEPTH, NE, 8, 128, 2)
    m["bgu"] = f(np.transpose(bgu, (0, 3, 1, 2, 4)).reshape(DEPTH, 128, NE * 16))
    for k in ("identf", "identb", "ones", "cos", "sin", "rmask", "sbmask"):
        m[k] = consts[k]
    return m


def run_layers(inp, layers, n_exp=NE, stages=("mix", "moe", "ple"), cores=8):
    bld = Builder(layers, n_exp=n_exp, stages=stages)
    nc = bld.build()
    in_maps = [_host_inputs(inp, b, bld.consts, big=("moe" in stages)) for b in range(cores)]
    res = run_bass_kernel_spmd(nc, in_maps, core_ids=list(range(cores)))
    return np.stack([res.results[b]["y"] for b in range(cores)], axis=0)


def kernel(**inputs):
    out = run_layers(inputs, layers=range(DEPTH))
    return out.astype(np.float32)
```

```python
import math
from contextlib import ExitStack

import ml_dtypes
import numpy as np

import concourse.bass as bass
import concourse.mybir as mybir
from concourse.bass_utils import run_bass_kernel_spmd

F32 = mybir.dt.float32
BF16 = mybir.dt.bfloat16
ALU = mybir.AluOpType
AF = mybir.ActivationFunctionType
AX = mybir.AxisListType

S_TOK = 2048
D = 1024
NT = 16
NCH = 8
DEPTH = 4
NE = 32
DN_ALPHA = float((2 * DEPTH) ** 0.25)
LN_EPS = 1e-5
GN_EPS = 1e-6
SEG = 30000
CAP = 64


class _Op:
    __slots__ = ("eng", "fn", "deps", "dma", "sig", "sigval", "idx")


class Sched:
    ENGS = ("pe", "act", "dve", "pool", "sp")

    def __init__(self, nc):
        self.nc = nc
        self.streams = {e: [] for e in self.ENGS}
        self.last_w = {}
        self.readers = {}
        self.dma_cnt = {}
        self.dma_last = {}
        self.barrier_ops = []
        self.barrier_seen = {e: True for e in self.ENGS}

    def barrier(self):
        ops = []
        for e in self.ENGS:
            for op in reversed(self.streams[e]):
                if op.dma is None:
                    ops.append(op)
                    break
        ops.extend(self.dma_last.values())
        self.barrier_ops = ops
        self.barrier_seen = {e: False for e in self.ENGS}
        self.last_w = {}
        self.readers = {}

    def add(self, eng, fn, reads=(), writes=(), dma=None):
        op = _Op()
        op.eng, op.fn, op.dma = eng, fn, None
        op.sig = False
        op.sigval = None
        ps_r = [k for k in reads if isinstance(k, tuple) and k[0] == "ps"]
        if ps_r:
            reads = [k for k in reads if not (isinstance(k, tuple) and k[0] == "ps")]
            writes = list(writes) + ps_r
        deps = {}
        for k in reads:
            w = self.last_w.get(k)
            if w is not None:
                deps[w] = True
        for k in writes:
            w = self.last_w.get(k)
            if w is not None:
                deps[w] = True
            for r in self.readers.get(k, ()):
                if r not in deps:
                    deps[r] = False
        if not self.barrier_seen[eng]:
            self.barrier_seen[eng] = True
            for b in self.barrier_ops:
                deps[b] = True
        for k in reads:
            self.readers.setdefault(k, []).append(op)
        for k in writes:
            self.last_w[k] = op
            self.readers[k] = []
        pruned = []
        latest = {}
        for d, strong in deps.items():
            if d is op:
                continue
            if d.dma is not None:
                pruned.append(d)
                continue
            if d.eng == eng and (eng == "pe" or not strong):
                continue
            o = latest.get(d.eng)
            if o is None or o.idx < d.idx:
                latest[d.eng] = d
        pruned.extend(latest.values())
        op.deps = pruned
        op.idx = len(self.streams[eng])
        if dma is not None:
            c = self.dma_cnt.get(dma, 0) + 1
            self.dma_cnt[dma] = c
            op.dma = (dma, 16 * c)
            self.dma_last[dma] = op
        self.streams[eng].append(op)
        return op

    def emit(self):
        nc = self.nc
        for e in self.ENGS:
            for op in self.streams[e]:
                for d in op.deps:
                    if d.dma is None:
                        d.sig = True
        nsegs = {}
        for e in self.ENGS:
            c = 0
            for op in self.streams[e]:
                if op.sig and op.dma is None:
                    op.sigval = (e, c // SEG, c % SEG + 1)
                    c += 1
            nsegs[e] = (c + SEG - 1) // SEG
        with ExitStack() as es:
            sems = {}
            for e in self.ENGS:
                for s in range(nsegs[e]):
                    sems[(e, s)] = es.enter_context(nc.semaphore(f"s_{e}_{s}"))
            dsems = {}
            for g in self.dma_cnt:
                dsems[g] = es.enter_context(nc.semaphore(f"d_{g}"))
            self.n_sems = len(sems) + len(dsems)
            block = es.enter_context(nc.Block())

            def run(ename, eng):
                waited = {}
                for op in self.streams[ename]:
                    need = {}
                    for d in op.deps:
                        if d.dma is not None:
                            key, val = ("d", d.dma[0]), d.dma[1]
                        else:
                            key, val = (d.sigval[0], d.sigval[1]), d.sigval[2]
                        if need.get(key, 0) < val:
                            need[key] = val
                    for key, val in need.items():
                        if waited.get(key, 0) >= val:
                            continue
                        waited[key] = val
                        sem = dsems[key[1]] if key[0] == "d" else sems[key]
                        eng.wait_ge(sem, val)
                    ins = op.fn(eng)
                    if op.dma is not None:
                        ins.then_inc(dsems[op.dma[0]], 16)
                    elif op.sig:
                        ins.then_inc(sems[(op.sigval[0], op.sigval[1])], 1)

            block.tensor(lambda eng: run("pe", eng))
            block.scalar(lambda eng: run("act", eng))
            block.vector(lambda eng: run("dve", eng))
            block.gpsimd(lambda eng: run("pool", eng))
            block.sync(lambda eng: run("sp", eng))


def _consts():
    c = {}
    c["identf"] = np.eye(128, dtype=np.float32)
    c["identb"] = np.eye(128, dtype=np.float32).astype(ml_dtypes.bfloat16)
    c["ones"] = np.ones((1, 128), dtype=np.float32)
    ii = np.arange(128)
    c["ut"] = (ii[:, None] < ii[None, :]).astype(np.float32).astype(ml_dtypes.bfloat16)
    c["iota"] = np.stack([ii[None, :] - CAP * q + 0 * ii[:, None] for q in range(128 // CAP)], axis=1).astype(np.float32)
    half = 128
    inv_freq = (1.0 / (10000.0 ** (np.arange(half, dtype=np.float32) / np.float32(half)))).astype(np.float32)
    ang = (np.arange(S_TOK, dtype=np.float32)[None, :] * inv_freq[:, None]).astype(np.float32)
    c["cos"] = np.cos(ang).astype(np.float32)
    c["sin"] = np.sin(ang).astype(np.float32)
    H = 4
    lg = np.log(1.0 - 2.0 ** (-5.0 - np.arange(H, dtype=np.float64)))
    sl = np.arange(128)[:, None].astype(np.float64)
    tl = np.arange(256)[None, :].astype(np.float64)
    masks = np.zeros((H, 3, 128, 256), dtype=np.float32)
    for h in range(H):
        masks[h, 0] = np.exp(lg[h] * (tl - sl)) / 16.0
        for m in range(2):
            s_abs = 128 * m + sl
            dec = np.exp(lg[h] * np.abs(tl - s_abs))
            ok = (np.floor(s_abs / 64) <= np.floor(tl / 64))
            masks[h, 1 + m] = dec * ok / 16.0
    c["rmask"] = np.ascontiguousarray(masks.transpose(2, 0, 1, 3))
    c["rgam"] = np.exp(lg)
    t = np.arange(128)[:, None]
    s = np.arange(128)[None, :]
    c["sbmask"] = np.where(s < t, 0.0, -30000.0).astype(np.float32)
    return c


class Builder:
    def __init__(self, layers, n_exp=NE, stages=("mix", "moe", "ple")):
        self.layers = list(layers)
        self.n_exp = n_exp
        self.stages = stages
        self.nc = bass.Bass("TRN2", target_bir_lowering=False)
        self.consts = _consts()

    def dram_in(self, name, shape, dt=F32):
        return self.nc.dram_tensor(name, list(shape), dt, kind="ExternalInput").ap()

    def build(self):
        nc = self.nc
        L = len(self.layers)
        d = {}
        d["x"] = self.dram_in("x", [S_TOK, D])
        d["pT"] = self.dram_in("pT", [DEPTH, 256, S_TOK])
        d["ret_w_in"] = self.dram_in("ret_w_in", [2, D, 6144])
        d["ret_w_out"] = self.dram_in("ret_w_out", [2, 2048, D])
        d["sb_w_in"] = self.dram_in("sb_w_in", [2, D, 3 * D])
        d["sb_w_out"] = self.dram_in("sb_w_out", [2, D, D])
        d["lnp"] = self.dram_in("lnp", [DEPTH, 4, D])
        d["router_w"] = self.dram_in("router_w", [DEPTH, D, NE])
        d["router_b"] = self.dram_in("router_b", [DEPTH, NE])
        big = "moe" in self.stages
        d["w_gate_up"] = self.dram_in("w_gate_up", [DEPTH, NE, D, 2 * D] if big else [1, 1, D, 2 * D])
        d["bgu"] = self.dram_in("bgu", [DEPTH, 128, NE * 16])
        d["w_down"] = self.dram_in("w_down", [DEPTH, NE, D, D] if big else [1, 1, D, D])
        d["b_down"] = self.dram_in("b_down", [DEPTH, NE, D])
        d["ple_w"] = self.dram_in("ple_w", [DEPTH, 256, D])
        d["ple_gate_w"] = self.dram_in("ple_gate_w", [DEPTH, D, D])
        d["ple_gate_b"] = self.dram_in("ple_gate_b", [DEPTH, D])
        d["identf"] = self.dram_in("identf", [128, 128])
        d["identb"] = self.dram_in("identb", [128, 128], BF16)
        d["ones"] = self.dram_in("ones", [1, 128])
        d["ut"] = self.dram_in("ut", [128, 128], BF16)
        d["iota"] = self.dram_in("iota", [128, 128 // CAP, 128])
        d["cos"] = self.dram_in("cos", [128, S_TOK])
        d["sin"] = self.dram_in("sin", [128, S_TOK])
        d["rmask"] = self.dram_in("rmask", [128, 4, 3, 256])
        d["sbmask"] = self.dram_in("sbmask", [128, 128])
        d["y"] = nc.dram_tensor("y", [S_TOK, D], F32, kind="ExternalOutput").ap()
        self.d = d

        AW = 27648
        with ExitStack() as es:
            sb = lambda n, s, dt: es.enter_context(nc.sbuf_tensor(n, s, dt))
            self.X = sb("X", [128, NT, D], F32)
            self.XT = sb("XT", [128, NCH, S_TOK], BF16)
            self.IDF = sb("IDF", [128, 128], F32)
            self.IDB = sb("IDB", [128, 128], BF16)
            self.ONES = sb("ONES", [1, 128], F32)
            self.GALL = sb("GALL", [128, NT, NE], F32)
            self.ARENA = sb("ARENA", [128, AW], F32)
            self.AW = AW
            self.PS = [es.enter_context(nc.psum_tensor(f"ps{i}", [128, 512], F32)) for i in range(8)]
            self.S = Sched(nc)
            self.uid = 0
            self.program()
            self.S.emit()
        return nc

    def arena_reset(self):
        self.aptr = 0

    def alloc(self, shape, dt=F32, parts=128):
        n = int(np.prod(shape))
        words = n if dt == F32 else (n + 1) // 2
        words = (words + 7) // 8 * 8
        a = self.ARENA[0:parts, self.aptr:self.aptr + words]
        self.aptr += words
        assert self.aptr <= self.AW, f"arena overflow {self.aptr} > {self.AW}"
        if dt != F32:
            a = a.bitcast(dt)[:, 0:n]
        else:
            a = a[:, 0:n]
        if len(shape) == 2:
            a = a.rearrange("p (a b) -> p a b", a=shape[0])
        elif len(shape) == 3:
            a = a.rearrange("p (a b c) -> p a b c", a=shape[0], b=shape[1])
        return a

    def key(self, base):
        self.uid += 1
        return (base, self.uid)

    def program(self):
        S, d = self.S, self.d
        X = self.X
        S.add("sp", lambda e: e.dma_start(out=self.IDF[:], in_=d["identf"]), writes=["IDF"], dma="c0")
        S.add("sp", lambda e: e.dma_start(out=self.IDB[:], in_=d["identb"]), writes=["IDB"], dma="c1")
        S.add("sp", lambda e: e.dma_start(out=self.ONES[:], in_=d["ones"]), writes=["ONES"], dma="c2")
        xin = d["x"].rearrange("(t p) f -> p t f", p=128)
        for q in range(4):
            S.add("sp", lambda e, q=q: e.dma_start(out=X[:, 4 * q:4 * q + 4, :], in_=xin[:, 4 * q:4 * q + 4, :]),
                  writes=[("X", t) for t in range(4 * q, 4 * q + 4)], dma=f"xin{q}")
        for l in self.layers:
            if "mix" in self.stages:
                self.arena_reset()
                S.barrier()
                self.make_xt(l, scale=DN_ALPHA, router=False)
                if l % 2 == 0:
                    self.retention(l)
                else:
                    self.stickbreak(l)
                self.layernorm(l, 0)
            if "moe" in self.stages:
                self.arena_reset()
                S.barrier()
                self.moe(l)
                self.layernorm(l, 1)
            if "ple" in self.stages:
                self.arena_reset()
                S.barrier()
                self.ple(l)
        yout = d["y"].rearrange("(t p) f -> p t f", p=128)
        for q in range(4):
            S.add("sp", lambda e, q=q: e.dma_start(out=yout[:, 4 * q:4 * q + 4, :], in_=X[:, 4 * q:4 * q + 4, :]),
                  reads=[("X", t) for t in range(4 * q, 4 * q + 4)], writes=[("yout", q)], dma=f"yout{q}")
        S.add("sp", lambda e: e.nop(), reads=[("yout", q) for q in range(4)])

    def make_xt(self, l, scale=None, router=False):
        S, d, X, XT, PS = self.S, self.d, self.X, self.XT, self.PS
        if router:
            RW = self.alloc([NCH, NE])
            RB = self.alloc([NE], parts=1)
            XT32 = [self.alloc([NCH, 128]) for _ in range(2)]
            LG = [self.alloc([NE]) for _ in range(2)]
            EXm = [self.alloc([NE]) for _ in range(2)]
            MSK = [self.alloc([NE]) for _ in range(2)]
            SM = [self.alloc([16]) for _ in range(2)]
            UT = self.alloc([128], BF16)
            kUT = self.key("UT")
            S.add("sp", lambda e: e.dma_start(out=UT, in_=d["ut"]), writes=[kUT], dma="ut")
            XB = self.XB
            kRW, kRB = self.key("RW"), self.key("RB")
            S.add("sp", lambda e: e.dma_start(out=RW, in_=d["router_w"][l].rearrange("(c p) n -> p c n", p=128)),
                  writes=[kRW], dma="rw")
            S.add("sp", lambda e: e.dma_start(out=RB, in_=d["router_b"][l:l + 1, :]), writes=[kRB], dma="rb")
        for t in range(NT):
            b = t % 2
            for h in range(2):
                pb = PS[2 * b + h]
                for j in range(4):
                    c = 4 * h + j
                    S.add("pe", lambda e, pb=pb, j=j, c=c, t=t: e.transpose(pb[:, j * 128:(j + 1) * 128], X[:, t, c * 128:(c + 1) * 128], self.IDF[:]),
                          reads=[("X", t), "IDF"], writes=[("ps", 2 * b + h)])
                if not router:
                    S.add("act", lambda e, pb=pb, h=h, t=t: e.activation(out=XT[:, 4 * h:4 * h + 4, t * 128:(t + 1) * 128],
                                                                          in_=pb[:].rearrange("p (a b) -> p a b", a=4), func=AF.Copy),
                          reads=[("ps", 2 * b + h)], writes=[("XT", t)])
                if router:
                    S.add("dve", lambda e, pb=pb, h=h, b=b: e.tensor_copy(out=XT32[b][:, 4 * h:4 * h + 4, :],
                                                                           in_=pb[:].rearrange("p (a b) -> p a b", a=4)),
                          reads=[("ps", 2 * b + h)], writes=[("XT32", b, h)])
            if router:
                S.add("act", lambda e, t=t: e.activation(out=XB[:, t, :], in_=X[:, t, :], func=AF.Copy), reads=[("X", t)], writes=[("XB", t)])
            if scale is not None:
                S.add("pool", lambda e, t=t: e.tensor_scalar(out=X[:, t, :], in0=X[:, t, :], scalar1=float(scale), scalar2=None, op0=ALU.mult),
                      reads=[("X", t)], writes=[("X", t)])
            if router:
                pl = PS[4 + b]
                for c in range(NCH):
                    S.add("pe", lambda e, pl=pl, c=c, b=b: e.matmul(pl[:, 0:NE], XT32[b][:, c, :], RW[:, c, :], start=(c == 0), stop=False),
                          reads=[("XT32", b, c // 4), kRW], writes=[("ps", 4 + b)])
                S.add("pe", lambda e, pl=pl: e.matmul(pl[:, 0:NE], self.ONES[0:1, :], RB[0:1, :], start=False, stop=True),
                      reads=["ONES", kRB], writes=[("ps", 4 + b)])
                lg, ex, mk, sm = LG[b], EXm[b], MSK[b], SM[b]
                kl = ("rt", b)
                S.add("dve", lambda e, pl=pl, lg=lg: e.tensor_copy(out=lg, in_=pl[:, 0:NE]), reads=[("ps", 4 + b)], writes=[(kl, "lg")])
                S.add("dve", lambda e, lg=lg, sm=sm: e.max(out=sm[:, 0:8], in_=lg), reads=[(kl, "lg")], writes=[(kl, "top")])
                S.add("dve", lambda e, lg=lg, sm=sm, mk=mk: e.tensor_scalar(out=mk, in0=lg, scalar1=sm[:, 3:4], scalar2=None, op0=ALU.is_ge),
                      reads=[(kl, "lg"), (kl, "top")], writes=[(kl, "mk")])
                S.add("dve", lambda e, sm=sm: e.tensor_scalar(out=sm[:, 8:9], in0=sm[:, 0:1], scalar1=-1.0, scalar2=None, op0=ALU.mult),
                      reads=[(kl, "top")], writes=[(kl, "nm")])
                S.add("dve", lambda e, mk=mk, t=t: e.tensor_copy(out=self.MB[:, t, :], in_=mk), reads=[(kl, "mk")], writes=[("MB", t)])
                S.add("pe", lambda e, pl=pl, t=t: e.matmul(pl[:, 64:64 + NE], UT, self.MB[:, t, :], start=True, stop=True),
                      reads=[("MB", t), kUT], writes=[("ps", 4 + b)])
                S.add("dve", lambda e, pl=pl, mk=mk, t=t: e.scalar_tensor_tensor(out=self.VAL[:, t, :], in0=pl[:, 64:64 + NE], scalar=float(CAP) - 0.5, in1=mk, op0=ALU.is_lt, op1=ALU.mult),
                      reads=[("ps", 4 + b), (kl, "mk")], writes=[("VAL", t)])
                S.add("dve", lambda e, pl=pl, t=t: e.tensor_copy(out=self.RK[:, t, :], in_=pl[:, 64:64 + NE]), reads=[("ps", 4 + b)], writes=[("RK", t)])
                S.add("act", lambda e, lg=lg, ex=ex, sm=sm: e.activation(out=ex, in_=lg, func=AF.Exp, bias=sm[:, 8:9], scale=1.0),
                      reads=[(kl, "lg"), (kl, "nm")], writes=[(kl, "ex")])
                S.add("dve", lambda e, ex=ex, mk=mk: e.tensor_tensor(out=ex, in0=ex, in1=mk, op=ALU.mult),
                      reads=[(kl, "ex"), (kl, "mk")], writes=[(kl, "ex")])
                S.add("dve", lambda e, ex=ex, sm=sm: e.reduce_sum(out=sm[:, 9:10], in_=ex, axis=AX.X),
                      reads=[(kl, "ex")], writes=[(kl, "ss")])
                S.add("dve", lambda e, sm=sm: e.reciprocal(out=sm[:, 10:11], in_=sm[:, 9:10]), reads=[(kl, "ss")], writes=[(kl, "rs")])
                S.add("dve", lambda e, ex=ex, sm=sm, t=t: e.tensor_scalar(out=self.GALL[:, t, :], in0=ex, scalar1=sm[:, 10:11], scalar2=None, op0=ALU.mult),
                      reads=[(kl, "ex"), (kl, "rs")], writes=[("G", t)])
                pg = PS[6 + b]
                S.add("pe", lambda e, pg=pg, t=t: e.transpose(pg[0:NE, 0:128], self.GALL[:, t, :], self.IDF[:]),
                      reads=[("G", t), "IDF"], writes=[("ps", 6 + b)])
                S.add("act", lambda e, pg=pg, t=t: e.activation(out=self.GT[0:NE, t * 128:(t + 1) * 128], in_=pg[0:NE, 0:128], func=AF.Copy),
                      reads=[("ps", 6 + b)], writes=[("GT", t)])

    def ln_alloc(self):
        return (self.alloc([D]), self.alloc([D]), [self.alloc([16]) for _ in range(2)])

    def layernorm(self, l, which, bufs=None):
        S, d, X = self.S, self.d, self.X
        G, B, ST = bufs if bufs is not None else self.ln_alloc()
        kG, kB = self.key("lng"), self.key("lnb")
        S.add("sp", lambda e: e.dma_start(out=G, in_=d["lnp"][l, 2 * which:2 * which + 1, :].to_broadcast([128, D])), writes=[kG], dma="lng")
        S.add("sp", lambda e: e.dma_start(out=B, in_=d["lnp"][l, 2 * which + 1:2 * which + 2, :].to_broadcast([128, D])), writes=[kB], dma="lnb")
        for t in range(NT):
            st = ST[t % 2]
            ks = ("lnst", t % 2)
            xt = X[:, t, :]
            S.add("dve", lambda e, st=st, xt=xt: e.bn_stats(out=st[:, 0:6], in_=xt[:, 0:512]), reads=[("X", t)], writes=[(ks, 0)])
            S.add("dve", lambda e, st=st, xt=xt: e.bn_stats(out=st[:, 6:12], in_=xt[:, 512:1024]), reads=[("X", t)], writes=[(ks, 1)])
            S.add("dve", lambda e, st=st: e.bn_aggr(out=st[:, 12:14], in_=st[:, 0:12]),
                  reads=[(ks, 0), (ks, 1)], writes=[(ks, 2)])
            S.add("dve", lambda e, st=st: e.tensor_scalar(out=st[:, 14:15], in0=st[:, 13:14], scalar1=float(LN_EPS), scalar2=None, op0=ALU.add),
                  reads=[(ks, 2)], writes=[(ks, 3)])
            S.add("act", lambda e, st=st: e.activation(out=st[:, 14:15], in_=st[:, 14:15], func=AF.Sqrt), reads=[(ks, 3)], writes=[(ks, 3)])
            S.add("dve", lambda e, st=st: e.reciprocal(out=st[:, 15:16], in_=st[:, 14:15]), reads=[(ks, 3)], writes=[(ks, 4)])
            S.add("dve", lambda e, st=st, xt=xt: e.tensor_scalar(out=xt, in0=xt, scalar1=st[:, 12:13], scalar2=st[:, 15:16], op0=ALU.subtract, op1=ALU.mult),
                  reads=[("X", t), (ks, 2), (ks, 4)], writes=[("X", t)])
            S.add("pool", lambda e, xt=xt: e.tensor_tensor(out=xt, in0=xt, in1=G, op=ALU.mult), reads=[("X", t), kG], writes=[("X", t)])
            S.add("pool", lambda e, xt=xt: e.tensor_tensor(out=xt, in0=xt, in1=B, op=ALU.add), reads=[("X", t), kB], writes=[("X", t)])

    def moe_bias(self, l, GT, BD, BGU):
        S, d, X, PS = self.S, self.d, self.X, self.PS
        kBD, kBGU = self.key("BD"), self.key("BGU")
        self.moe_keys = (kBD, kBGU)
        S.add("sp", lambda e: e.dma_start(out=BD, in_=d["b_down"][l]), writes=[kBD], dma="bd")
        S.add("sp", lambda e: e.dma_start(out=BGU, in_=d["bgu"][l]), writes=[kBGU], dma="bgu")
        for t in range(NT):
            for h in range(2):
                pb = PS[6 + h]
                S.add("pe", lambda e, pb=pb, t=t, h=h: e.matmul(pb[:], GT[0:NE, t * 128:(t + 1) * 128], BD[0:NE, h * 512:(h + 1) * 512], start=True, stop=True),
                      reads=[("GT", t), kBD], writes=[("ps", 6 + h)])
                S.add("dve", lambda e, pb=pb, t=t, h=h: e.tensor_tensor(out=X[:, t, h * 512:(h + 1) * 512], in0=pb[:], in1=X[:, t, h * 512:(h + 1) * 512], op=ALU.add),
                      reads=[("ps", 6 + h), ("X", t)], writes=[("X", t)])

    def moe(self, l):
        S, d, X, PS = self.S, self.d, self.X, self.PS
        PSB = [p[:].bitcast(BF16) for p in PS]
        NQ = 128 // CAP
        NSL = NT * CAP
        NSC = NSL // 512
        NST = NSL // 128
        self.XB = self.XT[:].rearrange("p c s -> p (c s)").rearrange("p (t f) -> p t f", t=NT)
        XB = self.XB
        BGU = self.alloc([NE * 16])
        self.RK = self.alloc([NT, NE])
        self.VAL = self.alloc([NT, NE])
        self.MB = self.alloc([NT, NE], BF16)
        IOTA = self.alloc([NQ, 128])
        kIO = self.key("iota")
        S.add("sp", lambda e: e.dma_start(out=IOTA, in_=d["iota"]), writes=[kIO], dma="iota")
        mark = self.aptr
        self.GT = self.alloc([S_TOK], parts=32)
        BD = self.alloc([D], parts=32)
        self.make_xt(l, scale=DN_ALPHA, router=True)
        self.moe_bias(l, self.GT, BD, BGU)
        S.barrier()
        self.aptr = mark
        kBD, kBGU = self.moe_keys
        RK, VAL = self.RK, self.VAL
        XG = self.alloc([NCH, NSL], BF16)
        ACTT = self.alloc([NCH, NSL], BF16)
        YS = self.alloc([NST, D], BF16)
        PA = self.alloc([NT, 128], BF16)
        PTA = self.alloc([NT, 128], BF16)
        NSLOT = 4
        WGU = [self.alloc([NCH, 256], BF16) for _ in range(NSLOT)]
        WD = self.alloc([NCH, D], BF16)
        NTMP = 2
        TG = [self.alloc([512]) for _ in range(NTMP)]
        TU = [self.alloc([512]) for _ in range(NTMP)]
        TS = [self.alloc([512]) for _ in range(NTMP)]
        wgu_src = d["w_gate_up"]
        wd_src = d["w_down"]
        n_chunks = self.n_exp * 8

        def dma_wgu(g):
            if g >= n_chunks:
                return
            slot = g % NSLOT
            wsrc = wgu_src[l, g // 8].rearrange("(c p) f -> p c f", p=128)
            j = g % 8
            S.add("pool", lambda e, slot=slot, j=j, wsrc=wsrc: e.dma_start(out=WGU[slot], in_=wsrc[:, :, 256 * j:256 * (j + 1)]),
                  writes=[("WGU", slot)], dma=f"wgu{slot}")

        def dma_wd(ei):
            src = wd_src[l, ei].rearrange("(c p) f -> p c f", p=128)
            for q in range(2):
                S.add("pool", lambda e, q=q, src=src: e.dma_start(out=WD[:, 4 * q:4 * q + 4, :], in_=src[:, 4 * q:4 * q + 4, :]),
                      writes=[("WD", q)], dma=f"wd{q}")

        def build_sel(ei):
            for t in range(NT):
                S.add("dve", lambda e, t=t, ei=ei: e.tensor_scalar(out=PA[:, t, :], in0=IOTA[:, t % NQ, :], scalar1=RK[:, t, ei:ei + 1], scalar2=VAL[:, t, ei:ei + 1],
                                                                    op0=ALU.is_equal, op1=ALU.mult),
                      reads=[kIO], writes=[("PA", t)])

        for g in range(NSLOT - 1):
            dma_wgu(g)
        build_sel(0)
        unit = 0
        gcnt = 0
        ycnt = 0
        for ei in range(self.n_exp):
            for g8 in range(NT // 8):
                ptb = PSB[6 + g8]
                for i in range(8):
                    t = 8 * g8 + i
                    S.add("pe", lambda e, ptb=ptb, i=i, t=t: e.transpose(ptb[:, i * 128:(i + 1) * 128], PA[:, t, :], self.IDB[:]),
                          reads=[("PA", t), "IDB"], writes=[("ps", 6 + g8)])
                S.add("act", lambda e, ptb=ptb, g8=g8: e.activation(out=PTA[:, 8 * g8:8 * g8 + 8, :], in_=ptb[:].rearrange("p (a b) -> p a b", a=8), func=AF.Copy),
                      reads=[("ps", 6 + g8)], writes=[("PTA", g8)])
            tpc = 512 // CAP
            for dch in range(NCH):
                for sc in range(NSC):
                    bi = gcnt % 6
                    gcnt += 1
                    pb = PS[bi]
                    for i in range(tpc):
                        t = sc * tpc + i
                        q = t % NQ
                        S.add("pe", lambda e, pb=pb, i=i, t=t, q=q, dch=dch: e.matmul(pb[:, i * CAP:(i + 1) * CAP], XB[:, t, dch * 128:(dch + 1) * 128], PA[:, t, q * CAP:(q + 1) * CAP], start=True, stop=True),
                              reads=[("PA", t)], writes=[("ps", bi)])
                    eng = "act" if (gcnt % 2 == 0) else "dve"
                    if eng == "act":
                        S.add("act", lambda e, pb=pb, dch=dch, sc=sc: e.activation(out=XG[:, dch, sc * 512:(sc + 1) * 512], in_=pb[:], func=AF.Copy), reads=[("ps", bi)], writes=[("XG", dch, sc)])
                    else:
                        S.add("dve", lambda e, pb=pb, dch=dch, sc=sc: e.tensor_copy(out=XG[:, dch, sc * 512:(sc + 1) * 512], in_=pb[:]), reads=[("ps", bi)], writes=[("XG", dch, sc)])
            for j in range(8):
                dma_wgu(ei * 8 + j + NSLOT - 1)
                if j == 1:
                    dma_wd(ei)
                slot = (ei * 8 + j) % NSLOT
                W = WGU[slot]
                for c in range(NSC):
                    pr = unit % 3
                    pg_, pu_ = PS[2 * pr], PS[2 * pr + 1]
                    tb = unit % NTMP
                    unit += 1
                    tok = slice(c * 512, (c + 1) * 512)
                    for k in range(NCH):
                        S.add("pe", lambda e, pg_=pg_, W=W, k=k, tok=tok: e.matmul(pg_[:], W[:, k, 0:256:2], XG[:, k, tok], start=(k == 0), stop=(k == 7)),
                              reads=[("WGU", slot), ("XG", k, c)], writes=[("ps", 2 * pr)])
                    for k in range(NCH):
                        S.add("pe", lambda e, pu_=pu_, W=W, k=k, tok=tok: e.matmul(pu_[:], W[:, k, 1:256:2], XG[:, k, tok], start=(k == 0), stop=(k == 7)),
                              reads=[("WGU", slot), ("XG", k, c)], writes=[("ps", 2 * pr + 1)])
                    bg = BGU[:, ei * 16 + 2 * j:ei * 16 + 2 * j + 1]
                    bu = BGU[:, ei * 16 + 2 * j + 1:ei * 16 + 2 * j + 2]
                    tg, tu, ts = TG[tb], TU[tb], TS[tb]
                    S.add("dve", lambda e, tg=tg, pg_=pg_, bg=bg: e.tensor_scalar(out=tg, in0=pg_[:], scalar1=bg, scalar2=7.0, op0=ALU.add, op1=ALU.min),
                          reads=[("ps", 2 * pr), kBGU], writes=[("TG", tb)])
                    S.add("act", lambda e, tu=tu, pu_=pu_, bu=bu: e.activation(out=tu, in_=pu_[:], func=AF.Identity, bias=bu, scale=1.0),
                          reads=[("ps", 2 * pr + 1), kBGU], writes=[("TU", tb)])
                    S.add("act", lambda e, ts=ts, tg=tg: e.activation(out=ts, in_=tg, func=AF.Sigmoid, scale=1.702),
                          reads=[("TG", tb)], writes=[("TS", tb)])
                    S.add("pool", lambda e, tu=tu: e.tensor_scalar(out=tu, in0=tu, scalar1=7.0, scalar2=-7.0, op0=ALU.min, op1=ALU.max),
                          reads=[("TU", tb)], writes=[("TU", tb)])
                    S.add("pool", lambda e, tg=tg, ts=ts: e.tensor_tensor(out=tg, in0=tg, in1=ts, op=ALU.mult),
                          reads=[("TG", tb), ("TS", tb)], writes=[("TG", tb)])
                    S.add("dve", lambda e, tu=tu, tg=tg, j=j, tok=tok: e.scalar_tensor_tensor(out=ACTT[:, j, tok], in0=tu, scalar=1.0, in1=tg, op0=ALU.add, op1=ALU.mult),
                          reads=[("TU", tb), ("TG", tb)], writes=[("ACTT", j, c)])
            for st in range(NST):
                for h in range(2):
                    pi = 6 + (ycnt % 2)
                    ycnt += 1
                    py = PS[pi]
                    for k in range(NCH):
                        S.add("pe", lambda e, py=py, k=k, st=st, h=h: e.matmul(py[:], ACTT[:, k, st * 128:(st + 1) * 128], WD[:, k, h * 512:(h + 1) * 512], start=(k == 0), stop=(k == 7)),
                              reads=[("ACTT", k, st // 4), ("WD", k // 4)], writes=[("ps", pi)])
                    S.add("act", lambda e, py=py, st=st, h=h: e.activation(out=YS[:, st, h * 512:(h + 1) * 512], in_=py[:], func=AF.Copy), reads=[("ps", pi)], writes=[("YS", st, h)])
            if ei + 1 < self.n_exp:
                build_sel(ei + 1)
            for t in range(NT):
                for h in range(2):
                    pi = 6 + (ycnt % 2)
                    ycnt += 1
                    py = PS[pi]
                    S.add("pe", lambda e, py=py, t=t, h=h: e.matmul(py[:], PTA[:, t, :], YS[:, t // NQ, h * 512:(h + 1) * 512], start=True, stop=True),
                          reads=[("PTA", t // 8), ("YS", t // NQ, h)], writes=[("ps", pi)])
                    S.add("dve", lambda e, py=py, t=t, h=h, ei=ei: e.scalar_tensor_tensor(out=X[:, t, h * 512:(h + 1) * 512], in0=py[:], scalar=self.GALL[:, t, ei:ei + 1],
                                                                                          in1=X[:, t, h * 512:(h + 1) * 512], op0=ALU.mult, op1=ALU.add),
                          reads=[("ps", pi), ("X", t)], writes=[("X", t)])
        S.barrier()
        self.arena_reset()

    def ple(self, l):
        S, d, X, XT, PS = self.S, self.d, self.X, self.XT, self.PS
        self.make_xt(l, scale=None, router=False)
        WG = self.alloc([NCH, D], BF16)
        WP = self.alloc([2, D], BF16)
        PT_ = self.alloc([2, S_TOK], BF16)
        BG = self.alloc([D], parts=1)
        SG = [self.alloc([D]) for _ in range(2)]
        kWG, kWP, kPT, kBG = self.key("WG"), self.key("WP"), self.key("PT"), self.key("BG")
        S.add("pool", lambda e: e.dma_start(out=WG, in_=d["ple_gate_w"][l].rearrange("(c p) f -> p c f", p=128)), writes=[kWG], dma="pwg")
        S.add("pool", lambda e: e.dma_start(out=WP, in_=d["ple_w"][l].rearrange("(c p) f -> p c f", p=128)), writes=[kWP], dma="pwp")
        S.add("pool", lambda e: e.dma_start(out=PT_, in_=d["pT"][l].rearrange("(c p) s -> p c s", p=128)), writes=[kPT], dma="ppt")
        S.add("sp", lambda e: e.dma_start(out=BG, in_=d["ple_gate_b"][l:l + 1, :]), writes=[kBG], dma="pbg")
        for t in range(NT):
            sg = SG[t % 2]
            for h in range(2):
                pg = PS[4 + h]
                pp = PS[6 + h]
                cols = slice(h * 512, (h + 1) * 512)
                for k in range(NCH):
                    S.add("pe", lambda e, pg=pg, k=k, t=t, cols=cols: e.matmul(pg[:], XT[:, k, t * 128:(t + 1) * 128], WG[:, k, cols], start=(k == 0), stop=False),
                          reads=[("XT", t), kWG], writes=[("ps", 4 + h)])
                S.add("pe", lambda e, pg=pg, cols=cols: e.matmul(pg[:], self.ONES[0:1, :], BG[0:1, cols], start=False, stop=True),
                      reads=["ONES", kBG], writes=[("ps", 4 + h)])
                for k in range(2):
                    S.add("pe", lambda e, pp=pp, k=k, t=t, cols=cols: e.matmul(pp[:], PT_[:, k, t * 128:(t + 1) * 128], WP[:, k, cols], start=(k == 0), stop=(k == 1)),
                          reads=[kPT, kWP], writes=[("ps", 6 + h)])
                S.add("act", lambda e, sg=sg, pg=pg, cols=cols: e.activation(out=sg[:, cols], in_=pg[:], func=AF.Sigmoid),
                      reads=[("ps", 4 + h)], writes=[("SG", t % 2, h)])
                S.add("dve", lambda e, sg=sg, pp=pp, cols=cols: e.tensor_tensor(out=sg[:, cols], in0=pp[:], in1=sg[:, cols], op=ALU.mult),
                      reads=[("ps", 6 + h), ("SG", t % 2, h)], writes=[("SG", t % 2, h)])
                S.add("pool", lambda e, sg=sg, t=t, cols=cols: e.tensor_tensor(out=X[:, t, cols], in0=X[:, t, cols], in1=sg[:, cols], op=ALU.add),
                      reads=[("SG", t % 2, h), ("X", t)], writes=[("X", t)])

    def retention(self, l):
        S, d, X, XT, PS = self.S, self.d, self.X, self.XT, self.PS
        jl = l // 2
        w_in = d["ret_w_in"][jl].rearrange("(c p) f -> p c f", p=128)
        w_out = d["ret_w_out"][jl]
        gam = self.consts["rgam"]
        COS = self.alloc([S_TOK])
        SIN = self.alloc([S_TOK])
        QT = self.alloc([2, S_TOK], BF16)
        KT = self.alloc([2, S_TOK], BF16)
        V = self.alloc([NT, 512], BF16)
        WC = self.alloc([NCH, 512], BF16)
        WO = self.alloc([4, D], BF16)
        MASK = self.alloc([4, 3, 256])
        kC, kSn, kM = self.key("cos"), self.key("sin"), self.key("rmask")
        S.add("sp", lambda e: e.dma_start(out=COS, in_=d["cos"]), writes=[kC], dma="cos")
        S.add("sp", lambda e: e.dma_start(out=SIN, in_=d["sin"]), writes=[kSn], dma="sin")
        S.add("sp", lambda e: e.dma_start(out=MASK, in_=d["rmask"]), writes=[kM], dma="rmask")
        mark = self.aptr
        PSB = [p[:].bitcast(BF16) for p in PS]
        for h in range(4):
            if h > 0:
                S.barrier()
            self.aptr = mark
            WA = self.alloc([NCH, 512], BF16)
            WB = self.alloc([NCH, 512], BF16)
            T1 = [self.alloc([512]) for _ in range(2)]
            T2 = [self.alloc([512]) for _ in range(2)]
            kWA, kWB, kWC, kWO = self.key("WA"), self.key("WB"), self.key("WC"), self.key("WO")
            S.add("pool", lambda e, h=h, WA=WA: e.dma_start(out=WA[:, :, 0:256], in_=w_in[:, :, h * 256:(h + 1) * 256]), writes=[(kWA, 0)], dma="rwa0")
            S.add("pool", lambda e, h=h, WA=WA: e.dma_start(out=WA[:, :, 256:512], in_=w_in[:, :, 1024 + h * 256:1024 + (h + 1) * 256]), writes=[(kWA, 1)], dma="rwa1")
            S.add("pool", lambda e, h=h, WB=WB: e.dma_start(out=WB, in_=w_in[:, :, 2048 + h * 512:2048 + (h + 1) * 512]), writes=[kWB], dma="rwb")
            S.add("pool", lambda e, h=h: e.dma_start(out=WC, in_=w_in[:, :, 4096 + h * 512:4096 + (h + 1) * 512]), writes=[kWC], dma="rwc")
            S.add("pool", lambda e, h=h: e.dma_start(out=WO, in_=w_out[h * 512:(h + 1) * 512, :].rearrange("(c p) f -> p c f", p=128)), writes=[kWO], dma="rwo")
            u = 0
            for qk in range(2):
                DST = QT if qk == 0 else KT
                for c in range(4):
                    tok = slice(c * 512, (c + 1) * 512)
                    p1, p2 = PS[2 * (u % 2)], PS[2 * (u % 2) + 1]
                    tb = u % 2
                    u += 1
                    for a, pp in ((0, p1), (1, p2)):
                        for kk in range(NCH):
                            S.add("pe", lambda e, pp=pp, kk=kk, a=a, qk=qk, tok=tok, WA=WA: e.matmul(pp[:], WA[:, kk, qk * 256 + a * 128:qk * 256 + (a + 1) * 128], XT[:, kk, tok],
                                                                                              start=(kk == 0), stop=(kk == 7)),
                                  reads=[(kWA, qk)] + [("XT", tt) for tt in range(4 * c, 4 * c + 4)], writes=[("ps", 2 * tb + a)])
                    t1, t2 = T1[tb], T2[tb]
                    k1, k2 = ("T1", tb), ("T2", tb)
                    S.add("dve", lambda e, t1=t1, p1=p1, tok=tok: e.tensor_tensor(out=t1, in0=p1[:], in1=COS[:, tok], op=ALU.mult), reads=[("ps", 2 * tb), kC], writes=[k1])
                    S.add("dve", lambda e, t2=t2, p2=p2, tok=tok: e.tensor_tensor(out=t2, in0=p2[:], in1=SIN[:, tok], op=ALU.mult), reads=[("ps", 2 * tb + 1), kSn], writes=[k2])
                    S.add("pool", lambda e, t1=t1, t2=t2, DST=DST, tok=tok: e.tensor_tensor(out=DST[:, 0, tok], in0=t1, in1=t2, op=ALU.subtract), reads=[k1, k2], writes=[("QK", qk, c, 0)])
                    S.add("dve", lambda e, t1=t1, p1=p1, tok=tok: e.tensor_tensor(out=t1, in0=p1[:], in1=SIN[:, tok], op=ALU.mult), reads=[("ps", 2 * tb), kSn], writes=[k1])
                    S.add("dve", lambda e, t2=t2, p2=p2, tok=tok: e.tensor_tensor(out=t2, in0=p2[:], in1=COS[:, tok], op=ALU.mult), reads=[("ps", 2 * tb + 1), kC], writes=[k2])
                    S.add("pool", lambda e, t1=t1, t2=t2, DST=DST, tok=tok: e.tensor_tensor(out=DST[:, 1, tok], in0=t1, in1=t2, op=ALU.add), reads=[k1, k2], writes=[("QK", qk, c, 1)])
            for t in range(NT):
                pv = PS[4 + t % 2]
                for kk in range(NCH):
                    S.add("pe", lambda e, pv=pv, kk=kk, t=t, WB=WB: e.matmul(pv[:], XT[:, kk, t * 128:(t + 1) * 128], WB[:, kk, :], start=(kk == 0), stop=(kk == 7)),
                          reads=[kWB, ("XT", t)], writes=[("ps", 4 + t % 2)])
                S.add("act", lambda e, pv=pv, t=t: e.activation(out=V[:, t, :], in_=pv[:], func=AF.Copy), reads=[("ps", 4 + t % 2)], writes=[("V", t)])
            S.barrier()
            self.aptr = mark
            PT = self.alloc([NT, 256], BF16)
            RB_ = [self.alloc([512]) for _ in range(2)]
            SG = [self.alloc([512]) for _ in range(2)]
            Y = [self.alloc([512], BF16) for _ in range(2)]
            YT = [self.alloc([4, 128], BF16) for _ in range(2)]
            ST = [self.alloc([16]) for _ in range(2)]
            sc = 0
            for c in range(8):
                q0 = 256 * c
                nk = 2 * c + 2
                for ks in range(nk):
                    pi = sc % 3
                    sc += 1
                    pss = PS[pi]
                    for a in range(2):
                        S.add("pe", lambda e, pss=pss, a=a, ks=ks, q0=q0: e.matmul(pss[:, 0:256], KT[:, a, ks * 128:(ks + 1) * 128], QT[:, a, q0:q0 + 256], start=(a == 0), stop=(a == 1)),
                              reads=[], writes=[("ps", pi)])
                    if ks >= 2 * c:
                        S.add("dve", lambda e, pss=pss, ks=ks, c=c, h=h: e.tensor_tensor(out=PT[:, ks, :], in0=pss[:, 0:256], in1=MASK[:, h, 1 + ks - 2 * c, :], op=ALU.mult),
                              reads=[("ps", pi), kM], writes=[("PT", ks)])
                    else:
                        off = q0 - 128 * ks
                        gv = float(gam[h] ** off)
                        S.add("dve", lambda e, pss=pss, ks=ks, gv=gv, h=h: e.scalar_tensor_tensor(out=PT[:, ks, :], in0=pss[:, 0:256], scalar=gv, in1=MASK[:, h, 0, :], op0=ALU.mult, op1=ALU.mult),
                              reads=[("ps", pi), kM], writes=[("PT", ks)])
                for qi in range(2):
                    qt = 2 * c + qi
                    b = qt % 2
                    po, pg, ptr_, px0, px1 = PS[3], PS[4], PSB[5], PS[6], PS[7]
                    for ks in range(qt + 1):
                        S.add("pe", lambda e, po=po, ks=ks, qi=qi, qt=qt: e.matmul(po[:], PT[:, ks, qi * 128:(qi + 1) * 128], V[:, ks, :], start=(ks == 0), stop=(ks == qt)),
                              reads=[("PT", ks), ("V", ks)], writes=[("ps", 3)])
                    for kk in range(NCH):
                        S.add("pe", lambda e, pg=pg, kk=kk, qt=qt: e.matmul(pg[:], XT[:, kk, qt * 128:(qt + 1) * 128], WC[:, kk, :], start=(kk == 0), stop=(kk == 7)),
                              reads=[kWC, ("XT", qt)], writes=[("ps", 4)])
                    st, rb, sg, y, yt = ST[b], RB_[b], SG[b], Y[b], YT[b]
                    ks_ = ("gn", b)
                    S.add("dve", lambda e, st=st, po=po: e.bn_stats(out=st[:, 0:6], in_=po[:]), reads=[("ps", 3)], writes=[(ks_, 0)])
                    S.add("dve", lambda e, st=st: e.bn_aggr(out=st[:, 6:8], in_=st[:, 0:6]), reads=[(ks_, 0)], writes=[(ks_, 1)])
                    S.add("dve", lambda e, st=st: e.tensor_scalar(out=st[:, 8:9], in0=st[:, 7:8], scalar1=float(GN_EPS), scalar2=None, op0=ALU.add), reads=[(ks_, 1)], writes=[(ks_, 2)])
                    S.add("act", lambda e, st=st: e.activation(out=st[:, 8:9], in_=st[:, 8:9], func=AF.Sqrt), reads=[(ks_, 2)], writes=[(ks_, 2)])
                    S.add("dve", lambda e, st=st: e.reciprocal(out=st[:, 9:10], in_=st[:, 8:9]), reads=[(ks_, 2)], writes=[(ks_, 3)])
                    S.add("dve", lambda e, st=st, po=po, rb=rb: e.tensor_scalar(out=rb, in0=po[:], scalar1=st[:, 6:7], scalar2=st[:, 9:10], op0=ALU.subtract, op1=ALU.mult),
                          reads=[("ps", 3), (ks_, 1), (ks_, 3)], writes=[("RB", b)])
                    S.add("act", lambda e, sg=sg, pg=pg: e.activation(out=sg, in_=pg[:], func=AF.Silu), reads=[("ps", 4)], writes=[("SGr", b)])
                    S.add("pool", lambda e, y=y, rb=rb, sg=sg: e.tensor_tensor(out=y, in0=rb, in1=sg, op=ALU.mult), reads=[("RB", b), ("SGr", b)], writes=[("Y", b)])
                    for a in range(4):
                        S.add("pe", lambda e, ptr_=ptr_, a=a, y=y: e.transpose(ptr_[:, a * 128:(a + 1) * 128], y[:, a * 128:(a + 1) * 128], self.IDB[:]),
                              reads=[("Y", b), "IDB"], writes=[("ps", 5)])
                    S.add("act", lambda e, ptr_=ptr_, yt=yt: e.activation(out=yt, in_=ptr_[:, 0:512].rearrange("p (a b) -> p a b", a=4), func=AF.Copy), reads=[("ps", 5)], writes=[("YT", b)])
                    for hh, px in ((0, px0), (1, px1)):
                        for a in range(4):
                            S.add("pe", lambda e, px=px, a=a, yt=yt, hh=hh: e.matmul(px[:], yt[:, a, :], WO[:, a, hh * 512:(hh + 1) * 512], start=(a == 0), stop=(a == 3)),
                                  reads=[("YT", b), kWO], writes=[("ps", 6 + hh)])
                        S.add("dve", lambda e, px=px, qt=qt, hh=hh: e.tensor_tensor(out=X[:, qt, hh * 512:(hh + 1) * 512], in0=px[:], in1=X[:, qt, hh * 512:(hh + 1) * 512], op=ALU.add),
                              reads=[("ps", 6 + hh), ("X", qt)], writes=[("X", qt)])
        S.barrier()
        self.arena_reset()

    def stickbreak(self, l):
        S, d, X, XT, PS = self.S, self.d, self.X, self.XT, self.PS
        jl = l // 2
        w_in = d["sb_w_in"][jl].rearrange("(c p) f -> p c f", p=128)
        w_out = d["sb_w_out"][jl]
        PSB = [p[:].bitcast(BF16) for p in PS]
        SBM = self.alloc([128])
        kSBM = self.key("sbm")
        S.add("sp", lambda e: e.dma_start(out=SBM, in_=d["sbmask"]), writes=[kSBM], dma="sbm")
        WQ = [self.alloc([NCH, 128], BF16) for _ in range(2)]
        WK = [self.alloc([NCH, 128], BF16) for _ in range(2)]
        WV = [self.alloc([NCH, 128], BF16) for _ in range(2)]
        WOp = [self.alloc([D], BF16) for _ in range(2)]
        QT = self.alloc([S_TOK], BF16)
        KT = self.alloc([S_TOK], BF16)
        V = self.alloc([NT, 128], BF16)
        Z = [self.alloc([S_TOK]) for _ in range(2)]
        B1 = [self.alloc([S_TOK]) for _ in range(2)]
        B2 = [self.alloc([S_TOK]) for _ in range(2)]
        A = [self.alloc([S_TOK], BF16) for _ in range(2)]
        AT = [self.alloc([NT, 128], BF16) for _ in range(2)]
        SM = [self.alloc([8]) for _ in range(2)]
        OS = [self.alloc([128], BF16) for _ in range(2)]
        OT = [self.alloc([128], BF16) for _ in range(2)]
        for s_ in range(2):
            S.add("pool", lambda e, s_=s_: e.memset(B2[s_][:, 0:1], 0.0), writes=[("B2", s_)])

        def load_w(m):
            wb = m % 2
            S.add("pool", lambda e, m=m, wb=wb: e.dma_start(out=WQ[wb], in_=w_in[:, :, m * 128:(m + 1) * 128]), writes=[("WQ", wb)], dma=f"swq{wb}")
            S.add("pool", lambda e, m=m, wb=wb: e.dma_start(out=WK[wb], in_=w_in[:, :, 1024 + m * 128:1024 + (m + 1) * 128]), writes=[("WK", wb)], dma=f"swk{wb}")
            S.add("pool", lambda e, m=m, wb=wb: e.dma_start(out=WV[wb], in_=w_in[:, :, 2048 + m * 128:2048 + (m + 1) * 128]), writes=[("WV", wb)], dma=f"swv{wb}")
            S.add("pool", lambda e, m=m, wb=wb: e.dma_start(out=WOp[wb], in_=w_out[m * 128:(m + 1) * 128, :]), writes=[("WOp", wb)], dma=f"swo{wb}")

        load_w(0)
        unit = 0
        for m in range(8):
            wb = m % 2
            if m + 1 < 8:
                load_w(m + 1)
            for c in range(4):
                tok = slice(c * 512, (c + 1) * 512)
                for which, W_, DST, scl in ((0, WQ[wb], QT, 0.125), (1, WK[wb], KT, 1.0)):
                    pp = PS[which]
                    for kk in range(NCH):
                        S.add("pe", lambda e, pp=pp, kk=kk, W_=W_, tok=tok: e.matmul(pp[:], W_[:, kk, :], XT[:, kk, tok], start=(kk == 0), stop=(kk == 7)),
                              reads=[("WQ" if which == 0 else "WK", wb)] + [("XT", tt) for tt in range(4 * c, 4 * c + 4)], writes=[("ps", which)])
                    S.add("act", lambda e, pp=pp, DST=DST, tok=tok, scl=scl: e.activation(out=DST[:, tok], in_=pp[:], func=AF.Copy, scale=float(scl)),
                          reads=[("ps", which)], writes=[("QTKT", which, c)])
            for t in range(NT):
                pv = PS[2 + t % 2]
                for kk in range(NCH):
                    S.add("pe", lambda e, pv=pv, kk=kk, t=t, wb=wb: e.matmul(pv[:, 0:128], XT[:, kk, t * 128:(t + 1) * 128], WV[wb][:, kk, :], start=(kk == 0), stop=(kk == 7)),
                          reads=[("WV", wb), ("XT", t)], writes=[("ps", 2 + t % 2)])
                S.add("dve", lambda e, pv=pv, t=t: e.tensor_copy(out=V[:, t, :], in_=pv[:, 0:128]), reads=[("ps", 2 + t % 2)], writes=[("V", t)])
            for qt in range(NT):
                n = 128 * (qt + 1)
                ob = qt % 2
                for hh in range(2):
                    sb_ = unit % 2
                    unit += 1
                    z, b1, b2, a_, at, sm = Z[sb_], B1[sb_], B2[sb_], A[sb_], AT[sb_], SM[sb_]
                    hp = slice(64 * hh, 64 * hh + 64)
                    nkb = (n + 511) // 512
                    for kb in range(nkb):
                        w = min(512, n - 512 * kb)
                        pz = PS[kb % 2]
                        S.add("pe", lambda e, pz=pz, w=w, kb=kb, hp=hp, qt=qt: e.matmul(pz[:, 0:w], QT[hp, qt * 128:(qt + 1) * 128], KT[hp, 512 * kb:512 * kb + w], start=True, stop=True),
                              reads=[("QTKT", 0, qt // 4)] + [("QTKT", 1, cc) for cc in range(kb, kb + 1)], writes=[("ps", kb % 2)])
                        last = (kb == nkb - 1)
                        wc = w - 128 if last else w
                        if wc > 0:
                            S.add("act", lambda e, pz=pz, z=z, kb=kb, wc=wc: e.activation(out=z[:, 512 * kb:512 * kb + wc], in_=pz[:, 0:wc], func=AF.Copy),
                                  reads=[("ps", kb % 2)], writes=[("Z", sb_, kb)])
                        if last:
                            S.add("dve", lambda e, pz=pz, z=z, kb=kb, w=w: e.tensor_tensor(out=z[:, 512 * kb + w - 128:512 * kb + w], in0=pz[:, w - 128:w], in1=SBM, op=ALU.add),
                                  reads=[("ps", kb % 2), kSBM], writes=[("Zd", sb_)])
                    zk = [("Z", sb_, kb) for kb in range(nkb)] + [("Zd", sb_)]
                    S.add("act", lambda e, z=z, b1=b1, n=n: e.activation(out=b1[:, 0:n], in_=z[:, 0:n], func=AF.Exp), reads=zk, writes=[("B1", sb_)])
                    S.add("act", lambda e, b1=b1, n=n, sm=sm: e.activation(out=b1[:, 0:n], in_=b1[:, 0:n], func=AF.Ln, bias=1.0, scale=1.0, accum_out=sm[:, 0:1]),
                          reads=[("B1", sb_)], writes=[("B1", sb_), ("TT", sb_)])
                    S.add("dve", lambda e, sm=sm: e.tensor_scalar(out=sm[:, 1:2], in0=sm[:, 0:1], scalar1=-1.0, scalar2=None, op0=ALU.mult), reads=[("TT", sb_)], writes=[("NTT", sb_)])
                    S.add("dve", lambda e, b1=b1, b2=b2, n=n: e.tensor_tensor_scan(out=b2[:, 1:n], data0=b1[:, 0:n - 1], data1=b1[:, 0:n - 1], initial=0.0, op0=ALU.add, op1=ALU.max),
                          reads=[("B1", sb_)], writes=[("B2", sb_)])
                    S.add("pool", lambda e, z=z, b2=b2, n=n: e.tensor_tensor(out=z[:, 0:n], in0=z[:, 0:n], in1=b2[:, 0:n], op=ALU.add), reads=zk + [("B2", sb_)], writes=[("Zw", sb_)] + zk)
                    S.add("act", lambda e, z=z, a_=a_, n=n, sm=sm: e.activation(out=a_[:, 0:n], in_=z[:, 0:n], func=AF.Exp, bias=sm[:, 1:2], scale=1.0),
                          reads=[("Zw", sb_), ("NTT", sb_)] + zk, writes=[("A", sb_)])
                    for g in range((qt + 8) // 8):
                        nb = min(8, qt + 1 - 8 * g)
                        ptb = PSB[2 + g % 2]
                        for i in range(nb):
                            kb2 = 8 * g + i
                            S.add("pe", lambda e, ptb=ptb, i=i, kb2=kb2, a_=a_: e.transpose(ptb[:, i * 128:(i + 1) * 128], a_[:, kb2 * 128:(kb2 + 1) * 128], self.IDB[:]),
                                  reads=[("A", sb_), "IDB"], writes=[("ps", 2 + g % 2)])
                        S.add("dve", lambda e, ptb=ptb, at=at, g=g, nb=nb: e.tensor_copy(out=at[:, 8 * g:8 * g + nb, :], in_=ptb[:, 0:nb * 128].rearrange("p (a b) -> p a b", a=nb)),
                              reads=[("ps", 2 + g % 2)], writes=[("AT", sb_, g)])
                    po = PS[4 + hh]
                    for kb2 in range(qt + 1):
                        S.add("pe", lambda e, po=po, kb2=kb2, at=at, hp=hp, qt=qt: e.matmul(po[:, 0:64], at[:, kb2, :], V[:, kb2, hp], start=(kb2 == 0), stop=(kb2 == qt)),
                              reads=[("AT", sb_, kb2 // 8), ("V", kb2)], writes=[("ps", 4 + hh)])
                    S.add("act", lambda e, po=po, hp=hp, ob=ob: e.activation(out=OS[ob][:, hp], in_=po[:, 0:64], func=AF.Copy), reads=[("ps", 4 + hh)], writes=[("OS", ob, hh)])
                ptb = PSB[3]
                S.add("pe", lambda e, ptb=ptb, ob=ob: e.transpose(ptb[:, 0:128], OS[ob], self.IDB[:]), reads=[("OS", ob, 0), ("OS", ob, 1), "IDB"], writes=[("ps", 3)])
                S.add("dve", lambda e, ptb=ptb, ob=ob: e.tensor_copy(out=OT[ob], in_=ptb[:, 0:128]), reads=[("ps", 3)], writes=[("OT", ob)])
                for hf in range(2):
                    px = PS[6 + hf]
                    S.add("pe", lambda e, px=px, ob=ob, hf=hf, wb=wb: e.matmul(px[:], OT[ob], WOp[wb][:, hf * 512:(hf + 1) * 512], start=True, stop=True),
                          reads=[("OT", ob), ("WOp", wb)], writes=[("ps", 6 + hf)])
                    S.add("dve", lambda e, px=px, qt=qt, hf=hf: e.tensor_tensor(out=X[:, qt, hf * 512:(hf + 1) * 512], in0=px[:], in1=X[:, qt, hf * 512:(hf + 1) * 512], op=ALU.add),
                          reads=[("ps", 6 + hf), ("X", qt)], writes=[("X", qt)])
        S.barrier()
        self.arena_reset()


def _host_inputs(inp, b, consts, big=True):
    f = lambda a: np.ascontiguousarray(np.asarray(a, dtype=np.float32))
    m = {}
    m["x"] = f(inp["x"][b])
    m["pT"] = f(np.transpose(np.asarray(inp["p"])[:, b], (0, 2, 1)))
    for k in ("ret_w_in", "ret_w_out", "sb_w_in", "sb_w_out", "router_w", "router_b", "w_gate_up", "w_down",
              "b_down", "ple_w", "ple_gate_w", "ple_gate_b"):
        if not big and k in ("w_gate_up", "w_down"):
            m[k] = f(np.asarray(inp[k])[0:1, 0:1])
        else:
            m[k] = f(inp[k])
    m["lnp"] = f(np.stack([inp["ln1_g"], inp["ln1_b"], inp["ln2_g"], inp["ln2_b"]], axis=1))
    bgu = np.asarray(inp["b_gate_up"], dtype=np.float32).reshape(DEPTH, NE, 8, 128, 2)
    m["bgu"] = f(np.transpose(bgu, (0, 3, 1, 2, 4)).reshape(DEPTH, 128, NE * 16))
    for k in ("identf", "identb", "ones", "ut", "iota", "cos", "sin", "rmask", "sbmask"):
        m[k] = consts[k]
    return m


def run_layers(inp, layers, n_exp=NE, stages=("mix", "moe", "ple"), cores=8):
    bld = Builder(layers, n_exp=n_exp, stages=stages)
    nc = bld.build()
    in_maps = [_host_inputs(inp, b, bld.consts, big=("moe" in stages)) for b in range(cores)]
    res = run_bass_kernel_spmd(nc, in_maps, core_ids=list(range(cores)))
    return np.stack([res.results[b]["y"] for b in range(cores)], axis=0)


def kernel(**inputs):
    out = run_layers(inputs, layers=range(DEPTH))
    return out.astype(np.float32)
```

```python
import math
from contextlib import ExitStack

import ml_dtypes
import numpy as np

import concourse.bass as bass
import concourse.mybir as mybir
from concourse.bass_utils import run_bass_kernel_spmd

F32 = mybir.dt.float32
BF16 = mybir.dt.bfloat16
ALU = mybir.AluOpType
AF = mybir.ActivationFunctionType
AX = mybir.AxisListType

S_TOK = 2048
D = 1024
NT = 16
NCH = 8
DEPTH = 4
NE = 32
DN_ALPHA = float((2 * DEPTH) ** 0.25)
LN_EPS = 1e-5
GN_EPS = 1e-6
SEG = 30000
GRP = 4


class _Op:
    __slots__ = ("eng", "fn", "deps", "dma", "sig", "sigval", "idx")


class Sched:
    ENGS = ("pe", "act", "dve", "pool", "sp")

    def __init__(self, nc):
        self.nc = nc
        self.streams = {e: [] for e in self.ENGS}
        self.last_w = {}
        self.readers = {}
        self.dma_cnt = {}
        self.dma_last = {}
        self.barrier_ops = []
        self.barrier_seen = {e: True for e in self.ENGS}

    def barrier(self):
        ops = []
        for e in self.ENGS:
            for op in reversed(self.streams[e]):
                if op.dma is None:
                    ops.append(op)
                    break
        ops.extend(self.dma_last.values())
        self.barrier_ops = ops
        self.barrier_seen = {e: False for e in self.ENGS}
        self.last_w = {}
        self.readers = {}

    def add(self, eng, fn, reads=(), writes=(), dma=None):
        op = _Op()
        op.eng, op.fn, op.dma = eng, fn, None
        op.sig = False
        op.sigval = None
        ps_r = [k for k in reads if isinstance(k, tuple) and k[0] == "ps"]
        if ps_r:
            reads = [k for k in reads if not (isinstance(k, tuple) and k[0] == "ps")]
            writes = list(writes) + ps_r
        deps = {}
        for k in reads:
            w = self.last_w.get(k)
            if w is not None:
                deps[w] = True
        for k in writes:
            w = self.last_w.get(k)
            if w is not None:
                deps[w] = True
            for r in self.readers.get(k, ()):
                if r not in deps:
                    deps[r] = False
        if not self.barrier_seen[eng]:
            self.barrier_seen[eng] = True
            for b in self.barrier_ops:
                deps[b] = True
        for k in reads:
            self.readers.setdefault(k, []).append(op)
        for k in writes:
            self.last_w[k] = op
            self.readers[k] = []
        pruned = []
        latest = {}
        for d, strong in deps.items():
            if d is op:
                continue
            if d.dma is not None:
                pruned.append(d)
                continue
            if d.eng == eng and (eng == "pe" or not strong):
                continue
            o = latest.get(d.eng)
            if o is None or o.idx < d.idx:
                latest[d.eng] = d
        pruned.extend(latest.values())
        op.deps = pruned
        op.idx = len(self.streams[eng])
        if dma is not None:
            c = self.dma_cnt.get(dma, 0) + 1
            self.dma_cnt[dma] = c
            op.dma = (dma, 16 * c)
            self.dma_last[dma] = op
        self.streams[eng].append(op)
        return op

    def emit(self):
        nc = self.nc
        for e in self.ENGS:
            for op in self.streams[e]:
                for d in op.deps:
                    if d.dma is None:
                        d.sig = True
        nsegs = {}
        for e in self.ENGS:
            c = 0
            for op in self.streams[e]:
                if op.sig and op.dma is None:
                    op.sigval = (e, c // SEG, c % SEG + 1)
                    c += 1
            nsegs[e] = (c + SEG - 1) // SEG
        with ExitStack() as es:
            sems = {}
            for e in self.ENGS:
                for s in range(nsegs[e]):
                    sems[(e, s)] = es.enter_context(nc.semaphore(f"s_{e}_{s}"))
            dsems = {}
            for g in self.dma_cnt:
                dsems[g] = es.enter_context(nc.semaphore(f"d_{g}"))
            self.n_sems = len(sems) + len(dsems)
            block = es.enter_context(nc.Block())

            def run(ename, eng):
                waited = {}
                for op in self.streams[ename]:
                    need = {}
                    for d in op.deps:
                        if d.dma is not None:
                            key, val = ("d", d.dma[0]), d.dma[1]
                        else:
                            key, val = (d.sigval[0], d.sigval[1]), d.sigval[2]
                        if need.get(key, 0) < val:
                            need[key] = val
                    for key, val in need.items():
                        if waited.get(key, 0) >= val:
                            continue
                        waited[key] = val
                        sem = dsems[key[1]] if key[0] == "d" else sems[key]
                        eng.wait_ge(sem, val)
                    ins = op.fn(eng)
                    if op.dma is not None:
                        ins.then_inc(dsems[op.dma[0]], 16)
                    elif op.sig:
                        ins.then_inc(sems[(op.sigval[0], op.sigval[1])], 1)

            block.tensor(lambda eng: run("pe", eng))
            block.scalar(lambda eng: run("act", eng))
            block.vector(lambda eng: run("dve", eng))
            block.gpsimd(lambda eng: run("pool", eng))
            block.sync(lambda eng: run("sp", eng))


def _consts():
    c = {}
    c["identf"] = np.eye(128, dtype=np.float32)
    c["identb"] = np.eye(128, dtype=np.float32).astype(ml_dtypes.bfloat16)
    c["ones"] = np.ones((1, 128), dtype=np.float32)
    ii = np.arange(128)
    c["ut"] = (ii[:, None] < ii[None, :]).astype(np.float32).astype(ml_dtypes.bfloat16)
    c["iota"] = np.ascontiguousarray(np.broadcast_to(ii[None, :], (128, 128))).astype(np.float32)
    c["onesb"] = np.ones((128, 128), dtype=np.float32).astype(ml_dtypes.bfloat16)
    half = 128
    inv_freq = (1.0 / (10000.0 ** (np.arange(half, dtype=np.float32) / np.float32(half)))).astype(np.float32)
    ang = (np.arange(S_TOK, dtype=np.float32)[None, :] * inv_freq[:, None]).astype(np.float32)
    c["cos"] = np.cos(ang).astype(np.float32)
    c["sin"] = np.sin(ang).astype(np.float32)
    H = 4
    lg = np.log(1.0 - 2.0 ** (-5.0 - np.arange(H, dtype=np.float64)))
    sl = np.arange(128)[:, None].astype(np.float64)
    tl = np.arange(256)[None, :].astype(np.float64)
    masks = np.zeros((H, 3, 128, 256), dtype=np.float32)
    for h in range(H):
        masks[h, 0] = np.exp(lg[h] * (tl - sl)) / 16.0
        for m in range(2):
            s_abs = 128 * m + sl
            dec = np.exp(lg[h] * np.abs(tl - s_abs))
            ok = (np.floor(s_abs / 64) <= np.floor(tl / 64))
            masks[h, 1 + m] = dec * ok / 16.0
    c["rmask"] = np.ascontiguousarray(masks.transpose(2, 0, 1, 3))
    c["rgam"] = np.exp(lg)
    t = np.arange(128)[:, None]
    s = np.arange(128)[None, :]
    c["sbmask"] = np.where(s < t, 0.0, -30000.0).astype(np.float32)
    return c


class Builder:
    def __init__(self, layers, n_exp=NE, stages=("mix", "moe", "ple")):
        self.layers = list(layers)
        self.n_exp = n_exp
        self.stages = stages
        self.nc = bass.Bass("TRN2", target_bir_lowering=False)
        self.consts = _consts()

    def dram_in(self, name, shape, dt=F32):
        return self.nc.dram_tensor(name, list(shape), dt, kind="ExternalInput").ap()

    def build(self):
        nc = self.nc
        L = len(self.layers)
        d = {}
        d["x"] = self.dram_in("x", [S_TOK, D])
        d["pT"] = self.dram_in("pT", [DEPTH, 256, S_TOK])
        d["ret_w_in"] = self.dram_in("ret_w_in", [2, D, 6144])
        d["ret_w_out"] = self.dram_in("ret_w_out", [2, 2048, D])
        d["sb_w_in"] = self.dram_in("sb_w_in", [2, D, 3 * D])
        d["sb_w_out"] = self.dram_in("sb_w_out", [2, D, D])
        d["lnp"] = self.dram_in("lnp", [DEPTH, 4, D])
        d["router_w"] = self.dram_in("router_w", [DEPTH, D, NE])
        d["router_b"] = self.dram_in("router_b", [DEPTH, NE])
        big = "moe" in self.stages
        d["w_gate_up"] = self.dram_in("w_gate_up", [DEPTH, NE, D, 2 * D] if big else [1, 1, D, 2 * D])
        d["bgu"] = self.dram_in("bgu", [DEPTH, 128, NE * 16])
        d["w_down"] = self.dram_in("w_down", [DEPTH, NE, D, D] if big else [1, 1, D, D])
        d["b_down"] = self.dram_in("b_down", [DEPTH, NE, D])
        d["ple_w"] = self.dram_in("ple_w", [DEPTH, 256, D])
        d["ple_gate_w"] = self.dram_in("ple_gate_w", [DEPTH, D, D])
        d["ple_gate_b"] = self.dram_in("ple_gate_b", [DEPTH, D])
        d["identf"] = self.dram_in("identf", [128, 128])
        d["identb"] = self.dram_in("identb", [128, 128], BF16)
        d["ones"] = self.dram_in("ones", [1, 128])
        d["ut"] = self.dram_in("ut", [128, 128], BF16)
        d["iota"] = self.dram_in("iota", [128, 128])
        d["onesb"] = self.dram_in("onesb", [128, 128], BF16)
        d["cos"] = self.dram_in("cos", [128, S_TOK])
        d["sin"] = self.dram_in("sin", [128, S_TOK])
        d["rmask"] = self.dram_in("rmask", [128, 4, 3, 256])
        d["sbmask"] = self.dram_in("sbmask", [128, 128])
        d["y"] = nc.dram_tensor("y", [S_TOK, D], F32, kind="ExternalOutput").ap()
        self.d = d

        AW = 27648
        with ExitStack() as es:
            sb = lambda n, s, dt: es.enter_context(nc.sbuf_tensor(n, s, dt))
            self.X = sb("X", [128, NT, D], F32)
            self.XT = sb("XT", [128, NCH, S_TOK], BF16)
            self.IDF = sb("IDF", [128, 128], F32)
            self.IDB = sb("IDB", [128, 128], BF16)
            self.ONES = sb("ONES", [1, 128], F32)
            self.GALL = sb("GALL", [128, NT, NE], F32)
            self.ARENA = sb("ARENA", [128, AW], F32)
            self.AW = AW
            self.PS = [es.enter_context(nc.psum_tensor(f"ps{i}", [128, 512], F32)) for i in range(8)]
            self.S = Sched(nc)
            self.uid = 0
            self.program()
            self.S.emit()
        return nc

    def arena_reset(self):
        self.aptr = 0

    def alloc(self, shape, dt=F32, parts=128):
        n = int(np.prod(shape))
        words = n if dt == F32 else (n + 1) // 2
        words = (words + 7) // 8 * 8
        a = self.ARENA[0:parts, self.aptr:self.aptr + words]
        self.aptr += words
        assert self.aptr <= self.AW, f"arena overflow {self.aptr} > {self.AW}"
        if dt != F32:
            a = a.bitcast(dt)[:, 0:n]
        else:
            a = a[:, 0:n]
        if len(shape) == 2:
            a = a.rearrange("p (a b) -> p a b", a=shape[0])
        elif len(shape) == 3:
            a = a.rearrange("p (a b c) -> p a b c", a=shape[0], b=shape[1])
        return a

    def key(self, base):
        self.uid += 1
        return (base, self.uid)

    def program(self):
        S, d = self.S, self.d
        X = self.X
        S.add("sp", lambda e: e.dma_start(out=self.IDF[:], in_=d["identf"]), writes=["IDF"], dma="c0")
        S.add("sp", lambda e: e.dma_start(out=self.IDB[:], in_=d["identb"]), writes=["IDB"], dma="c1")
        S.add("sp", lambda e: e.dma_start(out=self.ONES[:], in_=d["ones"]), writes=["ONES"], dma="c2")
        xin = d["x"].rearrange("(t p) f -> p t f", p=128)
        for q in range(4):
            S.add("sp", lambda e, q=q: e.dma_start(out=X[:, 4 * q:4 * q + 4, :], in_=xin[:, 4 * q:4 * q + 4, :]),
                  writes=[("X", t) for t in range(4 * q, 4 * q + 4)], dma=f"xin{q}")
        for l in self.layers:
            if "mix" in self.stages:
                self.arena_reset()
                S.barrier()
                self.make_xt(l, scale=DN_ALPHA, router=False)
                if l % 2 == 0:
                    self.retention(l)
                else:
                    self.stickbreak(l)
                self.layernorm(l, 0)
            if "moe" in self.stages:
                self.arena_reset()
                S.barrier()
                self.moe(l)
                self.layernorm(l, 1)
            if "ple" in self.stages:
                self.arena_reset()
                S.barrier()
                self.ple(l)
        yout = d["y"].rearrange("(t p) f -> p t f", p=128)
        for q in range(4):
            S.add("sp", lambda e, q=q: e.dma_start(out=yout[:, 4 * q:4 * q + 4, :], in_=X[:, 4 * q:4 * q + 4, :]),
                  reads=[("X", t) for t in range(4 * q, 4 * q + 4)], writes=[("yout", q)], dma=f"yout{q}")
        S.add("sp", lambda e: e.nop(), reads=[("yout", q) for q in range(4)])

    def make_xt(self, l, scale=None, router=False):
        S, d, X, XT, PS = self.S, self.d, self.X, self.XT, self.PS
        if router:
            RW = self.alloc([NCH, NE])
            RB = self.alloc([NE], parts=1)
            XT32 = [self.alloc([NCH, 128]) for _ in range(2)]
            LG = [self.alloc([NE]) for _ in range(2)]
            EXm = [self.alloc([NE]) for _ in range(2)]
            MSK = [self.alloc([NE]) for _ in range(2)]
            SM = [self.alloc([16]) for _ in range(2)]
            UT = self.alloc([128], BF16)
            kUT = self.key("UT")
            S.add("sp", lambda e: e.dma_start(out=UT, in_=d["ut"]), writes=[kUT], dma="ut")
            ONB = self.alloc([128], BF16)
            kONB = self.key("ONB")
            S.add("sp", lambda e: e.dma_start(out=ONB, in_=d["onesb"]), writes=[kONB], dma="onb")
            XB = self.XB
            kRW, kRB = self.key("RW"), self.key("RB")
            S.add("sp", lambda e: e.dma_start(out=RW, in_=d["router_w"][l].rearrange("(c p) n -> p c n", p=128)),
                  writes=[kRW], dma="rw")
            S.add("sp", lambda e: e.dma_start(out=RB, in_=d["router_b"][l:l + 1, :]), writes=[kRB], dma="rb")
        for t in range(NT):
            b = t % 2
            for h in range(2):
                pb = PS[2 * b + h]
                for j in range(4):
                    c = 4 * h + j
                    S.add("pe", lambda e, pb=pb, j=j, c=c, t=t: e.transpose(pb[:, j * 128:(j + 1) * 128], X[:, t, c * 128:(c + 1) * 128], self.IDF[:]),
                          reads=[("X", t), "IDF"], writes=[("ps", 2 * b + h)])
                if not router:
                    S.add("act", lambda e, pb=pb, h=h, t=t: e.activation(out=XT[:, 4 * h:4 * h + 4, t * 128:(t + 1) * 128],
                                                                          in_=pb[:].rearrange("p (a b) -> p a b", a=4), func=AF.Copy),
                          reads=[("ps", 2 * b + h)], writes=[("XT", t)])
                if router:
                    S.add("dve", lambda e, pb=pb, h=h, b=b: e.tensor_copy(out=XT32[b][:, 4 * h:4 * h + 4, :],
                                                                           in_=pb[:].rearrange("p (a b) -> p a b", a=4)),
                          reads=[("ps", 2 * b + h)], writes=[("XT32", b, h)])
            if router:
                S.add("act", lambda e, t=t: e.activation(out=XB[:, t, :], in_=X[:, t, :], func=AF.Copy), reads=[("X", t)], writes=[("XB", t)])
            if scale is not None:
                S.add("pool", lambda e, t=t: e.tensor_scalar(out=X[:, t, :], in0=X[:, t, :], scalar1=float(scale), scalar2=None, op0=ALU.mult),
                      reads=[("X", t)], writes=[("X", t)])
            if router:
                pl = PS[4 + b]
                for c in range(NCH):
                    S.add("pe", lambda e, pl=pl, c=c, b=b: e.matmul(pl[:, 0:NE], XT32[b][:, c, :], RW[:, c, :], start=(c == 0), stop=False),
                          reads=[("XT32", b, c // 4), kRW], writes=[("ps", 4 + b)])
                S.add("pe", lambda e, pl=pl: e.matmul(pl[:, 0:NE], self.ONES[0:1, :], RB[0:1, :], start=False, stop=True),
                      reads=["ONES", kRB], writes=[("ps", 4 + b)])
                lg, ex, mk, sm = LG[b], EXm[b], MSK[b], SM[b]
                kl = ("rt", b)
                S.add("dve", lambda e, pl=pl, lg=lg: e.tensor_copy(out=lg, in_=pl[:, 0:NE]), reads=[("ps", 4 + b)], writes=[(kl, "lg")])
                S.add("dve", lambda e, lg=lg, sm=sm: e.max(out=sm[:, 0:8], in_=lg), reads=[(kl, "lg")], writes=[(kl, "top")])
                S.add("dve", lambda e, lg=lg, sm=sm, mk=mk: e.tensor_scalar(out=mk, in0=lg, scalar1=sm[:, 3:4], scalar2=None, op0=ALU.is_ge),
                      reads=[(kl, "lg"), (kl, "top")], writes=[(kl, "mk")])
                S.add("dve", lambda e, sm=sm: e.tensor_scalar(out=sm[:, 8:9], in0=sm[:, 0:1], scalar1=-1.0, scalar2=None, op0=ALU.mult),
                      reads=[(kl, "top")], writes=[(kl, "nm")])
                S.add("dve", lambda e, mk=mk, t=t: e.tensor_copy(out=self.MB[:, t, :], in_=mk), reads=[(kl, "mk")], writes=[("MB", t)])
                gi = t % GRP
                S.add("pe", lambda e, pl=pl, t=t, gi=gi: e.matmul(pl[:, 64:64 + NE], UT, self.MB[:, t, :], start=True, stop=(gi == 0)),
                      reads=[("MB", t), kUT], writes=[("ps", 4 + b)])
                for tp in range(t - gi, t):
                    S.add("pe", lambda e, pl=pl, tp=tp, t=t: e.matmul(pl[:, 64:64 + NE], ONB, self.MB[:, tp, :], start=False, stop=(tp == t - 1)),
                          reads=[("MB", tp), kONB], writes=[("ps", 4 + b)])
                S.add("dve", lambda e, pl=pl, mk=mk, t=t: e.scalar_tensor_tensor(out=self.VAL[:, t, :], in0=pl[:, 64:64 + NE], scalar=127.5, in1=mk, op0=ALU.is_lt, op1=ALU.mult),
                      reads=[("ps", 4 + b), (kl, "mk")], writes=[("VAL", t)])
                S.add("dve", lambda e, pl=pl, t=t: e.tensor_copy(out=self.RK[:, t, :], in_=pl[:, 64:64 + NE]), reads=[("ps", 4 + b)], writes=[("RK", t)])
                S.add("act", lambda e, lg=lg, ex=ex, sm=sm: e.activation(out=ex, in_=lg, func=AF.Exp, bias=sm[:, 8:9], scale=1.0),
                      reads=[(kl, "lg"), (kl, "nm")], writes=[(kl, "ex")])
                S.add("dve", lambda e, ex=ex, mk=mk: e.tensor_tensor(out=ex, in0=ex, in1=mk, op=ALU.mult),
                      reads=[(kl, "ex"), (kl, "mk")], writes=[(kl, "ex")])
                S.add("dve", lambda e, ex=ex, sm=sm: e.reduce_sum(out=sm[:, 9:10], in_=ex, axis=AX.X),
                      reads=[(kl, "ex")], writes=[(kl, "ss")])
                S.add("dve", lambda e, sm=sm: e.reciprocal(out=sm[:, 10:11], in_=sm[:, 9:10]), reads=[(kl, "ss")], writes=[(kl, "rs")])
                S.add("dve", lambda e, ex=ex, sm=sm, t=t: e.tensor_scalar(out=self.GALL[:, t, :], in0=ex, scalar1=sm[:, 10:11], scalar2=None, op0=ALU.mult),
                      reads=[(kl, "ex"), (kl, "rs")], writes=[("G", t)])
                pg = PS[6 + b]
                S.add("pe", lambda e, pg=pg, t=t: e.transpose(pg[0:NE, 0:128], self.GALL[:, t, :], self.IDF[:]),
                      reads=[("G", t), "IDF"], writes=[("ps", 6 + b)])
                S.add("act", lambda e, pg=pg, t=t: e.activation(out=self.GT[0:NE, t * 128:(t + 1) * 128], in_=pg[0:NE, 0:128], func=AF.Copy),
                      reads=[("ps", 6 + b)], writes=[("GT", t)])

    def ln_alloc(self):
        return (self.alloc([D]), self.alloc([D]), [self.alloc([16]) for _ in range(2)])

    def layernorm(self, l, which, bufs=None):
        S, d, X = self.S, self.d, self.X
        G, B, ST = bufs if bufs is not None else self.ln_alloc()
        kG, kB = self.key("lng"), self.key("lnb")
        S.add("sp", lambda e: e.dma_start(out=G, in_=d["lnp"][l, 2 * which:2 * which + 1, :].to_broadcast([128, D])), writes=[kG], dma="lng")
        S.add("sp", lambda e: e.dma_start(out=B, in_=d["lnp"][l, 2 * which + 1:2 * which + 2, :].to_broadcast([128, D])), writes=[kB], dma="lnb")
        for t in range(NT):
            st = ST[t % 2]
            ks = ("lnst", t % 2)
            xt = X[:, t, :]
            S.add("dve", lambda e, st=st, xt=xt: e.bn_stats(out=st[:, 0:6], in_=xt[:, 0:512]), reads=[("X", t)], writes=[(ks, 0)])
            S.add("dve", lambda e, st=st, xt=xt: e.bn_stats(out=st[:, 6:12], in_=xt[:, 512:1024]), reads=[("X", t)], writes=[(ks, 1)])
            S.add("dve", lambda e, st=st: e.bn_aggr(out=st[:, 12:14], in_=st[:, 0:12]),
                  reads=[(ks, 0), (ks, 1)], writes=[(ks, 2)])
            S.add("dve", lambda e, st=st: e.tensor_scalar(out=st[:, 14:15], in0=st[:, 13:14], scalar1=float(LN_EPS), scalar2=None, op0=ALU.add),
                  reads=[(ks, 2)], writes=[(ks, 3)])
            S.add("act", lambda e, st=st: e.activation(out=st[:, 14:15], in_=st[:, 14:15], func=AF.Sqrt), reads=[(ks, 3)], writes=[(ks, 3)])
            S.add("dve", lambda e, st=st: e.reciprocal(out=st[:, 15:16], in_=st[:, 14:15]), reads=[(ks, 3)], writes=[(ks, 4)])
            S.add("dve", lambda e, st=st, xt=xt: e.tensor_scalar(out=xt, in0=xt, scalar1=st[:, 12:13], scalar2=st[:, 15:16], op0=ALU.subtract, op1=ALU.mult),
                  reads=[("X", t), (ks, 2), (ks, 4)], writes=[("X", t)])
            S.add("pool", lambda e, xt=xt: e.tensor_tensor(out=xt, in0=xt, in1=G, op=ALU.mult), reads=[("X", t), kG], writes=[("X", t)])
            S.add("pool", lambda e, xt=xt: e.tensor_tensor(out=xt, in0=xt, in1=B, op=ALU.add), reads=[("X", t), kB], writes=[("X", t)])

    def moe_bias(self, l, GT, BD, BGU):
        S, d, X, PS = self.S, self.d, self.X, self.PS
        kBD, kBGU = self.key("BD"), self.key("BGU")
        self.moe_keys = (kBD, kBGU)
        S.add("sp", lambda e: e.dma_start(out=BD, in_=d["b_down"][l]), writes=[kBD], dma="bd")
        S.add("sp", lambda e: e.dma_start(out=BGU, in_=d["bgu"][l]), writes=[kBGU], dma="bgu")
        for t in range(NT):
            for h in range(2):
                pb = PS[6 + h]
                S.add("pe", lambda e, pb=pb, t=t, h=h: e.matmul(pb[:], GT[0:NE, t * 128:(t + 1) * 128], BD[0:NE, h * 512:(h + 1) * 512], start=True, stop=True),
                      reads=[("GT", t), kBD], writes=[("ps", 6 + h)])
                S.add("dve", lambda e, pb=pb, t=t, h=h: e.tensor_tensor(out=X[:, t, h * 512:(h + 1) * 512], in0=pb[:], in1=X[:, t, h * 512:(h + 1) * 512], op=ALU.add),
                      reads=[("ps", 6 + h), ("X", t)], writes=[("X", t)])

    def moe(self, l):
        S, d, X, PS = self.S, self.d, self.X, self.PS
        PSB = [p[:].bitcast(BF16) for p in PS]
        NQ = GRP
        NST = NT // GRP
        NSL = NST * 128
        NSC = NSL // 512
        self.XB = self.XT[:].rearrange("p c s -> p (c s)").rearrange("p (t f) -> p t f", t=NT)
        XB = self.XB
        BGU = self.alloc([NE * 16])
        self.RK = self.alloc([NT, NE])
        self.VAL = self.alloc([NT, NE])
        self.MB = self.alloc([NT, NE], BF16)
        IOTA = self.alloc([128])
        kIO = self.key("iota")
        S.add("sp", lambda e: e.dma_start(out=IOTA, in_=d["iota"]), writes=[kIO], dma="iota")
        mark = self.aptr
        self.GT = self.alloc([S_TOK], parts=32)
        BD = self.alloc([D], parts=32)
        self.make_xt(l, scale=DN_ALPHA, router=True)
        self.moe_bias(l, self.GT, BD, BGU)
        S.barrier()
        self.aptr = mark
        kBD, kBGU = self.moe_keys
        RK, VAL = self.RK, self.VAL
        XG = self.alloc([NCH, NSL], BF16)
        ACTT = self.alloc([NCH, NSL], BF16)
        YS = self.alloc([NST, D], BF16)
        PA = self.alloc([NT, 128], BF16)
        PTA = self.alloc([NT, 128], BF16)
        NSLOT = 8
        WGU = [self.alloc([NCH, 256], BF16) for _ in range(NSLOT)]
        WD = self.alloc([NCH, D], BF16)
        NTMP = 3
        TG = [self.alloc([512]) for _ in range(NTMP)]
        TU = [self.alloc([512]) for _ in range(NTMP)]
        TS = [self.alloc([512]) for _ in range(NTMP)]
        wgu_src = d["w_gate_up"]
        wd_src = d["w_down"]
        n_chunks = self.n_exp * 8

        def dma_wgu(g):
            if g >= n_chunks:
                return
            slot = g % NSLOT
            wsrc = wgu_src[l, g // 8].rearrange("(c p) f -> p c f", p=128)
            j = g % 8
            S.add("pool", lambda e, slot=slot, j=j, wsrc=wsrc: e.dma_start(out=WGU[slot], in_=wsrc[:, :, 256 * j:256 * (j + 1)]),
                  writes=[("WGU", slot)], dma=f"wgu{slot}")

        def dma_wd(ei):
            src = wd_src[l, ei].rearrange("(c p) f -> p c f", p=128)
            for q in range(2):
                S.add("pool", lambda e, q=q, src=src: e.dma_start(out=WD[:, 4 * q:4 * q + 4, :], in_=src[:, 4 * q:4 * q + 4, :]),
                      writes=[("WD", q)], dma=f"wd{q}")

        def build_sel(ei):
            for t in range(NT):
                S.add("dve", lambda e, t=t, ei=ei: e.tensor_scalar(out=PA[:, t, :], in0=IOTA, scalar1=RK[:, t, ei:ei + 1], scalar2=VAL[:, t, ei:ei + 1],
                                                                    op0=ALU.is_equal, op1=ALU.mult),
                      reads=[kIO], writes=[("PA", t)])

        for g in range(NSLOT - 1):
            dma_wgu(g)
        build_sel(0)
        unit = 0
        gcnt = 0
        ycnt = 0
        for ei in range(self.n_exp):
            for g8 in range(NT // 8):
                ptb = PSB[6 + g8]
                for i in range(8):
                    t = 8 * g8 + i
                    S.add("pe", lambda e, ptb=ptb, i=i, t=t: e.transpose(ptb[:, i * 128:(i + 1) * 128], PA[:, t, :], self.IDB[:]),
                          reads=[("PA", t), "IDB"], writes=[("ps", 6 + g8)])
                S.add("act", lambda e, ptb=ptb, g8=g8: e.activation(out=PTA[:, 8 * g8:8 * g8 + 8, :], in_=ptb[:].rearrange("p (a b) -> p a b", a=8), func=AF.Copy),
                      reads=[("ps", 6 + g8)], writes=[("PTA", g8)])
            for dch in range(NCH):
                for sc in range(NSC):
                    bi = gcnt % 6
                    gcnt += 1
                    pb = PS[bi]
                    for gl in range(4):
                        g_ = sc * 4 + gl
                        for i in range(GRP):
                            t = g_ * GRP + i
                            S.add("pe", lambda e, pb=pb, gl=gl, i=i, t=t, dch=dch: e.matmul(pb[:, gl * 128:(gl + 1) * 128], XB[:, t, dch * 128:(dch + 1) * 128], PA[:, t, :],
                                                                                         start=(i == 0), stop=(i == GRP - 1)),
                                  reads=[("PA", t)], writes=[("ps", bi)])
                    eng = "act" if (gcnt % 2 == 0) else "dve"
                    if eng == "act":
                        S.add("act", lambda e, pb=pb, dch=dch, sc=sc: e.activation(out=XG[:, dch, sc * 512:(sc + 1) * 512], in_=pb[:], func=AF.Copy), reads=[("ps", bi)], writes=[("XG", dch, sc)])
                    else:
                        S.add("dve", lambda e, pb=pb, dch=dch, sc=sc: e.tensor_copy(out=XG[:, dch, sc * 512:(sc + 1) * 512], in_=pb[:]), reads=[("ps", bi)], writes=[("XG", dch, sc)])
            for j in range(8):
                dma_wgu(ei * 8 + j + NSLOT - 1)
                if j == 1:
                    dma_wd(ei)
                slot = (ei * 8 + j) % NSLOT
                W = WGU[slot]
                for c in range(NSC):
                    pr = unit % 3
                    pg_, pu_ = PS[2 * pr], PS[2 * pr + 1]
                    tb = unit % NTMP
                    unit += 1
                    tok = slice(c * 512, (c + 1) * 512)
                    for k in range(NCH):
                        S.add("pe", lambda e, pg_=pg_, W=W, k=k, tok=tok: e.matmul(pg_[:], W[:, k, 0:256:2], XG[:, k, tok], start=(k == 0), stop=(k == 7)),
                              reads=[("WGU", slot), ("XG", k, c)], writes=[("ps", 2 * pr)])
                    for k in range(NCH):
                        S.add("pe", lambda e, pu_=pu_, W=W, k=k, tok=tok: e.matmul(pu_[:], W[:, k, 1:256:2], XG[:, k, tok], start=(k == 0), stop=(k == 7)),
                              reads=[("WGU", slot), ("XG", k, c)], writes=[("ps", 2 * pr + 1)])
                    bg = BGU[:, ei * 16 + 2 * j:ei * 16 + 2 * j + 1]
                    bu = BGU[:, ei * 16 + 2 * j + 1:ei * 16 + 2 * j + 2]
                    tg, tu, ts = TG[tb], TU[tb], TS[tb]
                    S.add("dve", lambda e, tg=tg, pg_=pg_, bg=bg: e.tensor_scalar(out=tg, in0=pg_[:], scalar1=bg, scalar2=7.0, op0=ALU.add, op1=ALU.min),
                          reads=[("ps", 2 * pr), kBGU], writes=[("TG", tb)])
                    S.add("act", lambda e, tu=tu, pu_=pu_, bu=bu: e.activation(out=tu, in_=pu_[:], func=AF.Identity, bias=bu, scale=1.0),
                          reads=[("ps", 2 * pr + 1), kBGU], writes=[("TU", tb)])
                    S.add("act", lambda e, ts=ts, tg=tg: e.activation(out=ts, in_=tg, func=AF.Sigmoid, scale=1.702),
                          reads=[("TG", tb)], writes=[("TS", tb)])
                    S.add("pool", lambda e, tu=tu: e.tensor_scalar(out=tu, in0=tu, scalar1=7.0, scalar2=-7.0, op0=ALU.min, op1=ALU.max),
                          reads=[("TU", tb)], writes=[("TU", tb)])
                    S.add("pool", lambda e, tg=tg, ts=ts: e.tensor_tensor(out=tg, in0=tg, in1=ts, op=ALU.mult),
                          reads=[("TG", tb), ("TS", tb)], writes=[("TG", tb)])
                    S.add("dve", lambda e, tu=tu, tg=tg, j=j, tok=tok: e.scalar_tensor_tensor(out=ACTT[:, j, tok], in0=tu, scalar=1.0, in1=tg, op0=ALU.add, op1=ALU.mult),
                          reads=[("TU", tb), ("TG", tb)], writes=[("ACTT", j, c)])
            for st in range(NST):
                for h in range(2):
                    pi = 6 + (ycnt % 2)
                    ycnt += 1
                    py = PS[pi]
                    for k in range(NCH):
                        S.add("pe", lambda e, py=py, k=k, st=st, h=h: e.matmul(py[:], ACTT[:, k, st * 128:(st + 1) * 128], WD[:, k, h * 512:(h + 1) * 512], start=(k == 0), stop=(k == 7)),
                              reads=[("ACTT", k, st // 4), ("WD", k // 4)], writes=[("ps", pi)])
                    S.add("act", lambda e, py=py, st=st, h=h: e.activation(out=YS[:, st, h * 512:(h + 1) * 512], in_=py[:], func=AF.Copy), reads=[("ps", pi)], writes=[("YS", st, h)])
            if ei + 1 < self.n_exp:
                build_sel(ei + 1)
            for t in range(NT):
                for h in range(2):
                    pi = 6 + (ycnt % 2)
                    ycnt += 1
                    py = PS[pi]
                    S.add("pe", lambda e, py=py, t=t, h=h: e.matmul(py[:], PTA[:, t, :], YS[:, t // NQ, h * 512:(h + 1) * 512], start=True, stop=True),
                          reads=[("PTA", t // 8), ("YS", t // NQ, h)], writes=[("ps", pi)])
                    S.add("dve", lambda e, py=py, t=t, h=h, ei=ei: e.scalar_tensor_tensor(out=X[:, t, h * 512:(h + 1) * 512], in0=py[:], scalar=self.GALL[:, t, ei:ei + 1],
                                                                                          in1=X[:, t, h * 512:(h + 1) * 512], op0=ALU.mult, op1=ALU.add),
                          reads=[("ps", pi), ("X", t)], writes=[("X", t)])
        S.barrier()
        self.arena_reset()

    def ple(self, l):
        S, d, X, XT, PS = self.S, self.d, self.X, self.XT, self.PS
        self.make_xt(l, scale=None, router=False)
        WG = self.alloc([NCH, D], BF16)
        WP = self.alloc([2, D], BF16)
        PT_ = self.alloc([2, S_TOK], BF16)
        BG = self.alloc([D], parts=1)
        SG = [self.alloc([D]) for _ in range(2)]
        kWG, kWP, kPT, kBG = self.key("WG"), self.key("WP"), self.key("PT"), self.key("BG")
        S.add("pool", lambda e: e.dma_start(out=WG, in_=d["ple_gate_w"][l].rearrange("(c p) f -> p c f", p=128)), writes=[kWG], dma="pwg")
        S.add("pool", lambda e: e.dma_start(out=WP, in_=d["ple_w"][l].rearrange("(c p) f -> p c f", p=128)), writes=[kWP], dma="pwp")
        S.add("pool", lambda e: e.dma_start(out=PT_, in_=d["pT"][l].rearrange("(c p) s -> p c s", p=128)), writes=[kPT], dma="ppt")
        S.add("sp", lambda e: e.dma_start(out=BG, in_=d["ple_gate_b"][l:l + 1, :]), writes=[kBG], dma="pbg")
        for t in range(NT):
            sg = SG[t % 2]
            for h in range(2):
                pg = PS[4 + h]
                pp = PS[6 + h]
                cols = slice(h * 512, (h + 1) * 512)
                for k in range(NCH):
                    S.add("pe", lambda e, pg=pg, k=k, t=t, cols=cols: e.matmul(pg[:], XT[:, k, t * 128:(t + 1) * 128], WG[:, k, cols], start=(k == 0), stop=False),
                          reads=[("XT", t), kWG], writes=[("ps", 4 + h)])
                S.add("pe", lambda e, pg=pg, cols=cols: e.matmul(pg[:], self.ONES[0:1, :], BG[0:1, cols], start=False, stop=True),
                      reads=["ONES", kBG], writes=[("ps", 4 + h)])
                for k in range(2):
                    S.add("pe", lambda e, pp=pp, k=k, t=t, cols=cols: e.matmul(pp[:], PT_[:, k, t * 128:(t + 1) * 128], WP[:, k, cols], start=(k == 0), stop=(k == 1)),
                          reads=[kPT, kWP], writes=[("ps", 6 + h)])
                S.add("act", lambda e, sg=sg, pg=pg, cols=cols: e.activation(out=sg[:, cols], in_=pg[:], func=AF.Sigmoid),
                      reads=[("ps", 4 + h)], writes=[("SG", t % 2, h)])
                S.add("dve", lambda e, sg=sg, pp=pp, cols=cols: e.tensor_tensor(out=sg[:, cols], in0=pp[:], in1=sg[:, cols], op=ALU.mult),
                      reads=[("ps", 6 + h), ("SG", t % 2, h)], writes=[("SG", t % 2, h)])
                S.add("pool", lambda e, sg=sg, t=t, cols=cols: e.tensor_tensor(out=X[:, t, cols], in0=X[:, t, cols], in1=sg[:, cols], op=ALU.add),
                      reads=[("SG", t % 2, h), ("X", t)], writes=[("X", t)])

    def retention(self, l):
        S, d, X, XT, PS = self.S, self.d, self.X, self.XT, self.PS
        jl = l // 2
        w_in = d["ret_w_in"][jl].rearrange("(c p) f -> p c f", p=128)
        w_out = d["ret_w_out"][jl]
        gam = self.consts["rgam"]
        COS = self.alloc([S_TOK])
        SIN = self.alloc([S_TOK])
        QT = self.alloc([2, S_TOK], BF16)
        KT = self.alloc([2, S_TOK], BF16)
        V = self.alloc([NT, 512], BF16)
        WC = self.alloc([NCH, 512], BF16)
        WO = self.alloc([4, D], BF16)
        MASK = self.alloc([4, 3, 256])
        kC, kSn, kM = self.key("cos"), self.key("sin"), self.key("rmask")
        S.add("sp", lambda e: e.dma_start(out=COS, in_=d["cos"]), writes=[kC], dma="cos")
        S.add("sp", lambda e: e.dma_start(out=SIN, in_=d["sin"]), writes=[kSn], dma="sin")
        S.add("sp", lambda e: e.dma_start(out=MASK, in_=d["rmask"]), writes=[kM], dma="rmask")
        mark = self.aptr
        PSB = [p[:].bitcast(BF16) for p in PS]
        for h in range(4):
            if h > 0:
                S.barrier()
            self.aptr = mark
            WA = self.alloc([NCH, 512], BF16)
            WB = self.alloc([NCH, 512], BF16)
            T1 = [self.alloc([512]) for _ in range(2)]
            T2 = [self.alloc([512]) for _ in range(2)]
            kWA, kWB, kWC, kWO = self.key("WA"), self.key("WB"), self.key("WC"), self.key("WO")
            S.add("pool", lambda e, h=h, WA=WA: e.dma_start(out=WA[:, :, 0:256], in_=w_in[:, :, h * 256:(h + 1) * 256]), writes=[(kWA, 0)], dma="rwa0")
            S.add("pool", lambda e, h=h, WA=WA: e.dma_start(out=WA[:, :, 256:512], in_=w_in[:, :, 1024 + h * 256:1024 + (h + 1) * 256]), writes=[(kWA, 1)], dma="rwa1")
            S.add("pool", lambda e, h=h, WB=WB: e.dma_start(out=WB, in_=w_in[:, :, 2048 + h * 512:2048 + (h + 1) * 512]), writes=[kWB], dma="rwb")
            S.add("pool", lambda e, h=h: e.dma_start(out=WC, in_=w_in[:, :, 4096 + h * 512:4096 + (h + 1) * 512]), writes=[kWC], dma="rwc")
            S.add("pool", lambda e, h=h: e.dma_start(out=WO, in_=w_out[h * 512:(h + 1) * 512, :].rearrange("(c p) f -> p c f", p=128)), writes=[kWO], dma="rwo")
            u = 0
            for qk in range(2):
                DST = QT if qk == 0 else KT
                for c in range(4):
                    tok = slice(c * 512, (c + 1) * 512)
                    p1, p2 = PS[2 * (u % 2)], PS[2 * (u % 2) + 1]
                    tb = u % 2
                    u += 1
                    for a, pp in ((0, p1), (1, p2)):
                        for kk in range(NCH):
                            S.add("pe", lambda e, pp=pp, kk=kk, a=a, qk=qk, tok=tok, WA=WA: e.matmul(pp[:], WA[:, kk, qk * 256 + a * 128:qk * 256 + (a + 1) * 128], XT[:, kk, tok],
                                                                                              start=(kk == 0), stop=(kk == 7)),
                                  reads=[(kWA, qk)] + [("XT", tt) for tt in range(4 * c, 4 * c + 4)], writes=[("ps", 2 * tb + a)])
                    t1, t2 = T1[tb], T2[tb]
                    k1, k2 = ("T1", tb), ("T2", tb)
                    S.add("dve", lambda e, t1=t1, p1=p1, tok=tok: e.tensor_tensor(out=t1, in0=p1[:], in1=COS[:, tok], op=ALU.mult), reads=[("ps", 2 * tb), kC], writes=[k1])
                    S.add("dve", lambda e, t2=t2, p2=p2, tok=tok: e.tensor_tensor(out=t2, in0=p2[:], in1=SIN[:, tok], op=ALU.mult), reads=[("ps", 2 * tb + 1), kSn], writes=[k2])
                    S.add("pool", lambda e, t1=t1, t2=t2, DST=DST, tok=tok: e.tensor_tensor(out=DST[:, 0, tok], in0=t1, in1=t2, op=ALU.subtract), reads=[k1, k2], writes=[("QK", qk, c, 0)])
                    S.add("dve", lambda e, t1=t1, p1=p1, tok=tok: e.tensor_tensor(out=t1, in0=p1[:], in1=SIN[:, tok], op=ALU.mult), reads=[("ps", 2 * tb), kSn], writes=[k1])
                    S.add("dve", lambda e, t2=t2, p2=p2, tok=tok: e.tensor_tensor(out=t2, in0=p2[:], in1=COS[:, tok], op=ALU.mult), reads=[("ps", 2 * tb + 1), kC], writes=[k2])
                    S.add("pool", lambda e, t1=t1, t2=t2, DST=DST, tok=tok: e.tensor_tensor(out=DST[:, 1, tok], in0=t1, in1=t2, op=ALU.add), reads=[k1, k2], writes=[("QK", qk, c, 1)])
            for t in range(NT):
                pv = PS[4 + t % 2]
                for kk in range(NCH):
                    S.add("pe", lambda e, pv=pv, kk=kk, t=t, WB=WB: e.matmul(pv[:], XT[:, kk, t * 128:(t + 1) * 128], WB[:, kk, :], start=(kk == 0), stop=(kk == 7)),
                          reads=[kWB, ("XT", t)], writes=[("ps", 4 + t % 2)])
                S.add("act", lambda e, pv=pv, t=t: e.activation(out=V[:, t, :], in_=pv[:], func=AF.Copy), reads=[("ps", 4 + t % 2)], writes=[("V", t)])
            S.barrier()
            self.aptr = mark
            PT = self.alloc([NT, 256], BF16)
            RB_ = [self.alloc([512]) for _ in range(2)]
            SG = [self.alloc([512]) for _ in range(2)]
            Y = [self.alloc([512], BF16) for _ in range(2)]
            YT = [self.alloc([4, 128], BF16) for _ in range(2)]
            ST = [self.alloc([16]) for _ in range(2)]
            sc = 0
            for c in range(8):
                q0 = 256 * c
                nk = 2 * c + 2
                for ks in range(nk):
                    pi = sc % 3
                    sc += 1
                    pss = PS[pi]
                    for a in range(2):
                        S.add("pe", lambda e, pss=pss, a=a, ks=ks, q0=q0: e.matmul(pss[:, 0:256], KT[:, a, ks * 128:(ks + 1) * 128], QT[:, a, q0:q0 + 256], start=(a == 0), stop=(a == 1)),
                              reads=[], writes=[("ps", pi)])
                    if ks >= 2 * c:
                        S.add("dve", lambda e, pss=pss, ks=ks, c=c, h=h: e.tensor_tensor(out=PT[:, ks, :], in0=pss[:, 0:256], in1=MASK[:, h, 1 + ks - 2 * c, :], op=ALU.mult),
                              reads=[("ps", pi), kM], writes=[("PT", ks)])
                    else:
                        off = q0 - 128 * ks
                        gv = float(gam[h] ** off)
                        S.add("dve", lambda e, pss=pss, ks=ks, gv=gv, h=h: e.scalar_tensor_tensor(out=PT[:, ks, :], in0=pss[:, 0:256], scalar=gv, in1=MASK[:, h, 0, :], op0=ALU.mult, op1=ALU.mult),
                              reads=[("ps", pi), kM], writes=[("PT", ks)])
                for qi in range(2):
                    qt = 2 * c + qi
                    b = qt % 2
                    po, pg, ptr_, px0, px1 = PS[3], PS[4], PSB[5], PS[6], PS[7]
                    for ks in range(qt + 1):
                        S.add("pe", lambda e, po=po, ks=ks, qi=qi, qt=qt: e.matmul(po[:], PT[:, ks, qi * 128:(qi + 1) * 128], V[:, ks, :], start=(ks == 0), stop=(ks == qt)),
                              reads=[("PT", ks), ("V", ks)], writes=[("ps", 3)])
                    for kk in range(NCH):
                        S.add("pe", lambda e, pg=pg, kk=kk, qt=qt: e.matmul(pg[:], XT[:, kk, qt * 128:(qt + 1) * 128], WC[:, kk, :], start=(kk == 0), stop=(kk == 7)),
                              reads=[kWC, ("XT", qt)], writes=[("ps", 4)])
                    st, rb, sg, y, yt = ST[b], RB_[b], SG[b], Y[b], YT[b]
                    ks_ = ("gn", b)
                    S.add("dve", lambda e, st=st, po=po: e.bn_stats(out=st[:, 0:6], in_=po[:]), reads=[("ps", 3)], writes=[(ks_, 0)])
                    S.add("dve", lambda e, st=st: e.bn_aggr(out=st[:, 6:8], in_=st[:, 0:6]), reads=[(ks_, 0)], writes=[(ks_, 1)])
                    S.add("dve", lambda e, st=st: e.tensor_scalar(out=st[:, 8:9], in0=st[:, 7:8], scalar1=float(GN_EPS), scalar2=None, op0=ALU.add), reads=[(ks_, 1)], writes=[(ks_, 2)])
                    S.add("act", lambda e, st=st: e.activation(out=st[:, 8:9], in_=st[:, 8:9], func=AF.Sqrt), reads=[(ks_, 2)], writes=[(ks_, 2)])
                    S.add("dve", lambda e, st=st: e.reciprocal(out=st[:, 9:10], in_=st[:, 8:9]), reads=[(ks_, 2)], writes=[(ks_, 3)])
                    S.add("dve", lambda e, st=st, po=po, rb=rb: e.tensor_scalar(out=rb, in0=po[:], scalar1=st[:, 6:7], scalar2=st[:, 9:10], op0=ALU.subtract, op1=ALU.mult),
                          reads=[("ps", 3), (ks_, 1), (ks_, 3)], writes=[("RB", b)])
                    S.add("act", lambda e, sg=sg, pg=pg: e.activation(out=sg, in_=pg[:], func=AF.Silu), reads=[("ps", 4)], writes=[("SGr", b)])
                    S.add("pool", lambda e, y=y, rb=rb, sg=sg: e.tensor_tensor(out=y, in0=rb, in1=sg, op=ALU.mult), reads=[("RB", b), ("SGr", b)], writes=[("Y", b)])
                    for a in range(4):
                        S.add("pe", lambda e, ptr_=ptr_, a=a, y=y: e.transpose(ptr_[:, a * 128:(a + 1) * 128], y[:, a * 128:(a + 1) * 128], self.IDB[:]),
                              reads=[("Y", b), "IDB"], writes=[("ps", 5)])
                    S.add("act", lambda e, ptr_=ptr_, yt=yt: e.activation(out=yt, in_=ptr_[:, 0:512].rearrange("p (a b) -> p a b", a=4), func=AF.Copy), reads=[("ps", 5)], writes=[("YT", b)])
                    for hh, px in ((0, px0), (1, px1)):
                        for a in range(4):
                            S.add("pe", lambda e, px=px, a=a, yt=yt, hh=hh: e.matmul(px[:], yt[:, a, :], WO[:, a, hh * 512:(hh + 1) * 512], start=(a == 0), stop=(a == 3)),
                                  reads=[("YT", b), kWO], writes=[("ps", 6 + hh)])
                        S.add("dve", lambda e, px=px, qt=qt, hh=hh: e.tensor_tensor(out=X[:, qt, hh * 512:(hh + 1) * 512], in0=px[:], in1=X[:, qt, hh * 512:(hh + 1) * 512], op=ALU.add),
                              reads=[("ps", 6 + hh), ("X", qt)], writes=[("X", qt)])
        S.barrier()
        self.arena_reset()

    def stickbreak(self, l):
        S, d, X, XT, PS = self.S, self.d, self.X, self.XT, self.PS
        jl = l // 2
        w_in = d["sb_w_in"][jl].rearrange("(c p) f -> p c f", p=128)
        w_out = d["sb_w_out"][jl]
        PSB = [p[:].bitcast(BF16) for p in PS]
        NSET = 3
        SBMf = self.alloc([128])
        SBM = self.alloc([128], BF16)
        kSBM = self.key("sbm")
        S.add("sp", lambda e: e.dma_start(out=SBMf, in_=d["sbmask"]), writes=[kSBM], dma="sbm")
        S.add("dve", lambda e: e.tensor_copy(out=SBM, in_=SBMf), reads=[kSBM], writes=[kSBM])
        WQ = self.alloc([NCH, 128], BF16)
        WK = self.alloc([NCH, 128], BF16)
        WV = self.alloc([NCH, 128], BF16)
        WOp = self.alloc([D], BF16)
        QT = self.alloc([S_TOK], BF16)
        KT = self.alloc([S_TOK], BF16)
        V = self.alloc([NT, 128], BF16)
        E1 = [self.alloc([S_TOK]) for _ in range(NSET)]
        SP = [self.alloc([S_TOK]) for _ in range(NSET)]
        EX = [self.alloc([S_TOK]) for _ in range(NSET)]
        AT = [self.alloc([NT, 128], BF16) for _ in range(NSET)]
        SM = [self.alloc([8]) for _ in range(NSET)]
        OS = [self.alloc([128], BF16) for _ in range(2)]
        OT = [self.alloc([128], BF16) for _ in range(2)]
        for s_ in range(NSET):
            S.add("pool", lambda e, s_=s_: e.memset(EX[s_][:, 0:1], 0.0), writes=[("EX", s_)])

        def load_w(m):
            S.add("pool", lambda e, m=m: e.dma_start(out=WQ, in_=w_in[:, :, m * 128:(m + 1) * 128]), writes=["WQ"], dma="swq")
            S.add("pool", lambda e, m=m: e.dma_start(out=WK, in_=w_in[:, :, 1024 + m * 128:1024 + (m + 1) * 128]), writes=["WK"], dma="swk")
            S.add("pool", lambda e, m=m: e.dma_start(out=WV, in_=w_in[:, :, 2048 + m * 128:2048 + (m + 1) * 128]), writes=["WV"], dma="swv")

        unit = 0
        zc = 0
        for m in range(8):
            load_w(m)
            for c in range(4):
                tok = slice(c * 512, (c + 1) * 512)
                for which, W_, DST, scl in ((0, WQ, QT, 0.125), (1, WK, KT, 1.0)):
                    pp = PS[which]
                    for kk in range(NCH):
                        S.add("pe", lambda e, pp=pp, kk=kk, W_=W_, tok=tok: e.matmul(pp[:], W_[:, kk, :], XT[:, kk, tok], start=(kk == 0), stop=(kk == 7)),
                              reads=["WQ" if which == 0 else "WK"] + [("XT", tt) for tt in range(4 * c, 4 * c + 4)], writes=[("ps", which)])
                    S.add("act", lambda e, pp=pp, DST=DST, tok=tok, scl=scl: e.activation(out=DST[:, tok], in_=pp[:], func=AF.Copy, scale=float(scl)),
                          reads=[("ps", which)], writes=[("QTKT", which, c)])
            for t in range(NT):
                pv = PS[2 + t % 2]
                for kk in range(NCH):
                    S.add("pe", lambda e, pv=pv, kk=kk, t=t: e.matmul(pv[:, 0:128], XT[:, kk, t * 128:(t + 1) * 128], WV[:, kk, :], start=(kk == 0), stop=(kk == 7)),
                          reads=["WV", ("XT", t)], writes=[("ps", 2 + t % 2)])
                S.add("dve", lambda e, pv=pv, t=t: e.tensor_copy(out=V[:, t, :], in_=pv[:, 0:128]), reads=[("ps", 2 + t % 2)], writes=[("V", t)])
            S.add("pool", lambda e, m=m: e.dma_start(out=WOp, in_=w_out[m * 128:(m + 1) * 128, :]), writes=["WOp"], dma="swo")
            for qt in range(NT):
                n = 128 * (qt + 1)
                ob = qt % 2
                for hh in range(2):
                    sb_ = unit % NSET
                    unit += 1
                    e1, sp, ex, at, sm = E1[sb_], SP[sb_], EX[sb_], AT[sb_], SM[sb_]
                    a_ = ex.bitcast(BF16)[:, S_TOK:2 * S_TOK]
                    hp = slice(64 * hh, 64 * hh + 64)
                    nkb = (n + 511) // 512
                    for kb in range(nkb):
                        w = min(512, n - 512 * kb)
                        zb = zc % 2
                        zc += 1
                        pz = PS[zb]
                        last = (kb == nkb - 1)
                        S.add("pe", lambda e, pz=pz, w=w, kb=kb, hp=hp, qt=qt, last=last: e.matmul(pz[:, 0:w], QT[hp, qt * 128:(qt + 1) * 128], KT[hp, 512 * kb:512 * kb + w], start=True, stop=(not last)),
                              reads=[("QTKT", 0, qt // 4), ("QTKT", 1, kb)], writes=[("ps", zb)])
                        if last:
                            S.add("pe", lambda e, pz=pz, w=w: e.matmul(pz[:, w - 128:w], self.IDB[:], SBM, start=False, stop=True),
                                  reads=["IDB", kSBM], writes=[("ps", zb)])
                        S.add("act", lambda e, pz=pz, e1=e1, kb=kb, w=w: e.activation(out=e1[:, 512 * kb:512 * kb + w], in_=pz[:, 0:w], func=AF.Exp),
                              reads=[("ps", zb)], writes=[("E1", sb_)])
                    S.add("act", lambda e, e1=e1, sp=sp, n=n, sm=sm: e.activation(out=sp[:, 0:n], in_=e1[:, 0:n], func=AF.Ln, bias=1.0, scale=1.0, accum_out=sm[:, 0:1]),
                          reads=[("E1", sb_)], writes=[("SP", sb_), ("TT", sb_)])
                    S.add("dve", lambda e, sm=sm: e.tensor_scalar(out=sm[:, 1:2], in0=sm[:, 0:1], scalar1=-1.0, scalar2=None, op0=ALU.mult), reads=[("TT", sb_)], writes=[("NTT", sb_)])
                    S.add("dve", lambda e, sp=sp, ex=ex, n=n: e.tensor_tensor_scan(out=ex[:, 1:n], data0=sp[:, 0:n - 1], data1=sp[:, 0:n - 1], initial=0.0, op0=ALU.add, op1=ALU.max),
                          reads=[("SP", sb_)], writes=[("EX", sb_)])
                    S.add("act", lambda e, ex=ex, sp=sp, n=n, sm=sm: e.activation(out=sp[:, 0:n], in_=ex[:, 0:n], func=AF.Exp, bias=sm[:, 1:2], scale=1.0),
                          reads=[("EX", sb_), ("NTT", sb_)], writes=[("SP", sb_)])
                    S.add("dve", lambda e, e1=e1, sp=sp, a_=a_, n=n: e.tensor_tensor(out=a_[:, 0:n], in0=e1[:, 0:n], in1=sp[:, 0:n], op=ALU.mult),
                          reads=[("E1", sb_), ("SP", sb_)], writes=[("EX", sb_), ("A", sb_)])
                    for g in range((qt + 8) // 8):
                        nb = min(8, qt + 1 - 8 * g)
                        ptb = PSB[2 + g % 2]
                        for i in range(nb):
                            kb2 = 8 * g + i
                            S.add("pe", lambda e, ptb=ptb, i=i, kb2=kb2, a_=a_: e.transpose(ptb[:, i * 128:(i + 1) * 128], a_[:, kb2 * 128:(kb2 + 1) * 128], self.IDB[:]),
                                  reads=[("A", sb_), "IDB"], writes=[("ps", 2 + g % 2)])
                        S.add("act", lambda e, ptb=ptb, at=at, g=g, nb=nb: e.activation(out=at[:, 8 * g:8 * g + nb, :], in_=ptb[:, 0:nb * 128].rearrange("p (a b) -> p a b", a=nb), func=AF.Copy),
                              reads=[("ps", 2 + g % 2)], writes=[("AT", sb_, g)])
                    po = PS[4 + hh]
                    for kb2 in range(qt + 1):
                        S.add("pe", lambda e, po=po, kb2=kb2, at=at, hp=hp, qt=qt: e.matmul(po[:, 0:64], at[:, kb2, :], V[:, kb2, hp], start=(kb2 == 0), stop=(kb2 == qt)),
                              reads=[("AT", sb_, kb2 // 8), ("V", kb2)], writes=[("ps", 4 + hh)])
                    S.add("dve", lambda e, po=po, hp=hp, ob=ob: e.tensor_copy(out=OS[ob][:, hp], in_=po[:, 0:64]), reads=[("ps", 4 + hh)], writes=[("OS", ob, hh)])
                ptb = PSB[6]
                S.add("pe", lambda e, ptb=ptb, ob=ob: e.transpose(ptb[:, 0:128], OS[ob], self.IDB[:]), reads=[("OS", ob, 0), ("OS", ob, 1), "IDB"], writes=[("ps", 6)])
                S.add("dve", lambda e, ptb=ptb, ob=ob: e.tensor_copy(out=OT[ob], in_=ptb[:, 0:128]), reads=[("ps", 6)], writes=[("OT", ob)])
                for hf in range(2):
                    px = PS[6 + hf]
                    S.add("pe", lambda e, px=px, ob=ob, hf=hf: e.matmul(px[:], OT[ob], WOp[:, hf * 512:(hf + 1) * 512], start=True, stop=True),
                          reads=[("OT", ob), "WOp"], writes=[("ps", 6 + hf)])
                    S.add("dve", lambda e, px=px, qt=qt, hf=hf: e.tensor_tensor(out=X[:, qt, hf * 512:(hf + 1) * 512], in0=px[:], in1=X[:, qt, hf * 512:(hf + 1) * 512], op=ALU.add),
                          reads=[("ps", 6 + hf), ("X", qt)], writes=[("X", qt)])
        S.barrier()
        self.arena_reset()


def _host_inputs(inp, b, consts, big=True):
    f = lambda a: np.ascontiguousarray(np.asarray(a, dtype=np.float32))
    m = {}
    m["x"] = f(inp["x"][b])
    m["pT"] = f(np.transpose(np.asarray(inp["p"])[:, b], (0, 2, 1)))
    for k in ("ret_w_in", "ret_w_out", "sb_w_in", "sb_w_out", "router_w", "router_b", "w_gate_up", "w_down",
              "b_down", "ple_w", "ple_gate_w", "ple_gate_b"):
        if not big and k in ("w_gate_up", "w_down"):
            m[k] = f(np.asarray(inp[k])[0:1, 0:1])
        else:
            m[k] = f(inp[k])
    m["lnp"] = f(np.stack([inp["ln1_g"], inp["ln1_b"], inp["ln2_g"], inp["ln2_b"]], axis=1))
    bgu = np.asarray(inp["b_gate_up"], dtype=np.float32).reshape(DEPTH, NE, 8, 128, 2)
    m["bgu"] = f(np.transpose(bgu, (0, 3, 1, 2, 4)).reshape(DEPTH, 128, NE * 16))
    for k in ("identf", "identb", "ones", "ut", "iota", "onesb", "cos", "sin", "rmask", "sbmask"):
        m[k] = consts[k]
    return m


def run_layers(inp, layers, n_exp=NE, stages=("mix", "moe", "ple"), cores=8):
    bld = Builder(layers, n_exp=n_exp, stages=stages)
    nc = bld.build()
    in_maps = [_host_inputs(inp, b, bld.consts, big=("moe" in stages)) for b in range(cores)]
    res = run_bass_kernel_spmd(nc, in_maps, core_ids=list(range(cores)))
    return np.stack([res.results[b]["y"] for b in range(cores)], axis=0)


def kernel(**inputs):
    out = run_layers(inputs, layers=range(DEPTH))
    return out.astype(np.float32)
```

```python
import math
from contextlib import ExitStack

import ml_dtypes
import numpy as np

import concourse.bass as bass
import concourse.mybir as mybir
from concourse.bass_utils import run_bass_kernel_spmd

F32 = mybir.dt.float32
BF16 = mybir.dt.bfloat16
ALU = mybir.AluOpType
AF = mybir.ActivationFunctionType
AX = mybir.AxisListType

S_TOK = 2048
D = 1024
NT = 16
NCH = 8
DEPTH = 4
NE = 32
DN_ALPHA = float((2 * DEPTH) ** 0.25)
LN_EPS = 1e-5
GN_EPS = 1e-6
SEG = 30000
GRP = 4


class _Op:
    __slots__ = ("eng", "fn", "deps", "dma", "sig", "sigval", "idx")


class Sched:
    ENGS = ("pe", "act", "dve", "pool", "sp")

    def __init__(self, nc):
        self.nc = nc
        self.streams = {e: [] for e in self.ENGS}
        self.last_w = {}
        self.readers = {}
        self.dma_cnt = {}
        self.dma_last = {}
        self.barrier_ops = []
        self.barrier_seen = {e: True for e in self.ENGS}

    def barrier(self):
        ops = []
        for e in self.ENGS:
            for op in reversed(self.streams[e]):
                if op.dma is None:
                    ops.append(op)
                    break
        ops.extend(self.dma_last.values())
        self.barrier_ops = ops
        self.barrier_seen = {e: False for e in self.ENGS}
        self.last_w = {}
        self.readers = {}

    def add(self, eng, fn, reads=(), writes=(), dma=None):
        op = _Op()
        op.eng, op.fn, op.dma = eng, fn, None
        op.sig = False
        op.sigval = None
        ps_r = [k for k in reads if isinstance(k, tuple) and k[0] == "ps"]
        if ps_r:
            reads = [k for k in reads if not (isinstance(k, tuple) and k[0] == "ps")]
            writes = list(writes) + ps_r
        deps = {}
        for k in reads:
            w = self.last_w.get(k)
            if w is not None:
                deps[w] = True
        for k in writes:
            w = self.last_w.get(k)
            if w is not None:
                deps[w] = True
            for r in self.readers.get(k, ()):
                if r not in deps:
                    deps[r] = False
        if not self.barrier_seen[eng]:
            self.barrier_seen[eng] = True
            for b in self.barrier_ops:
                deps[b] = True
        for k in reads:
            self.readers.setdefault(k, []).append(op)
        for k in writes:
            self.last_w[k] = op
            self.readers[k] = []
        pruned = []
        latest = {}
        for d, strong in deps.items():
            if d is op:
                continue
            if d.dma is not None:
                pruned.append(d)
                continue
            if d.eng == eng and (eng == "pe" or not strong):
                continue
            o = latest.get(d.eng)
            if o is None or o.idx < d.idx:
                latest[d.eng] = d
        pruned.extend(latest.values())
        op.deps = pruned
        op.idx = len(self.streams[eng])
        if dma is not None:
            c = self.dma_cnt.get(dma, 0) + 1
            self.dma_cnt[dma] = c
            op.dma = (dma, 16 * c)
            self.dma_last[dma] = op
        self.streams[eng].append(op)
        return op

    def emit(self):
        nc = self.nc
        for e in self.ENGS:
            for op in self.streams[e]:
                for d in op.deps:
                    if d.dma is None:
                        d.sig = True
        nsegs = {}
        for e in self.ENGS:
            c = 0
            for op in self.streams[e]:
                if op.sig and op.dma is None:
                    op.sigval = (e, c // SEG, c % SEG + 1)
                    c += 1
            nsegs[e] = (c + SEG - 1) // SEG
        with ExitStack() as es:
            sems = {}
            for e in self.ENGS:
                for s in range(nsegs[e]):
                    sems[(e, s)] = es.enter_context(nc.semaphore(f"s_{e}_{s}"))
            dsems = {}
            for g in self.dma_cnt:
                dsems[g] = es.enter_context(nc.semaphore(f"d_{g}"))
            self.n_sems = len(sems) + len(dsems)
            block = es.enter_context(nc.Block())

            def run(ename, eng):
                waited = {}
                for op in self.streams[ename]:
                    need = {}
                    for d in op.deps:
                        if d.dma is not None:
                            key, val = ("d", d.dma[0]), d.dma[1]
                        else:
                            key, val = (d.sigval[0], d.sigval[1]), d.sigval[2]
                        if need.get(key, 0) < val:
                            need[key] = val
                    for key, val in need.items():
                        if waited.get(key, 0) >= val:
                            continue
                        waited[key] = val
                        sem = dsems[key[1]] if key[0] == "d" else sems[key]
                        eng.wait_ge(sem, val)
                    ins = op.fn(eng)
                    if op.dma is not None:
                        ins.then_inc(dsems[op.dma[0]], 16)
                    elif op.sig:
                        ins.then_inc(sems[(op.sigval[0], op.sigval[1])], 1)

            block.tensor(lambda eng: run("pe", eng))
            block.scalar(lambda eng: run("act", eng))
            block.vector(lambda eng: run("dve", eng))
            block.gpsimd(lambda eng: run("pool", eng))
            block.sync(lambda eng: run("sp", eng))


def _consts():
    c = {}
    c["identf"] = np.eye(128, dtype=np.float32)
    c["identb"] = np.eye(128, dtype=np.float32).astype(ml_dtypes.bfloat16)
    c["ones"] = np.ones((1, 128), dtype=np.float32)
    ii = np.arange(128)
    c["ut"] = (ii[:, None] < ii[None, :]).astype(np.float32).astype(ml_dtypes.bfloat16)
    c["iota"] = np.ascontiguousarray(np.broadcast_to(ii[None, :], (128, 128))).astype(np.float32)
    c["onesb"] = np.ones((128, 128), dtype=np.float32).astype(ml_dtypes.bfloat16)
    half = 128
    inv_freq = (1.0 / (10000.0 ** (np.arange(half, dtype=np.float32) / np.float32(half)))).astype(np.float32)
    ang = (np.arange(S_TOK, dtype=np.float32)[None, :] * inv_freq[:, None]).astype(np.float32)
    c["cos"] = np.cos(ang).astype(np.float32)
    c["sin"] = np.sin(ang).astype(np.float32)
    H = 4
    lg = np.log(1.0 - 2.0 ** (-5.0 - np.arange(H, dtype=np.float64)))
    sl = np.arange(128)[:, None].astype(np.float64)
    tl = np.arange(256)[None, :].astype(np.float64)
    masks = np.zeros((H, 3, 128, 256), dtype=np.float32)
    for h in range(H):
        masks[h, 0] = np.exp(lg[h] * (tl - sl)) / 16.0
        for m in range(2):
            s_abs = 128 * m + sl
            dec = np.exp(lg[h] * np.abs(tl - s_abs))
            ok = (np.floor(s_abs / 64) <= np.floor(tl / 64))
            masks[h, 1 + m] = dec * ok / 16.0
    c["rmask"] = np.ascontiguousarray(masks.transpose(2, 0, 1, 3))
    c["rgam"] = np.exp(lg)
    t = np.arange(128)[:, None]
    s = np.arange(128)[None, :]
    c["sbmask"] = np.where(s < t, 0.0, -30000.0).astype(np.float32)
    return c


class Builder:
    def __init__(self, layers, n_exp=NE, stages=("mix", "moe", "ple")):
        self.layers = list(layers)
        self.n_exp = n_exp
        self.stages = stages
        self.nc = bass.Bass("TRN2", target_bir_lowering=False)
        self.consts = _consts()

    def dram_in(self, name, shape, dt=F32):
        return self.nc.dram_tensor(name, list(shape), dt, kind="ExternalInput").ap()

    def build(self):
        nc = self.nc
        L = len(self.layers)
        d = {}
        d["x"] = self.dram_in("x", [S_TOK, D])
        d["pT"] = self.dram_in("pT", [DEPTH, 256, S_TOK])
        d["ret_w_in"] = self.dram_in("ret_w_in", [2, D, 6144])
        d["ret_w_out"] = self.dram_in("ret_w_out", [2, 2048, D])
        d["sb_w_in"] = self.dram_in("sb_w_in", [2, D, 3 * D])
        d["sb_w_out"] = self.dram_in("sb_w_out", [2, D, D])
        d["lnp"] = self.dram_in("lnp", [DEPTH, 4, D])
        d["router_w"] = self.dram_in("router_w", [DEPTH, D, NE])
        d["router_b"] = self.dram_in("router_b", [DEPTH, NE])
        big = "moe" in self.stages
        d["w_gate_up"] = self.dram_in("w_gate_up", [DEPTH, NE, D, 2 * D] if big else [1, 1, D, 2 * D])
        d["bgu"] = self.dram_in("bgu", [DEPTH, 128, NE * 16])
        d["w_down"] = self.dram_in("w_down", [DEPTH, NE, D, D] if big else [1, 1, D, D])
        d["b_down"] = self.dram_in("b_down", [DEPTH, NE, D])
        d["ple_w"] = self.dram_in("ple_w", [DEPTH, 256, D])
        d["ple_gate_w"] = self.dram_in("ple_gate_w", [DEPTH, D, D])
        d["ple_gate_b"] = self.dram_in("ple_gate_b", [DEPTH, D])
        d["identf"] = self.dram_in("identf", [128, 128])
        d["identb"] = self.dram_in("identb", [128, 128], BF16)
        d["ones"] = self.dram_in("ones", [1, 128])
        d["ut"] = self.dram_in("ut", [128, 128], BF16)
        d["iota"] = self.dram_in("iota", [128, 128])
        d["onesb"] = self.dram_in("onesb", [128, 128], BF16)
        d["cos"] = self.dram_in("cos", [128, S_TOK])
        d["sin"] = self.dram_in("sin", [128, S_TOK])
        d["rmask"] = self.dram_in("rmask", [128, 4, 3, 256])
        d["sbmask"] = self.dram_in("sbmask", [128, 128])
        d["y"] = nc.dram_tensor("y", [S_TOK, D], F32, kind="ExternalOutput").ap()
        self.d = d

        AW = 27648
        with ExitStack() as es:
            sb = lambda n, s, dt: es.enter_context(nc.sbuf_tensor(n, s, dt))
            self.X = sb("X", [128, NT, D], F32)
            self.XT = sb("XT", [128, NCH, S_TOK], BF16)
            self.IDF = sb("IDF", [128, 128], F32)
            self.IDB = sb("IDB", [128, 128], BF16)
            self.ONES = sb("ONES", [1, 128], F32)
            self.GALL = sb("GALL", [128, NT, NE], F32)
            self.ARENA = sb("ARENA", [128, AW], F32)
            self.AW = AW
            self.PS = [es.enter_context(nc.psum_tensor(f"ps{i}", [128, 512], F32)) for i in range(8)]
            self.S = Sched(nc)
            self.uid = 0
            self.program()
            self.S.emit()
        return nc

    def arena_reset(self):
        self.aptr = 0

    def alloc(self, shape, dt=F32, parts=128):
        n = int(np.prod(shape))
        words = n if dt == F32 else (n + 1) // 2
        words = (words + 7) // 8 * 8
        a = self.ARENA[0:parts, self.aptr:self.aptr + words]
        self.aptr += words
        assert self.aptr <= self.AW, f"arena overflow {self.aptr} > {self.AW}"
        if dt != F32:
            a = a.bitcast(dt)[:, 0:n]
        else:
            a = a[:, 0:n]
        if len(shape) == 2:
            a = a.rearrange("p (a b) -> p a b", a=shape[0])
        elif len(shape) == 3:
            a = a.rearrange("p (a b c) -> p a b c", a=shape[0], b=shape[1])
        return a

    def key(self, base):
        self.uid += 1
        return (base, self.uid)

    def program(self):
        S, d = self.S, self.d
        X = self.X
        S.add("sp", lambda e: e.dma_start(out=self.IDF[:], in_=d["identf"]), writes=["IDF"], dma="c0")
        S.add("sp", lambda e: e.dma_start(out=self.IDB[:], in_=d["identb"]), writes=["IDB"], dma="c1")
        S.add("sp", lambda e: e.dma_start(out=self.ONES[:], in_=d["ones"]), writes=["ONES"], dma="c2")
        xin = d["x"].rearrange("(t p) f -> p t f", p=128)
        for q in range(4):
            S.add("sp", lambda e, q=q: e.dma_start(out=X[:, 4 * q:4 * q + 4, :], in_=xin[:, 4 * q:4 * q + 4, :]),
                  writes=[("X", t) for t in range(4 * q, 4 * q + 4)], dma=f"xin{q}")
        for l in self.layers:
            if "mix" in self.stages:
                self.arena_reset()
                S.barrier()
                self.make_xt(l, scale=DN_ALPHA, router=False)
                if l % 2 == 0:
                    self.retention(l)
                else:
                    self.stickbreak(l)
                self.layernorm(l, 0)
            if "moe" in self.stages:
                self.arena_reset()
                S.barrier()
                self.moe(l)
                self.layernorm(l, 1)
            if "ple" in self.stages:
                self.arena_reset()
                S.barrier()
                self.ple(l)
        yout = d["y"].rearrange("(t p) f -> p t f", p=128)
        for q in range(4):
            S.add("sp", lambda e, q=q: e.dma_start(out=yout[:, 4 * q:4 * q + 4, :], in_=X[:, 4 * q:4 * q + 4, :]),
                  reads=[("X", t) for t in range(4 * q, 4 * q + 4)], writes=[("yout", q)], dma=f"yout{q}")
        S.add("sp", lambda e: e.nop(), reads=[("yout", q) for q in range(4)])

    def make_xt(self, l, scale=None, router=False):
        S, d, X, XT, PS = self.S, self.d, self.X, self.XT, self.PS
        if router:
            RW = self.alloc([NCH, NE])
            RB = self.alloc([NE], parts=1)
            XT32 = [self.alloc([NCH, 128]) for _ in range(2)]
            LG = [self.alloc([NE]) for _ in range(2)]
            EXm = [self.alloc([NE]) for _ in range(2)]
            MSK = [self.alloc([NE]) for _ in range(2)]
            SM = [self.alloc([16]) for _ in range(2)]
            UT = self.alloc([128], BF16)
            kUT = self.key("UT")
            S.add("sp", lambda e: e.dma_start(out=UT, in_=d["ut"]), writes=[kUT], dma="ut")
            ONB = self.alloc([128], BF16)
            kONB = self.key("ONB")
            S.add("sp", lambda e: e.dma_start(out=ONB, in_=d["onesb"]), writes=[kONB], dma="onb")
            XB = self.XB
            kRW, kRB = self.key("RW"), self.key("RB")
            S.add("sp", lambda e: e.dma_start(out=RW, in_=d["router_w"][l].rearrange("(c p) n -> p c n", p=128)),
                  writes=[kRW], dma="rw")
            S.add("sp", lambda e: e.dma_start(out=RB, in_=d["router_b"][l:l + 1, :]), writes=[kRB], dma="rb")
        for t in range(NT):
            b = t % 2
            for h in range(2):
                pb = PS[2 * b + h]
                for j in range(4):
                    c = 4 * h + j
                    S.add("pe", lambda e, pb=pb, j=j, c=c, t=t: e.transpose(pb[:, j * 128:(j + 1) * 128], X[:, t, c * 128:(c + 1) * 128], self.IDF[:]),
                          reads=[("X", t), "IDF"], writes=[("ps", 2 * b + h)])
                if not router:
                    S.add("act", lambda e, pb=pb, h=h, t=t: e.activation(out=XT[:, 4 * h:4 * h + 4, t * 128:(t + 1) * 128],
                                                                          in_=pb[:].rearrange("p (a b) -> p a b", a=4), func=AF.Copy),
                          reads=[("ps", 2 * b + h)], writes=[("XT", t)])
                if router:
                    S.add("dve", lambda e, pb=pb, h=h, b=b: e.tensor_copy(out=XT32[b][:, 4 * h:4 * h + 4, :],
                                                                           in_=pb[:].rearrange("p (a b) -> p a b", a=4)),
                          reads=[("ps", 2 * b + h)], writes=[("XT32", b, h)])
            if router:
                S.add("act", lambda e, t=t: e.activation(out=XB[:, t, :], in_=X[:, t, :], func=AF.Copy), reads=[("X", t)], writes=[("XB", t)])
            if scale is not None:
                S.add("pool", lambda e, t=t: e.tensor_scalar(out=X[:, t, :], in0=X[:, t, :], scalar1=float(scale), scalar2=None, op0=ALU.mult),
                      reads=[("X", t)], writes=[("X", t)])
            if router:
                pl = PS[4 + b]
                for c in range(NCH):
                    S.add("pe", lambda e, pl=pl, c=c, b=b: e.matmul(pl[:, 0:NE], XT32[b][:, c, :], RW[:, c, :], start=(c == 0), stop=False),
                          reads=[("XT32", b, c // 4), kRW], writes=[("ps", 4 + b)])
                S.add("pe", lambda e, pl=pl: e.matmul(pl[:, 0:NE], self.ONES[0:1, :], RB[0:1, :], start=False, stop=True),
                      reads=["ONES", kRB], writes=[("ps", 4 + b)])
                lg, ex, mk, sm = LG[b], EXm[b], MSK[b], SM[b]
                kl = ("rt", b)
                S.add("dve", lambda e, pl=pl, lg=lg: e.tensor_copy(out=lg, in_=pl[:, 0:NE]), reads=[("ps", 4 + b)], writes=[(kl, "lg")])
                S.add("dve", lambda e, lg=lg, sm=sm: e.max(out=sm[:, 0:8], in_=lg), reads=[(kl, "lg")], writes=[(kl, "top")])
                S.add("dve", lambda e, lg=lg, sm=sm, mk=mk: e.tensor_scalar(out=mk, in0=lg, scalar1=sm[:, 3:4], scalar2=None, op0=ALU.is_ge),
                      reads=[(kl, "lg"), (kl, "top")], writes=[(kl, "mk")])
                S.add("dve", lambda e, sm=sm: e.tensor_scalar(out=sm[:, 8:9], in0=sm[:, 0:1], scalar1=-1.0, scalar2=None, op0=ALU.mult),
                      reads=[(kl, "top")], writes=[(kl, "nm")])
                S.add("dve", lambda e, mk=mk, t=t: e.tensor_copy(out=self.MB[:, t, :], in_=mk), reads=[(kl, "mk")], writes=[("MB", t)])
                gi = t % GRP
                S.add("pe", lambda e, pl=pl, t=t, gi=gi: e.matmul(pl[:, 64:64 + NE], UT, self.MB[:, t, :], start=True, stop=(gi == 0)),
                      reads=[("MB", t), kUT], writes=[("ps", 4 + b)])
                for tp in range(t - gi, t):
                    S.add("pe", lambda e, pl=pl, tp=tp, t=t: e.matmul(pl[:, 64:64 + NE], ONB, self.MB[:, tp, :], start=False, stop=(tp == t - 1)),
                          reads=[("MB", tp), kONB], writes=[("ps", 4 + b)])
                S.add("dve", lambda e, pl=pl, mk=mk, t=t: e.scalar_tensor_tensor(out=self.VAL[:, t, :], in0=pl[:, 64:64 + NE], scalar=127.5, in1=mk, op0=ALU.is_lt, op1=ALU.mult),
                      reads=[("ps", 4 + b), (kl, "mk")], writes=[("VAL", t)])
                S.add("dve", lambda e, pl=pl, t=t: e.tensor_copy(out=self.RK[:, t, :], in_=pl[:, 64:64 + NE]), reads=[("ps", 4 + b)], writes=[("RK", t)])
                S.add("act", lambda e, lg=lg, ex=ex, sm=sm: e.activation(out=ex, in_=lg, func=AF.Exp, bias=sm[:, 8:9], scale=1.0),
                      reads=[(kl, "lg"), (kl, "nm")], writes=[(kl, "ex")])
                S.add("dve", lambda e, ex=ex, mk=mk: e.tensor_tensor(out=ex, in0=ex, in1=mk, op=ALU.mult),
                      reads=[(kl, "ex"), (kl, "mk")], writes=[(kl, "ex")])
                S.add("dve", lambda e, ex=ex, sm=sm: e.reduce_sum(out=sm[:, 9:10], in_=ex, axis=AX.X),
                      reads=[(kl, "ex")], writes=[(kl, "ss")])
                S.add("dve", lambda e, sm=sm: e.reciprocal(out=sm[:, 10:11], in_=sm[:, 9:10]), reads=[(kl, "ss")], writes=[(kl, "rs")])
                S.add("dve", lambda e, ex=ex, sm=sm, t=t: e.tensor_scalar(out=self.GALL[:, t, :], in0=ex, scalar1=sm[:, 10:11], scalar2=None, op0=ALU.mult),
                      reads=[(kl, "ex"), (kl, "rs")], writes=[("G", t)])
                pg = PS[6 + b]
                S.add("pe", lambda e, pg=pg, t=t: e.transpose(pg[0:NE, 0:128], self.GALL[:, t, :], self.IDF[:]),
                      reads=[("G", t), "IDF"], writes=[("ps", 6 + b)])
                S.add("act", lambda e, pg=pg, t=t: e.activation(out=self.GT[0:NE, t * 128:(t + 1) * 128], in_=pg[0:NE, 0:128], func=AF.Copy),
                      reads=[("ps", 6 + b)], writes=[("GT", t)])

    def ln_alloc(self):
        return (self.alloc([D]), self.alloc([D]), [self.alloc([16]) for _ in range(2)])

    def layernorm(self, l, which, bufs=None):
        S, d, X = self.S, self.d, self.X
        G, B, ST = bufs if bufs is not None else self.ln_alloc()
        kG, kB = self.key("lng"), self.key("lnb")
        S.add("sp", lambda e: e.dma_start(out=G, in_=d["lnp"][l, 2 * which:2 * which + 1, :].to_broadcast([128, D])), writes=[kG], dma="lng")
        S.add("sp", lambda e: e.dma_start(out=B, in_=d["lnp"][l, 2 * which + 1:2 * which + 2, :].to_broadcast([128, D])), writes=[kB], dma="lnb")
        for t in range(NT):
            st = ST[t % 2]
            ks = ("lnst", t % 2)
            xt = X[:, t, :]
            S.add("dve", lambda e, st=st, xt=xt: e.bn_stats(out=st[:, 0:6], in_=xt[:, 0:512]), reads=[("X", t)], writes=[(ks, 0)])
            S.add("dve", lambda e, st=st, xt=xt: e.bn_stats(out=st[:, 6:12], in_=xt[:, 512:1024]), reads=[("X", t)], writes=[(ks, 1)])
            S.add("dve", lambda e, st=st: e.bn_aggr(out=st[:, 12:14], in_=st[:, 0:12]),
                  reads=[(ks, 0), (ks, 1)], writes=[(ks, 2)])
            S.add("dve", lambda e, st=st: e.tensor_scalar(out=st[:, 14:15], in0=st[:, 13:14], scalar1=float(LN_EPS), scalar2=None, op0=ALU.add),
                  reads=[(ks, 2)], writes=[(ks, 3)])
            S.add("act", lambda e, st=st: e.activation(out=st[:, 14:15], in_=st[:, 14:15], func=AF.Sqrt), reads=[(ks, 3)], writes=[(ks, 3)])
            S.add("dve", lambda e, st=st: e.reciprocal(out=st[:, 15:16], in_=st[:, 14:15]), reads=[(ks, 3)], writes=[(ks, 4)])
            S.add("dve", lambda e, st=st, xt=xt: e.tensor_scalar(out=xt, in0=xt, scalar1=st[:, 12:13], scalar2=st[:, 15:16], op0=ALU.subtract, op1=ALU.mult),
                  reads=[("X", t), (ks, 2), (ks, 4)], writes=[("X", t)])
            S.add("pool", lambda e, xt=xt: e.tensor_tensor(out=xt, in0=xt, in1=G, op=ALU.mult), reads=[("X", t), kG], writes=[("X", t)])
            S.add("pool", lambda e, xt=xt: e.tensor_tensor(out=xt, in0=xt, in1=B, op=ALU.add), reads=[("X", t), kB], writes=[("X", t)])

    def moe_bias(self, l, GT, BD, BGU):
        S, d, X, PS = self.S, self.d, self.X, self.PS
        kBD, kBGU = self.key("BD"), self.key("BGU")
        self.moe_keys = (kBD, kBGU)
        S.add("sp", lambda e: e.dma_start(out=BD, in_=d["b_down"][l]), writes=[kBD], dma="bd")
        S.add("sp", lambda e: e.dma_start(out=BGU, in_=d["bgu"][l]), writes=[kBGU], dma="bgu")
        for t in range(NT):
            for h in range(2):
                pb = PS[6 + h]
                S.add("pe", lambda e, pb=pb, t=t, h=h: e.matmul(pb[:], GT[0:NE, t * 128:(t + 1) * 128], BD[0:NE, h * 512:(h + 1) * 512], start=True, stop=True),
                      reads=[("GT", t), kBD], writes=[("ps", 6 + h)])
                S.add("dve", lambda e, pb=pb, t=t, h=h: e.tensor_tensor(out=X[:, t, h * 512:(h + 1) * 512], in0=pb[:], in1=X[:, t, h * 512:(h + 1) * 512], op=ALU.add),
                      reads=[("ps", 6 + h), ("X", t)], writes=[("X", t)])

    def moe(self, l):
        S, d, X, PS = self.S, self.d, self.X, self.PS
        PSB = [p[:].bitcast(BF16) for p in PS]
        NQ = GRP
        NST = NT // GRP
        NSL = NST * 128
        NSC = NSL // 512
        self.XB = self.XT[:].rearrange("p c s -> p (c s)").rearrange("p (t f) -> p t f", t=NT)
        XB = self.XB
        BGU = self.alloc([NE * 16])
        self.RK = self.alloc([NT, NE])
        self.VAL = self.alloc([NT, NE])
        self.MB = self.alloc([NT, NE], BF16)
        IOTA = self.alloc([128])
        kIO = self.key("iota")
        S.add("sp", lambda e: e.dma_start(out=IOTA, in_=d["iota"]), writes=[kIO], dma="iota")
        mark = self.aptr
        self.GT = self.alloc([S_TOK], parts=32)
        BD = self.alloc([D], parts=32)
        self.make_xt(l, scale=DN_ALPHA, router=True)
        self.moe_bias(l, self.GT, BD, BGU)
        S.barrier()
        self.aptr = mark
        kBD, kBGU = self.moe_keys
        RK, VAL = self.RK, self.VAL
        XG = self.alloc([NCH, NSL], BF16)
        ACTT = self.alloc([NCH, NSL], BF16)
        YS = self.alloc([NST, D], BF16)
        PA = self.alloc([NT, 128], BF16)
        PTA = self.alloc([NT, 128], BF16)
        NSLOT = 8
        WGU = [self.alloc([NCH, 256], BF16) for _ in range(NSLOT)]
        WD = self.alloc([NCH, D], BF16)
        NTMP = 3
        TG = [self.alloc([512]) for _ in range(NTMP)]
        TU = [self.alloc([512]) for _ in range(NTMP)]
        TS = [self.alloc([512]) for _ in range(NTMP)]
        wgu_src = d["w_gate_up"]
        wd_src = d["w_down"]
        n_chunks = self.n_exp * 8

        def dma_wgu(g):
            if g >= n_chunks:
                return
            slot = g % NSLOT
            wsrc = wgu_src[l, g // 8].rearrange("(c p) f -> p c f", p=128)
            j = g % 8
            S.add("pool", lambda e, slot=slot, j=j, wsrc=wsrc: e.dma_start(out=WGU[slot], in_=wsrc[:, :, 256 * j:256 * (j + 1)]),
                  writes=[("WGU", slot)], dma=f"wgu{slot}")

        def dma_wd(ei):
            src = wd_src[l, ei].rearrange("(c p) f -> p c f", p=128)
            for q in range(2):
                S.add("pool", lambda e, q=q, src=src: e.dma_start(out=WD[:, 4 * q:4 * q + 4, :], in_=src[:, 4 * q:4 * q + 4, :]),
                      writes=[("WD", q)], dma=f"wd{q}")

        def build_sel(ei):
            for t in range(NT):
                S.add("dve", lambda e, t=t, ei=ei: e.tensor_scalar(out=PA[:, t, :], in0=IOTA, scalar1=RK[:, t, ei:ei + 1], scalar2=VAL[:, t, ei:ei + 1],
                                                                    op0=ALU.is_equal, op1=ALU.mult),
                      reads=[kIO], writes=[("PA", t)])

        for g in range(NSLOT - 1):
            dma_wgu(g)
        build_sel(0)
        unit = 0
        gcnt = 0
        ycnt = 0
        for ei in range(self.n_exp):
            for g8 in range(NT // 8):
                ptb = PSB[6 + g8]
                for i in range(8):
                    t = 8 * g8 + i
                    S.add("pe", lambda e, ptb=ptb, i=i, t=t: e.transpose(ptb[:, i * 128:(i + 1) * 128], PA[:, t, :], self.IDB[:]),
                          reads=[("PA", t), "IDB"], writes=[("ps", 6 + g8)])
                S.add("act", lambda e, ptb=ptb, g8=g8: e.activation(out=PTA[:, 8 * g8:8 * g8 + 8, :], in_=ptb[:].rearrange("p (a b) -> p a b", a=8), func=AF.Copy),
                      reads=[("ps", 6 + g8)], writes=[("PTA", g8)])
            for dch in range(NCH):
                for sc in range(NSC):
                    bi = gcnt % 6
                    gcnt += 1
                    pb = PS[bi]
                    for gl in range(4):
                        g_ = sc * 4 + gl
                        for i in range(GRP):
                            t = g_ * GRP + i
                            S.add("pe", lambda e, pb=pb, gl=gl, i=i, t=t, dch=dch: e.matmul(pb[:, gl * 128:(gl + 1) * 128], XB[:, t, dch * 128:(dch + 1) * 128], PA[:, t, :],
                                                                                         start=(i == 0), stop=(i == GRP - 1)),
                                  reads=[("PA", t)], writes=[("ps", bi)])
                    eng = "act" if (gcnt % 2 == 0) else "dve"
                    if eng == "act":
                        S.add("act", lambda e, pb=pb, dch=dch, sc=sc: e.activation(out=XG[:, dch, sc * 512:(sc + 1) * 512], in_=pb[:], func=AF.Copy), reads=[("ps", bi)], writes=[("XG", dch, sc)])
                    else:
                        S.add("dve", lambda e, pb=pb, dch=dch, sc=sc: e.tensor_copy(out=XG[:, dch, sc * 512:(sc + 1) * 512], in_=pb[:]), reads=[("ps", bi)], writes=[("XG", dch, sc)])
            for j in range(8):
                dma_wgu(ei * 8 + j + NSLOT - 1)
                if j == 1:
                    dma_wd(ei)
                slot = (ei * 8 + j) % NSLOT
                W = WGU[slot]
                for c in range(NSC):
                    pr = unit % 3
                    pg_, pu_ = PS[2 * pr], PS[2 * pr + 1]
                    tb = unit % NTMP
                    unit += 1
                    tok = slice(c * 512, (c + 1) * 512)
                    for k in range(NCH):
                        S.add("pe", lambda e, pg_=pg_, W=W, k=k, tok=tok: e.matmul(pg_[:], W[:, k, 0:256:2], XG[:, k, tok], start=(k == 0), stop=(k == 7)),
                              reads=[("WGU", slot), ("XG", k, c)], writes=[("ps", 2 * pr)])
                    for k in range(NCH):
                        S.add("pe", lambda e, pu_=pu_, W=W, k=k, tok=tok: e.matmul(pu_[:], W[:, k, 1:256:2], XG[:, k, tok], start=(k == 0), stop=(k == 7)),
                              reads=[("WGU", slot), ("XG", k, c)], writes=[("ps", 2 * pr + 1)])
                    bg = BGU[:, ei * 16 + 2 * j:ei * 16 + 2 * j + 1]
                    bu = BGU[:, ei * 16 + 2 * j + 1:ei * 16 + 2 * j + 2]
                    tg, tu, ts = TG[tb], TU[tb], TS[tb]
                    S.add("dve", lambda e, tg=tg, pg_=pg_, bg=bg: e.tensor_scalar(out=tg, in0=pg_[:], scalar1=bg, scalar2=7.0, op0=ALU.add, op1=ALU.min),
                          reads=[("ps", 2 * pr), kBGU], writes=[("TG", tb)])
                    S.add("act", lambda e, tu=tu, pu_=pu_, bu=bu: e.activation(out=tu, in_=pu_[:], func=AF.Identity, bias=bu, scale=1.0),
                          reads=[("ps", 2 * pr + 1), kBGU], writes=[("TU", tb)])
                    S.add("act", lambda e, ts=ts, tg=tg: e.activation(out=ts, in_=tg, func=AF.Sigmoid, scale=1.702),
                          reads=[("TG", tb)], writes=[("TS", tb)])
                    S.add("pool", lambda e, tu=tu: e.tensor_scalar(out=tu, in0=tu, scalar1=7.0, scalar2=-7.0, op0=ALU.min, op1=ALU.max),
                          reads=[("TU", tb)], writes=[("TU", tb)])
                    S.add("pool", lambda e, tg=tg, ts=ts: e.tensor_tensor(out=tg, in0=tg, in1=ts, op=ALU.mult),
                          reads=[("TG", tb), ("TS", tb)], writes=[("TG", tb)])
                    S.add("dve", lambda e, tu=tu, tg=tg, j=j, tok=tok: e.scalar_tensor_tensor(out=ACTT[:, j, tok], in0=tu, scalar=1.0, in1=tg, op0=ALU.add, op1=ALU.mult),
                          reads=[("TU", tb), ("TG", tb)], writes=[("ACTT", j, c)])
            for st in range(NST):
                for h in range(2):
                    pi = 6 + (ycnt % 2)
                    ycnt += 1
                    py = PS[pi]
                    for k in range(NCH):
                        S.add("pe", lambda e, py=py, k=k, st=st, h=h: e.matmul(py[:], ACTT[:, k, st * 128:(st + 1) * 128], WD[:, k, h * 512:(h + 1) * 512], start=(k == 0), stop=(k == 7)),
                              reads=[("ACTT", k, st // 4), ("WD", k // 4)], writes=[("ps", pi)])
                    S.add("act", lambda e, py=py, st=st, h=h: e.activation(out=YS[:, st, h * 512:(h + 1) * 512], in_=py[:], func=AF.Copy), reads=[("ps", pi)], writes=[("YS", st, h)])
            if ei + 1 < self.n_exp:
                build_sel(ei + 1)
            for t in range(NT):
                for h in range(2):
                    pi = 6 + (ycnt % 2)
                    ycnt += 1
                    py = PS[pi]
                    S.add("pe", lambda e, py=py, t=t, h=h: e.matmul(py[:], PTA[:, t, :], YS[:, t // NQ, h * 512:(h + 1) * 512], start=True, stop=True),
                          reads=[("PTA", t // 8), ("YS", t // NQ, h)], writes=[("ps", pi)])
                    S.add("dve", lambda e, py=py, t=t, h=h, ei=ei: e.scalar_tensor_tensor(out=X[:, t, h * 512:(h + 1) * 512], in0=py[:], scalar=self.GALL[:, t, ei:ei + 1],
                                                                                          in1=X[:, t, h * 512:(h + 1) * 512], op0=ALU.mult, op1=ALU.add),
                          reads=[("ps", pi), ("X", t)], writes=[("X", t)])
        S.barrier()
        self.arena_reset()

    def ple(self, l):
        S, d, X, XT, PS = self.S, self.d, self.X, self.XT, self.PS
        self.make_xt(l, scale=None, router=False)
        WG = self.alloc([NCH, D], BF16)
        WP = self.alloc([2, D], BF16)
        PT_ = self.alloc([2, S_TOK], BF16)
        BG = self.alloc([D], parts=1)
        SG = [self.alloc([D]) for _ in range(2)]
        kWG, kWP, kPT, kBG = self.key("WG"), self.key("WP"), self.key("PT"), self.key("BG")
        S.add("pool", lambda e: e.dma_start(out=WG, in_=d["ple_gate_w"][l].rearrange("(c p) f -> p c f", p=128)), writes=[kWG], dma="pwg")
        S.add("pool", lambda e: e.dma_start(out=WP, in_=d["ple_w"][l].rearrange("(c p) f -> p c f", p=128)), writes=[kWP], dma="pwp")
        S.add("pool", lambda e: e.dma_start(out=PT_, in_=d["pT"][l].rearrange("(c p) s -> p c s", p=128)), writes=[kPT], dma="ppt")
        S.add("sp", lambda e: e.dma_start(out=BG, in_=d["ple_gate_b"][l:l + 1, :]), writes=[kBG], dma="pbg")
        for t in range(NT):
            sg = SG[t % 2]
            for h in range(2):
                pg = PS[4 + h]
                pp = PS[6 + h]
                cols = slice(h * 512, (h + 1) * 512)
                for k in range(NCH):
                    S.add("pe", lambda e, pg=pg, k=k, t=t, cols=cols: e.matmul(pg[:], XT[:, k, t * 128:(t + 1) * 128], WG[:, k, cols], start=(k == 0), stop=False),
                          reads=[("XT", t), kWG], writes=[("ps", 4 + h)])
                S.add("pe", lambda e, pg=pg, cols=cols: e.matmul(pg[:], self.ONES[0:1, :], BG[0:1, cols], start=False, stop=True),
                      reads=["ONES", kBG], writes=[("ps", 4 + h)])
                for k in range(2):
                    S.add("pe", lambda e, pp=pp, k=k, t=t, cols=cols: e.matmul(pp[:], PT_[:, k, t * 128:(t + 1) * 128], WP[:, k, cols], start=(k == 0), stop=(k == 1)),
                          reads=[kPT, kWP], writes=[("ps", 6 + h)])
                S.add("act", lambda e, sg=sg, pg=pg, cols=cols: e.activation(out=sg[:, cols], in_=pg[:], func=AF.Sigmoid),
                      reads=[("ps", 4 + h)], writes=[("SG", t % 2, h)])
                S.add("dve", lambda e, sg=sg, pp=pp, cols=cols: e.tensor_tensor(out=sg[:, cols], in0=pp[:], in1=sg[:, cols], op=ALU.mult),
                      reads=[("ps", 6 + h), ("SG", t % 2, h)], writes=[("SG", t % 2, h)])
                S.add("pool", lambda e, sg=sg, t=t, cols=cols: e.tensor_tensor(out=X[:, t, cols], in0=X[:, t, cols], in1=sg[:, cols], op=ALU.add),
                      reads=[("SG", t % 2, h), ("X", t)], writes=[("X", t)])

    def retention(self, l):
        S, d, X, XT, PS = self.S, self.d, self.X, self.XT, self.PS
        jl = l // 2
        w_in = d["ret_w_in"][jl].rearrange("(c p) f -> p c f", p=128)
        w_out = d["ret_w_out"][jl]
        gam = self.consts["rgam"]
        COS = self.alloc([S_TOK])
        SIN = self.alloc([S_TOK])
        QT = self.alloc([2, S_TOK], BF16)
        KT = self.alloc([2, S_TOK], BF16)
        V = self.alloc([NT, 512], BF16)
        WC = self.alloc([NCH, 512], BF16)
        WO = self.alloc([4, D], BF16)
        MASK = self.alloc([4, 3, 256])
        kC, kSn, kM = self.key("cos"), self.key("sin"), self.key("rmask")
        S.add("sp", lambda e: e.dma_start(out=COS, in_=d["cos"]), writes=[kC], dma="cos")
        S.add("sp", lambda e: e.dma_start(out=SIN, in_=d["sin"]), writes=[kSn], dma="sin")
        S.add("sp", lambda e: e.dma_start(out=MASK, in_=d["rmask"]), writes=[kM], dma="rmask")
        mark = self.aptr
        PSB = [p[:].bitcast(BF16) for p in PS]
        for h in range(4):
            if h > 0:
                S.barrier()
            self.aptr = mark
            WA = self.alloc([NCH, 512], BF16)
            WB = self.alloc([NCH, 512], BF16)
            T1 = [self.alloc([512]) for _ in range(2)]
            T2 = [self.alloc([512]) for _ in range(2)]
            kWA, kWB, kWC, kWO = self.key("WA"), self.key("WB"), self.key("WC"), self.key("WO")
            S.add("pool", lambda e, h=h, WA=WA: e.dma_start(out=WA[:, :, 0:256], in_=w_in[:, :, h * 256:(h + 1) * 256]), writes=[(kWA, 0)], dma="rwa0")
            S.add("pool", lambda e, h=h, WA=WA: e.dma_start(out=WA[:, :, 256:512], in_=w_in[:, :, 1024 + h * 256:1024 + (h + 1) * 256]), writes=[(kWA, 1)], dma="rwa1")
            S.add("pool", lambda e, h=h, WB=WB: e.dma_start(out=WB, in_=w_in[:, :, 2048 + h * 512:2048 + (h + 1) * 512]), writes=[kWB], dma="rwb")
            S.add("pool", lambda e, h=h: e.dma_start(out=WC, in_=w_in[:, :, 4096 + h * 512:4096 + (h + 1) * 512]), writes=[kWC], dma="rwc")
            S.add("pool", lambda e, h=h: e.dma_start(out=WO, in_=w_out[h * 512:(h + 1) * 512, :].rearrange("(c p) f -> p c f", p=128)), writes=[kWO], dma="rwo")
            u = 0
            for qk in range(2):
                DST = QT if qk == 0 else KT
                for c in range(4):
                    tok = slice(c * 512, (c + 1) * 512)
                    p1, p2 = PS[2 * (u % 2)], PS[2 * (u % 2) + 1]
                    tb = u % 2
                    u += 1
                    for a, pp in ((0, p1), (1, p2)):
                        for kk in range(NCH):
                            S.add("pe", lambda e, pp=pp, kk=kk, a=a, qk=qk, tok=tok, WA=WA: e.matmul(pp[:], WA[:, kk, qk * 256 + a * 128:qk * 256 + (a + 1) * 128], XT[:, kk, tok],
                                                                                              start=(kk == 0), stop=(kk == 7)),
                                  reads=[(kWA, qk)] + [("XT", tt) for tt in range(4 * c, 4 * c + 4)], writes=[("ps", 2 * tb + a)])
                    t1, t2 = T1[tb], T2[tb]
                    k1, k2 = ("T1", tb), ("T2", tb)
                    S.add("dve", lambda e, t1=t1, p1=p1, tok=tok: e.tensor_tensor(out=t1, in0=p1[:], in1=COS[:, tok], op=ALU.mult), reads=[("ps", 2 * tb), kC], writes=[k1])
                    S.add("dve", lambda e, t2=t2, p2=p2, tok=tok: e.tensor_tensor(out=t2, in0=p2[:], in1=SIN[:, tok], op=ALU.mult), reads=[("ps", 2 * tb + 1), kSn], writes=[k2])
                    S.add("pool", lambda e, t1=t1, t2=t2, DST=DST, tok=tok: e.tensor_tensor(out=DST[:, 0, tok], in0=t1, in1=t2, op=ALU.subtract), reads=[k1, k2], writes=[("QK", qk, c, 0)])
                    S.add("dve", lambda e, t1=t1, p1=p1, tok=tok: e.tensor_tensor(out=t1, in0=p1[:], in1=SIN[:, tok], op=ALU.mult), reads=[("ps", 2 * tb), kSn], writes=[k1])
                    S.add("dve", lambda e, t2=t2, p2=p2, tok=tok: e.tensor_tensor(out=t2, in0=p2[:], in1=COS[:, tok], op=ALU.mult), reads=[("ps", 2 * tb + 1), kC], writes=[k2])
                    S.add("pool", lambda e, t1=t1, t2=t2, DST=DST, tok=tok: e.tensor_tensor(out=DST[:, 1, tok], in0=t1, in1=t2, op=ALU.add), reads=[k1, k2], writes=[("QK", qk, c, 1)])
            for t in range(NT):
                pv = PS[4 + t % 2]
                for kk in range(NCH):
                    S.add("pe", lambda e, pv=pv, kk=kk, t=t, WB=WB: e.matmul(pv[:], XT[:, kk, t * 128:(t + 1) * 128], WB[:, kk, :], start=(kk == 0), stop=(kk == 7)),
                          reads=[kWB, ("XT", t)], writes=[("ps", 4 + t % 2)])
                S.add("act", lambda e, pv=pv, t=t: e.activation(out=V[:, t, :], in_=pv[:], func=AF.Copy), reads=[("ps", 4 + t % 2)], writes=[("V", t)])
            S.barrier()
            self.aptr = mark
            PT = self.alloc([NT, 256], BF16)
            RB_ = [self.alloc([512]) for _ in range(2)]
            SG = [self.alloc([512]) for _ in range(2)]
            Y = [self.alloc([512], BF16) for _ in range(2)]
            YT = [self.alloc([4, 128], BF16) for _ in range(2)]
            ST = [self.alloc([16]) for _ in range(2)]
            sc = 0
            for c in range(8):
                q0 = 256 * c
                nk = 2 * c + 2
                for ks in range(nk):
                    pi = sc % 3
                    sc += 1
                    pss = PS[pi]
                    for a in range(2):
                        S.add("pe", lambda e, pss=pss, a=a, ks=ks, q0=q0: e.matmul(pss[:, 0:256], KT[:, a, ks * 128:(ks + 1) * 128], QT[:, a, q0:q0 + 256], start=(a == 0), stop=(a == 1)),
                              reads=[], writes=[("ps", pi)])
                    if ks >= 2 * c:
                        S.add("dve", lambda e, pss=pss, ks=ks, c=c, h=h: e.tensor_tensor(out=PT[:, ks, :], in0=pss[:, 0:256], in1=MASK[:, h, 1 + ks - 2 * c, :], op=ALU.mult),
                              reads=[("ps", pi), kM], writes=[("PT", ks)])
                    else:
                        off = q0 - 128 * ks
                        gv = float(gam[h] ** off)
                        S.add("dve", lambda e, pss=pss, ks=ks, gv=gv, h=h: e.scalar_tensor_tensor(out=PT[:, ks, :], in0=pss[:, 0:256], scalar=gv, in1=MASK[:, h, 0, :], op0=ALU.mult, op1=ALU.mult),
                              reads=[("ps", pi), kM], writes=[("PT", ks)])
                for qi in range(2):
                    qt = 2 * c + qi
                    b = qt % 2
                    po, pg, ptr_, px0, px1 = PS[3], PS[4], PSB[5], PS[6], PS[7]
                    for ks in range(qt + 1):
                        S.add("pe", lambda e, po=po, ks=ks, qi=qi, qt=qt: e.matmul(po[:], PT[:, ks, qi * 128:(qi + 1) * 128], V[:, ks, :], start=(ks == 0), stop=(ks == qt)),
                              reads=[("PT", ks), ("V", ks)], writes=[("ps", 3)])
                    for kk in range(NCH):
                        S.add("pe", lambda e, pg=pg, kk=kk, qt=qt: e.matmul(pg[:], XT[:, kk, qt * 128:(qt + 1) * 128], WC[:, kk, :], start=(kk == 0), stop=(kk == 7)),
                              reads=[kWC, ("XT", qt)], writes=[("ps", 4)])
                    st, rb, sg, y, yt = ST[b], RB_[b], SG[b], Y[b], YT[b]
                    ks_ = ("gn", b)
                    S.add("dve", lambda e, st=st, po=po: e.bn_stats(out=st[:, 0:6], in_=po[:]), reads=[("ps", 3)], writes=[(ks_, 0)])
                    S.add("dve", lambda e, st=st: e.bn_aggr(out=st[:, 6:8], in_=st[:, 0:6]), reads=[(ks_, 0)], writes=[(ks_, 1)])
                    S.add("dve", lambda e, st=st: e.tensor_scalar(out=st[:, 8:9], in0=st[:, 7:8], scalar1=float(GN_EPS), scalar2=None, op0=ALU.add), reads=[(ks_, 1)], writes=[(ks_, 2)])
                    S.add("act", lambda e, st=st: e.activation(out=st[:, 8:9], in_=st[:, 8:9], func=AF.Sqrt), reads=[(ks_, 2)], writes=[(ks_, 2)])
                    S.add("dve", lambda e, st=st: e.reciprocal(out=st[:, 9:10], in_=st[:, 8:9]), reads=[(ks_, 2)], writes=[(ks_, 3)])
                    S.add("dve", lambda e, st=st, po=po, rb=rb: e.tensor_scalar(out=rb, in0=po[:], scalar1=st[:, 6:7], scalar2=st[:, 9:10], op0=ALU.subtract, op1=ALU.mult),
                          reads=[("ps", 3), (ks_, 1), (ks_, 3)], writes=[("RB", b)])
                    S.add("act", lambda e, sg=sg, pg=pg: e.activation(out=sg, in_=pg[:], func=AF.Silu), reads=[("ps", 4)], writes=[("SGr", b)])
                    S.add("pool", lambda e, y=y, rb=rb, sg=sg: e.tensor_tensor(out=y, in0=rb, in1=sg, op=ALU.mult), reads=[("RB", b), ("SGr", b)], writes=[("Y", b)])
                    for a in range(4):
                        S.add("pe", lambda e, ptr_=ptr_, a=a, y=y: e.transpose(ptr_[:, a * 128:(a + 1) * 128], y[:, a * 128:(a + 1) * 128], self.IDB[:]),
                              reads=[("Y", b), "IDB"], writes=[("ps", 5)])
                    S.add("act", lambda e, ptr_=ptr_, yt=yt: e.activation(out=yt, in_=ptr_[:, 0:512].rearrange("p (a b) -> p a b", a=4), func=AF.Copy), reads=[("ps", 5)], writes=[("YT", b)])
                    for hh, px in ((0, px0), (1, px1)):
                        for a in range(4):
                            S.add("pe", lambda e, px=px, a=a, yt=yt, hh=hh: e.matmul(px[:], yt[:, a, :], WO[:, a, hh * 512:(hh + 1) * 512], start=(a == 0), stop=(a == 3)),
                                  reads=[("YT", b), kWO], writes=[("ps", 6 + hh)])
                        S.add("dve", lambda e, px=px, qt=qt, hh=hh: e.tensor_tensor(out=X[:, qt, hh * 512:(hh + 1) * 512], in0=px[:], in1=X[:, qt, hh * 512:(hh + 1) * 512], op=ALU.add),
                              reads=[("ps", 6 + hh), ("X", qt)], writes=[("X", qt)])
        S.barrier()
        self.arena_reset()

    def stickbreak(self, l):
        S, d, X, XT, PS = self.S, self.d, self.X, self.XT, self.PS
        jl = l // 2
        w_in = d["sb_w_in"][jl].rearrange("(c p) f -> p c f", p=128)
        w_out = d["sb_w_out"][jl]
        PSB = [p[:].bitcast(BF16) for p in PS]
        NSET = 3
        SBMf = self.alloc([128])
        SBM = self.alloc([128], BF16)
        kSBM = self.key("sbm")
        S.add("sp", lambda e: e.dma_start(out=SBMf, in_=d["sbmask"]), writes=[kSBM], dma="sbm")
        S.add("dve", lambda e: e.tensor_copy(out=SBM, in_=SBMf), reads=[kSBM], writes=[kSBM])
        WQ = self.alloc([NCH, 128], BF16)
        WK = self.alloc([NCH, 128], BF16)
        WV = self.alloc([NCH, 128], BF16)
        WOp = self.alloc([D], BF16)
        QT = self.alloc([S_TOK], BF16)
        KT = self.alloc([S_TOK], BF16)
        V = self.alloc([NT, 128], BF16)
        E1 = [self.alloc([S_TOK]) for _ in range(NSET)]
        SP = [self.alloc([S_TOK]) for _ in range(NSET)]
        EX = [self.alloc([S_TOK]) for _ in range(NSET)]
        AT = [self.alloc([NT, 128], BF16) for _ in range(NSET)]
        SM = [self.alloc([8]) for _ in range(NSET)]
        OS = [self.alloc([128], BF16) for _ in range(2)]
        OT = [self.alloc([128], BF16) for _ in range(2)]
        for s_ in range(NSET):
            S.add("pool", lambda e, s_=s_: e.memset(EX[s_][:, 0:1], 0.0), writes=[("EX", s_)])

        def load_w(m):
            S.add("pool", lambda e, m=m: e.dma_start(out=WQ, in_=w_in[:, :, m * 128:(m + 1) * 128]), writes=["WQ"], dma="swq")
            S.add("pool", lambda e, m=m: e.dma_start(out=WK, in_=w_in[:, :, 1024 + m * 128:1024 + (m + 1) * 128]), writes=["WK"], dma="swk")
            S.add("pool", lambda e, m=m: e.dma_start(out=WV, in_=w_in[:, :, 2048 + m * 128:2048 + (m + 1) * 128]), writes=["WV"], dma="swv")

        self._zc = 0

        def stA(u):
            m, qt, hh, sb_ = u
            n = 128 * (qt + 1)
            e1, sp, sm = E1[sb_], SP[sb_], SM[sb_]
            hp = slice(64 * hh, 64 * hh + 64)
            nkb = (n + 511) // 512
            for kb in range(nkb):
                w = min(512, n - 512 * kb)
                zb = self._zc % 2
                self._zc += 1
                pz = PS[zb]
                last = (kb == nkb - 1)
                S.add("pe", lambda e, pz=pz, w=w, kb=kb, hp=hp, qt=qt, last=last: e.matmul(pz[:, 0:w], QT[hp, qt * 128:(qt + 1) * 128], KT[hp, 512 * kb:512 * kb + w], start=True, stop=(not last)),
                      reads=[("QTKT", 0, qt // 4), ("QTKT", 1, kb)], writes=[("ps", zb)])
                if last:
                    S.add("pe", lambda e, pz=pz, w=w: e.matmul(pz[:, w - 128:w], self.IDB[:], SBM, start=False, stop=True),
                          reads=["IDB", kSBM], writes=[("ps", zb)])
                S.add("act", lambda e, pz=pz, e1=e1, kb=kb, w=w: e.activation(out=e1[:, 512 * kb:512 * kb + w], in_=pz[:, 0:w], func=AF.Exp),
                      reads=[("ps", zb)], writes=[("E1", sb_)])
            S.add("act", lambda e, e1=e1, sp=sp, n=n, sm=sm: e.activation(out=sp[:, 0:n], in_=e1[:, 0:n], func=AF.Ln, bias=1.0, scale=1.0, accum_out=sm[:, 0:1]),
                  reads=[("E1", sb_)], writes=[("SP", sb_), ("TT", sb_)])

        def stB1(u):
            m, qt, hh, sb_ = u
            n = 128 * (qt + 1)
            sp, ex, sm = SP[sb_], EX[sb_], SM[sb_]
            S.add("dve", lambda e, sm=sm: e.tensor_scalar(out=sm[:, 1:2], in0=sm[:, 0:1], scalar1=-1.0, scalar2=None, op0=ALU.mult), reads=[("TT", sb_)], writes=[("NTT", sb_)])
            S.add("dve", lambda e, sp=sp, ex=ex, n=n: e.tensor_tensor_scan(out=ex[:, 1:n], data0=sp[:, 0:n - 1], data1=sp[:, 0:n - 1], initial=0.0, op0=ALU.add, op1=ALU.max),
                  reads=[("SP", sb_)], writes=[("EX", sb_)])

        def stB2(u):
            m, qt, hh, sb_ = u
            n = 128 * (qt + 1)
            e1, sp, ex, sm = E1[sb_], SP[sb_], EX[sb_], SM[sb_]
            a_ = ex.bitcast(BF16)[:, S_TOK:2 * S_TOK]
            S.add("act", lambda e, ex=ex, sp=sp, n=n, sm=sm: e.activation(out=sp[:, 0:n], in_=ex[:, 0:n], func=AF.Exp, bias=sm[:, 1:2], scale=1.0),
                  reads=[("EX", sb_), ("NTT", sb_)], writes=[("SP", sb_)])
            S.add("dve", lambda e, e1=e1, sp=sp, a_=a_, n=n: e.tensor_tensor(out=a_[:, 0:n], in0=e1[:, 0:n], in1=sp[:, 0:n], op=ALU.mult),
                  reads=[("E1", sb_), ("SP", sb_)], writes=[("EX", sb_), ("A", sb_)])

        def stC1(u):
            m, qt, hh, sb_ = u
            ex, at = EX[sb_], AT[sb_]
            a_ = ex.bitcast(BF16)[:, S_TOK:2 * S_TOK]
            for g in range((qt + 8) // 8):
                nb = min(8, qt + 1 - 8 * g)
                ptb = PSB[2 + g % 2]
                for i in range(nb):
                    kb2 = 8 * g + i
                    S.add("pe", lambda e, ptb=ptb, i=i, kb2=kb2, a_=a_: e.transpose(ptb[:, i * 128:(i + 1) * 128], a_[:, kb2 * 128:(kb2 + 1) * 128], self.IDB[:]),
                          reads=[("A", sb_), "IDB"], writes=[("ps", 2 + g % 2)])

        def stC1b(u):
            m, qt, hh, sb_ = u
            at = AT[sb_]
            for g in range((qt + 8) // 8):
                nb = min(8, qt + 1 - 8 * g)
                ptb = PSB[2 + g % 2]
                S.add("dve", lambda e, ptb=ptb, at=at, g=g, nb=nb: e.tensor_copy(out=at[:, 8 * g:8 * g + nb, :], in_=ptb[:, 0:nb * 128].rearrange("p (a b) -> p a b", a=nb)),
                      reads=[("ps", 2 + g % 2)], writes=[("AT", sb_, g)])

        def stC2(u):
            m, qt, hh, sb_ = u
            at = AT[sb_]
            ob = qt % 2
            hp = slice(64 * hh, 64 * hh + 64)
            po = PS[4 + hh]
            for kb2 in range(qt + 1):
                S.add("pe", lambda e, po=po, kb2=kb2, at=at, hp=hp, qt=qt: e.matmul(po[:, 0:64], at[:, kb2, :], V[:, kb2, hp], start=(kb2 == 0), stop=(kb2 == qt)),
                      reads=[("AT", sb_, kb2 // 8), ("V", kb2)], writes=[("ps", 4 + hh)])
            S.add("dve", lambda e, po=po, hp=hp, ob=ob: e.tensor_copy(out=OS[ob][:, hp], in_=po[:, 0:64]), reads=[("ps", 4 + hh)], writes=[("OS", ob, hh)])
            if hh == 1:
                ptb = PSB[6]
                S.add("pe", lambda e, ptb=ptb, ob=ob: e.transpose(ptb[:, 0:128], OS[ob], self.IDB[:]), reads=[("OS", ob, 0), ("OS", ob, 1), "IDB"], writes=[("ps", 6)])
                S.add("dve", lambda e, ptb=ptb, ob=ob: e.tensor_copy(out=OT[ob], in_=ptb[:, 0:128]), reads=[("ps", 6)], writes=[("OT", ob)])
                for hf in range(2):
                    px = PS[6 + hf]
                    S.add("pe", lambda e, px=px, ob=ob, hf=hf: e.matmul(px[:], OT[ob], WOp[:, hf * 512:(hf + 1) * 512], start=True, stop=True),
                          reads=[("OT", ob), "WOp"], writes=[("ps", 6 + hf)])
                    S.add("dve", lambda e, px=px, qt=qt, hf=hf: e.tensor_tensor(out=X[:, qt, hf * 512:(hf + 1) * 512], in0=px[:], in1=X[:, qt, hf * 512:(hf + 1) * 512], op=ALU.add),
                          reads=[("ps", 6 + hf), ("X", qt)], writes=[("X", qt)])

        ucount = 0
        for m in range(8):
            load_w(m)
            for c in range(4):
                tok = slice(c * 512, (c + 1) * 512)
                for which, W_, DST, scl in ((0, WQ, QT, 0.125), (1, WK, KT, 1.0)):
                    pp = PS[which]
                    for kk in range(NCH):
                        S.add("pe", lambda e, pp=pp, kk=kk, W_=W_, tok=tok: e.matmul(pp[:], W_[:, kk, :], XT[:, kk, tok], start=(kk == 0), stop=(kk == 7)),
                              reads=["WQ" if which == 0 else "WK"] + [("XT", tt) for tt in range(4 * c, 4 * c + 4)], writes=[("ps", which)])
                    S.add("act", lambda e, pp=pp, DST=DST, tok=tok, scl=scl: e.activation(out=DST[:, tok], in_=pp[:], func=AF.Copy, scale=float(scl)),
                          reads=[("ps", which)], writes=[("QTKT", which, c)])
            for t in range(NT):
                pv = PS[2 + t % 2]
                for kk in range(NCH):
                    S.add("pe", lambda e, pv=pv, kk=kk, t=t: e.matmul(pv[:, 0:128], XT[:, kk, t * 128:(t + 1) * 128], WV[:, kk, :], start=(kk == 0), stop=(kk == 7)),
                          reads=["WV", ("XT", t)], writes=[("ps", 2 + t % 2)])
                S.add("dve", lambda e, pv=pv, t=t: e.tensor_copy(out=V[:, t, :], in_=pv[:, 0:128]), reads=[("ps", 2 + t % 2)], writes=[("V", t)])
            S.add("pool", lambda e, m=m: e.dma_start(out=WOp, in_=w_out[m * 128:(m + 1) * 128, :]), writes=["WOp"], dma="swo")
            units = []
            for qt in range(NT):
                for hh in range(2):
                    units.append((m, qt, hh, ucount % NSET))
                    ucount += 1
            nu = len(units)
            for step in range(nu + 2):
                if step < nu:
                    stA(units[step])
                if 0 <= step - 1 < nu:
                    stB1(units[step - 1])
                if 0 <= step - 2 < nu:
                    stC1(units[step - 2])
                    stC1b(units[step - 2])
                    stC2(units[step - 2])
                if 0 <= step - 1 < nu:
                    stB2(units[step - 1])
        S.barrier()
        self.arena_reset()


def _host_inputs(inp, b, consts, big=True):
    f = lambda a: np.ascontiguousarray(np.asarray(a, dtype=np.float32))
    m = {}
    m["x"] = f(inp["x"][b])
    m["pT"] = f(np.transpose(np.asarray(inp["p"])[:, b], (0, 2, 1)))
    for k in ("ret_w_in", "ret_w_out", "sb_w_in", "sb_w_out", "router_w", "router_b", "w_gate_up", "w_down",
              "b_down", "ple_w", "ple_gate_w", "ple_gate_b"):
        if not big and k in ("w_gate_up", "w_down"):
            m[k] = f(np.asarray(inp[k])[0:1, 0:1])
        else:
            m[k] = f(inp[k])
    m["lnp"] = f(np.stack([inp["ln1_g"], inp["ln1_b"], inp["ln2_g"], inp["ln2_b"]], axis=1))
    bgu = np.asarray(inp["b_gate_up"], dtype=np.float32).reshape(DEPTH, NE, 8, 128, 2)
    m["bgu"] = f(np.transpose(bgu, (0, 3, 1, 2, 4)).reshape(DEPTH, 128, NE * 16))
    for k in ("identf", "identb", "ones", "ut", "iota", "onesb", "cos", "sin", "rmask", "sbmask"):
        m[k] = consts[k]
    return m


def run_layers(inp, layers, n_exp=NE, stages=("mix", "moe", "ple"), cores=8):
    bld = Builder(layers, n_exp=n_exp, stages=stages)
    nc = bld.build()
    in_maps = [_host_inputs(inp, b, bld.consts, big=("moe" in stages)) for b in range(cores)]
    res = run_bass_kernel_spmd(nc, in_maps, core_ids=list(range(cores)))
    return np.stack([res.results[b]["y"] for b in range(cores)], axis=0)


def kernel(**inputs):
    out = run_layers(inputs, layers=range(DEPTH))
    return out.astype(np.float32)
```

```python
import math
from contextlib import ExitStack

import ml_dtypes
import numpy as np

import concourse.bass as bass
import concourse.mybir as mybir
from concourse.bass_utils import run_bass_kernel_spmd

F32 = mybir.dt.float32
BF16 = mybir.dt.bfloat16
ALU = mybir.AluOpType
AF = mybir.ActivationFunctionType
AX = mybir.AxisListType

S_TOK = 2048
D = 1024
NT = 16
NCH = 8
DEPTH = 4
NE = 32
DN_ALPHA = float((2 * DEPTH) ** 0.25)
LN_EPS = 1e-5
GN_EPS = 1e-6
SEG = 30000
GRP = 4


class _Op:
    __slots__ = ("eng", "fn", "deps", "dma", "sig", "sigval", "idx")


class Sched:
    ENGS = ("pe", "act", "dve", "pool", "sp")

    def __init__(self, nc):
        self.nc = nc
        self.streams = {e: [] for e in self.ENGS}
        self.last_w = {}
        self.readers = {}
        self.dma_cnt = {}
        self.dma_last = {}
        self.barrier_ops = []
        self.barrier_seen = {e: True for e in self.ENGS}

    def barrier(self):
        ops = []
        for e in self.ENGS:
            for op in reversed(self.streams[e]):
                if op.dma is None:
                    ops.append(op)
                    break
        ops.extend(self.dma_last.values())
        self.barrier_ops = ops
        self.barrier_seen = {e: False for e in self.ENGS}
        self.last_w = {}
        self.readers = {}

    def add(self, eng, fn, reads=(), writes=(), dma=None):
        op = _Op()
        op.eng, op.fn, op.dma = eng, fn, None
        op.sig = False
        op.sigval = None
        ps_r = [k for k in reads if isinstance(k, tuple) and k[0] == "ps"]
        if ps_r:
            reads = [k for k in reads if not (isinstance(k, tuple) and k[0] == "ps")]
            writes = list(writes) + ps_r
        deps = {}
        for k in reads:
            w = self.last_w.get(k)
            if w is not None:
                deps[w] = True
        for k in writes:
            w = self.last_w.get(k)
            if w is not None:
                deps[w] = True
            for r in self.readers.get(k, ()):
                if r not in deps:
                    deps[r] = False
        if not self.barrier_seen[eng]:
            self.barrier_seen[eng] = True
            for b in self.barrier_ops:
                deps[b] = True
        for k in reads:
            self.readers.setdefault(k, []).append(op)
        for k in writes:
            self.last_w[k] = op
            self.readers[k] = []
        pruned = []
        latest = {}
        for d, strong in deps.items():
            if d is op:
                continue
            if d.dma is not None:
                pruned.append(d)
                continue
            if d.eng == eng and (eng == "pe" or not strong):
                continue
            o = latest.get(d.eng)
            if o is None or o.idx < d.idx:
                latest[d.eng] = d
        pruned.extend(latest.values())
        op.deps = pruned
        op.idx = len(self.streams[eng])
        if dma is not None:
            c = self.dma_cnt.get(dma, 0) + 1
            self.dma_cnt[dma] = c
            op.dma = (dma, 16 * c)
            self.dma_last[dma] = op
        self.streams[eng].append(op)
        return op

    def emit(self):
        nc = self.nc
        for e in self.ENGS:
            for op in self.streams[e]:
                for d in op.deps:
                    if d.dma is None:
                        d.sig = True
        nsegs = {}
        for e in self.ENGS:
            c = 0
            for op in self.streams[e]:
                if op.sig and op.dma is None:
                    op.sigval = (e, c // SEG, c % SEG + 1)
                    c += 1
            nsegs[e] = (c + SEG - 1) // SEG
        with ExitStack() as es:
            sems = {}
            for e in self.ENGS:
                for s in range(nsegs[e]):
                    sems[(e, s)] = es.enter_context(nc.semaphore(f"s_{e}_{s}"))
            dsems = {}
            for g in self.dma_cnt:
                dsems[g] = es.enter_context(nc.semaphore(f"d_{g}"))
            self.n_sems = len(sems) + len(dsems)
            block = es.enter_context(nc.Block())

            def run(ename, eng):
                waited = {}
                for op in self.streams[ename]:
                    need = {}
                    for d in op.deps:
                        if d.dma is not None:
                            key, val = ("d", d.dma[0]), d.dma[1]
                        else:
                            key, val = (d.sigval[0], d.sigval[1]), d.sigval[2]
                        if need.get(key, 0) < val:
                            need[key] = val
                    for key, val in need.items():
                        if waited.get(key, 0) >= val:
                            continue
                        waited[key] = val
                        sem = dsems[key[1]] if key[0] == "d" else sems[key]
                        eng.wait_ge(sem, val)
                    ins = op.fn(eng)
                    if op.dma is not None:
                        ins.then_inc(dsems[op.dma[0]], 16)
                    elif op.sig:
                        ins.then_inc(sems[(op.sigval[0], op.sigval[1])], 1)

            block.tensor(lambda eng: run("pe", eng))
            block.scalar(lambda eng: run("act", eng))
            block.vector(lambda eng: run("dve", eng))
            block.gpsimd(lambda eng: run("pool", eng))
            block.sync(lambda eng: run("sp", eng))


def _consts():
    c = {}
    c["identf"] = np.eye(128, dtype=np.float32)
    c["identb"] = np.eye(128, dtype=np.float32).astype(ml_dtypes.bfloat16)
    c["ones"] = np.ones((1, 128), dtype=np.float32)
    ii = np.arange(128)
    c["ut"] = (ii[:, None] < ii[None, :]).astype(np.float32).astype(ml_dtypes.bfloat16)
    c["iota"] = np.ascontiguousarray(np.broadcast_to(ii[None, :], (128, 128))).astype(np.float32)
    c["onesb"] = np.ones((128, 128), dtype=np.float32).astype(ml_dtypes.bfloat16)
    half = 128
    inv_freq = (1.0 / (10000.0 ** (np.arange(half, dtype=np.float32) / np.float32(half)))).astype(np.float32)
    ang = (np.arange(S_TOK, dtype=np.float32)[None, :] * inv_freq[:, None]).astype(np.float32)
    c["cos"] = np.cos(ang).astype(np.float32)
    c["sin"] = np.sin(ang).astype(np.float32)
    H = 4
    lg = np.log(1.0 - 2.0 ** (-5.0 - np.arange(H, dtype=np.float64)))
    sl = np.arange(128)[:, None].astype(np.float64)
    tl = np.arange(256)[None, :].astype(np.float64)
    masks = np.zeros((H, 3, 128, 256), dtype=np.float32)
    for h in range(H):
        masks[h, 0] = np.exp(lg[h] * (tl - sl)) / 16.0
        for m in range(2):
            s_abs = 128 * m + sl
            dec = np.exp(lg[h] * np.abs(tl - s_abs))
            ok = (np.floor(s_abs / 64) <= np.floor(tl / 64))
            masks[h, 1 + m] = dec * ok / 16.0
    c["rmask"] = np.ascontiguousarray(masks.transpose(2, 0, 1, 3))
    c["rgam"] = np.exp(lg)
    t = np.arange(128)[:, None]
    s = np.arange(128)[None, :]
    c["sbmask"] = np.where(s < t, 0.0, -30000.0).astype(np.float32)
    return c


class Builder:
    def __init__(self, layers, n_exp=NE, stages=("mix", "moe", "ple")):
        self.layers = list(layers)
        self.n_exp = n_exp
        self.stages = stages
        self.nc = bass.Bass("TRN2", target_bir_lowering=False)
        self.consts = _consts()

    def dram_in(self, name, shape, dt=F32):
        return self.nc.dram_tensor(name, list(shape), dt, kind="ExternalInput").ap()

    def build(self):
        nc = self.nc
        L = len(self.layers)
        d = {}
        d["x"] = self.dram_in("x", [S_TOK, D])
        d["pT"] = self.dram_in("pT", [DEPTH, 256, S_TOK])
        d["ret_w_in"] = self.dram_in("ret_w_in", [2, D, 6144])
        d["ret_w_out"] = self.dram_in("ret_w_out", [2, 2048, D])
        d["sb_w_in"] = self.dram_in("sb_w_in", [2, D, 3 * D])
        d["sb_w_out"] = self.dram_in("sb_w_out", [2, D, D])
        d["lnp"] = self.dram_in("lnp", [DEPTH, 4, D])
        d["router_w"] = self.dram_in("router_w", [DEPTH, D, NE])
        d["router_b"] = self.dram_in("router_b", [DEPTH, NE])
        big = "moe" in self.stages
        d["w_gate_up"] = self.dram_in("w_gate_up", [DEPTH, NE, D, 2 * D] if big else [1, 1, D, 2 * D])
        d["bgu"] = self.dram_in("bgu", [DEPTH, 128, NE * 16])
        d["w_down"] = self.dram_in("w_down", [DEPTH, NE, D, D] if big else [1, 1, D, D])
        d["b_down"] = self.dram_in("b_down", [DEPTH, NE, D])
        d["ple_w"] = self.dram_in("ple_w", [DEPTH, 256, D])
        d["ple_gate_w"] = self.dram_in("ple_gate_w", [DEPTH, D, D])
        d["ple_gate_b"] = self.dram_in("ple_gate_b", [DEPTH, D])
        d["identf"] = self.dram_in("identf", [128, 128])
        d["identb"] = self.dram_in("identb", [128, 128], BF16)
        d["ones"] = self.dram_in("ones", [1, 128])
        d["ut"] = self.dram_in("ut", [128, 128], BF16)
        d["iota"] = self.dram_in("iota", [128, 128])
        d["onesb"] = self.dram_in("onesb", [128, 128], BF16)
        d["cos"] = self.dram_in("cos", [128, S_TOK])
        d["sin"] = self.dram_in("sin", [128, S_TOK])
        d["rmask"] = self.dram_in("rmask", [128, 4, 3, 256])
        d["sbmask"] = self.dram_in("sbmask", [128, 128])
        d["y"] = nc.dram_tensor("y", [S_TOK, D], F32, kind="ExternalOutput").ap()
        self.d = d

        AW = 27648
        with ExitStack() as es:
            sb = lambda n, s, dt: es.enter_context(nc.sbuf_tensor(n, s, dt))
            self.X = sb("X", [128, NT, D], F32)
            self.XT = sb("XT", [128, NCH, S_TOK], BF16)
            self.IDF = sb("IDF", [128, 128], F32)
            self.IDB = sb("IDB", [128, 128], BF16)
            self.ONES = sb("ONES", [1, 128], F32)
            self.GALL = sb("GALL", [128, NT, NE], F32)
            self.ARENA = sb("ARENA", [128, AW], F32)
            self.AW = AW
            self.PS = [es.enter_context(nc.psum_tensor(f"ps{i}", [128, 512], F32)) for i in range(8)]
            self.S = Sched(nc)
            self.uid = 0
            self.program()
            self.S.emit()
        return nc

    def arena_reset(self):
        self.aptr = 0

    def alloc(self, shape, dt=F32, parts=128):
        n = int(np.prod(shape))
        words = n if dt == F32 else (n + 1) // 2
        words = (words + 7) // 8 * 8
        a = self.ARENA[0:parts, self.aptr:self.aptr + words]
        self.aptr += words
        assert self.aptr <= self.AW, f"arena overflow {self.aptr} > {self.AW}"
        if dt != F32:
            a = a.bitcast(dt)[:, 0:n]
        else:
            a = a[:, 0:n]
        if len(shape) == 2:
            a = a.rearrange("p (a b) -> p a b", a=shape[0])
        elif len(shape) == 3:
            a = a.rearrange("p (a b c) -> p a b c", a=shape[0], b=shape[1])
        return a

    def key(self, base):
        self.uid += 1
        return (base, self.uid)

    def program(self):
        S, d = self.S, self.d
        X = self.X
        S.add("sp", lambda e: e.dma_start(out=self.IDF[:], in_=d["identf"]), writes=["IDF"], dma="c0")
        S.add("sp", lambda e: e.dma_start(out=self.IDB[:], in_=d["identb"]), writes=["IDB"], dma="c1")
        S.add("sp", lambda e: e.dma_start(out=self.ONES[:], in_=d["ones"]), writes=["ONES"], dma="c2")
        xin = d["x"].rearrange("(t p) f -> p t f", p=128)
        for q in range(4):
            S.add("sp", lambda e, q=q: e.dma_start(out=X[:, 4 * q:4 * q + 4, :], in_=xin[:, 4 * q:4 * q + 4, :]),
                  writes=[("X", t) for t in range(4 * q, 4 * q + 4)], dma=f"xin{q}")
        for l in self.layers:
            if "mix" in self.stages:
                self.arena_reset()
                S.barrier()
                self.make_xt(l, scale=DN_ALPHA, router=False)
                if l % 2 == 0:
                    self.retention(l)
                else:
                    self.stickbreak(l)
                self.layernorm(l, 0)
            if "moe" in self.stages:
                self.arena_reset()
                S.barrier()
                self.moe(l)
                self.layernorm(l, 1)
            if "ple" in self.stages:
                self.arena_reset()
                S.barrier()
                self.ple(l)
        yout = d["y"].rearrange("(t p) f -> p t f", p=128)
        for q in range(4):
            S.add("sp", lambda e, q=q: e.dma_start(out=yout[:, 4 * q:4 * q + 4, :], in_=X[:, 4 * q:4 * q + 4, :]),
                  reads=[("X", t) for t in range(4 * q, 4 * q + 4)], writes=[("yout", q)], dma=f"yout{q}")
        S.add("sp", lambda e: e.nop(), reads=[("yout", q) for q in range(4)])

    def make_xt(self, l, scale=None, router=False):
        S, d, X, XT, PS = self.S, self.d, self.X, self.XT, self.PS
        if router:
            RW = self.alloc([NCH, NE])
            RB = self.alloc([NE], parts=1)
            XT32 = [self.alloc([NCH, 128]) for _ in range(2)]
            LG = [self.alloc([NE]) for _ in range(2)]
            EXm = [self.alloc([NE]) for _ in range(2)]
            MSK = [self.alloc([NE]) for _ in range(2)]
            SM = [self.alloc([16]) for _ in range(2)]
            UT = self.alloc([128], BF16)
            kUT = self.key("UT")
            S.add("sp", lambda e: e.dma_start(out=UT, in_=d["ut"]), writes=[kUT], dma="ut")
            ONB = self.alloc([128], BF16)
            kONB = self.key("ONB")
            S.add("sp", lambda e: e.dma_start(out=ONB, in_=d["onesb"]), writes=[kONB], dma="onb")
            XB = self.XB
            kRW, kRB = self.key("RW"), self.key("RB")
            S.add("sp", lambda e: e.dma_start(out=RW, in_=d["router_w"][l].rearrange("(c p) n -> p c n", p=128)),
                  writes=[kRW], dma="rw")
            S.add("sp", lambda e: e.dma_start(out=RB, in_=d["router_b"][l:l + 1, :]), writes=[kRB], dma="rb")
        for t in range(NT):
            b = t % 2
            for h in range(2):
                pb = PS[2 * b + h]
                for j in range(4):
                    c = 4 * h + j
                    S.add("pe", lambda e, pb=pb, j=j, c=c, t=t: e.transpose(pb[:, j * 128:(j + 1) * 128], X[:, t, c * 128:(c + 1) * 128], self.IDF[:]),
                          reads=[("X", t), "IDF"], writes=[("ps", 2 * b + h)])
                if not router:
                    S.add("act", lambda e, pb=pb, h=h, t=t: e.activation(out=XT[:, 4 * h:4 * h + 4, t * 128:(t + 1) * 128],
                                                                          in_=pb[:].rearrange("p (a b) -> p a b", a=4), func=AF.Copy),
                          reads=[("ps", 2 * b + h)], writes=[("XT", t)])
                if router:
                    S.add("dve", lambda e, pb=pb, h=h, b=b: e.tensor_copy(out=XT32[b][:, 4 * h:4 * h + 4, :],
                                                                           in_=pb[:].rearrange("p (a b) -> p a b", a=4)),
                          reads=[("ps", 2 * b + h)], writes=[("XT32", b, h)])
            if router:
                S.add("act", lambda e, t=t: e.activation(out=XB[:, t, :], in_=X[:, t, :], func=AF.Copy), reads=[("X", t)], writes=[("XB", t)])
            if scale is not None:
                S.add("pool", lambda e, t=t: e.tensor_scalar(out=X[:, t, :], in0=X[:, t, :], scalar1=float(scale), scalar2=None, op0=ALU.mult),
                      reads=[("X", t)], writes=[("X", t)])
            if router:
                pl = PS[4 + b]
                for c in range(NCH):
                    S.add("pe", lambda e, pl=pl, c=c, b=b: e.matmul(pl[:, 0:NE], XT32[b][:, c, :], RW[:, c, :], start=(c == 0), stop=False),
                          reads=[("XT32", b, c // 4), kRW], writes=[("ps", 4 + b)])
                S.add("pe", lambda e, pl=pl: e.matmul(pl[:, 0:NE], self.ONES[0:1, :], RB[0:1, :], start=False, stop=True),
                      reads=["ONES", kRB], writes=[("ps", 4 + b)])
                lg, ex, mk, sm = LG[b], EXm[b], MSK[b], SM[b]
                kl = ("rt", b)
                S.add("dve", lambda e, pl=pl, lg=lg: e.tensor_copy(out=lg, in_=pl[:, 0:NE]), reads=[("ps", 4 + b)], writes=[(kl, "lg")])
                S.add("dve", lambda e, lg=lg, sm=sm: e.max(out=sm[:, 0:8], in_=lg), reads=[(kl, "lg")], writes=[(kl, "top")])
                S.add("dve", lambda e, lg=lg, sm=sm, mk=mk: e.tensor_scalar(out=mk, in0=lg, scalar1=sm[:, 3:4], scalar2=None, op0=ALU.is_ge),
                      reads=[(kl, "lg"), (kl, "top")], writes=[(kl, "mk")])
                S.add("dve", lambda e, sm=sm: e.tensor_scalar(out=sm[:, 8:9], in0=sm[:, 0:1], scalar1=-1.0, scalar2=None, op0=ALU.mult),
                      reads=[(kl, "top")], writes=[(kl, "nm")])
                S.add("dve", lambda e, mk=mk, t=t: e.tensor_copy(out=self.MB[:, t, :], in_=mk), reads=[(kl, "mk")], writes=[("MB", t)])
                gi = t % GRP
                S.add("pe", lambda e, pl=pl, t=t, gi=gi: e.matmul(pl[:, 64:64 + NE], UT, self.MB[:, t, :], start=True, stop=(gi == 0)),
                      reads=[("MB", t), kUT], writes=[("ps", 4 + b)])
                for tp in range(t - gi, t):
                    S.add("pe", lambda e, pl=pl, tp=tp, t=t: e.matmul(pl[:, 64:64 + NE], ONB, self.MB[:, tp, :], start=False, stop=(tp == t - 1)),
                          reads=[("MB", tp), kONB], writes=[("ps", 4 + b)])
                S.add("dve", lambda e, pl=pl, mk=mk, t=t: e.scalar_tensor_tensor(out=self.VAL[:, t, :], in0=pl[:, 64:64 + NE], scalar=127.5, in1=mk, op0=ALU.is_lt, op1=ALU.mult),
                      reads=[("ps", 4 + b), (kl, "mk")], writes=[("VAL", t)])
                S.add("dve", lambda e, pl=pl, t=t: e.tensor_copy(out=self.RK[:, t, :], in_=pl[:, 64:64 + NE]), reads=[("ps", 4 + b)], writes=[("RK", t)])
                S.add("act", lambda e, lg=lg, ex=ex, sm=sm: e.activation(out=ex, in_=lg, func=AF.Exp, bias=sm[:, 8:9], scale=1.0),
                      reads=[(kl, "lg"), (kl, "nm")], writes=[(kl, "ex")])
                S.add("dve", lambda e, ex=ex, mk=mk: e.tensor_tensor(out=ex, in0=ex, in1=mk, op=ALU.mult),
                      reads=[(kl, "ex"), (kl, "mk")], writes=[(kl, "ex")])
                S.add("dve", lambda e, ex=ex, sm=sm: e.reduce_sum(out=sm[:, 9:10], in_=ex, axis=AX.X),
                      reads=[(kl, "ex")], writes=[(kl, "ss")])
                S.add("dve", lambda e, sm=sm: e.reciprocal(out=sm[:, 10:11], in_=sm[:, 9:10]), reads=[(kl, "ss")], writes=[(kl, "rs")])
                S.add("dve", lambda e, ex=ex, sm=sm, t=t: e.tensor_scalar(out=self.GALL[:, t, :], in0=ex, scalar1=sm[:, 10:11], scalar2=None, op0=ALU.mult),
                      reads=[(kl, "ex"), (kl, "rs")], writes=[("G", t)])
                pg = PS[6 + b]
                S.add("pe", lambda e, pg=pg, t=t: e.transpose(pg[0:NE, 0:128], self.GALL[:, t, :], self.IDF[:]),
                      reads=[("G", t), "IDF"], writes=[("ps", 6 + b)])
                S.add("act", lambda e, pg=pg, t=t: e.activation(out=self.GT[0:NE, t * 128:(t + 1) * 128], in_=pg[0:NE, 0:128], func=AF.Copy),
                      reads=[("ps", 6 + b)], writes=[("GT", t)])

    def ln_alloc(self):
        return (self.alloc([D]), self.alloc([D]), [self.alloc([16]) for _ in range(2)])

    def layernorm(self, l, which, bufs=None):
        S, d, X = self.S, self.d, self.X
        G, B, ST = bufs if bufs is not None else self.ln_alloc()
        kG, kB = self.key("lng"), self.key("lnb")
        S.add("sp", lambda e: e.dma_start(out=G, in_=d["lnp"][l, 2 * which:2 * which + 1, :].to_broadcast([128, D])), writes=[kG], dma="lng")
        S.add("sp", lambda e: e.dma_start(out=B, in_=d["lnp"][l, 2 * which + 1:2 * which + 2, :].to_broadcast([128, D])), writes=[kB], dma="lnb")
        for t in range(NT):
            st = ST[t % 2]
            ks = ("lnst", t % 2)
            xt = X[:, t, :]
            S.add("dve", lambda e, st=st, xt=xt: e.bn_stats(out=st[:, 0:6], in_=xt[:, 0:512]), reads=[("X", t)], writes=[(ks, 0)])
            S.add("dve", lambda e, st=st, xt=xt: e.bn_stats(out=st[:, 6:12], in_=xt[:, 512:1024]), reads=[("X", t)], writes=[(ks, 1)])
            S.add("dve", lambda e, st=st: e.bn_aggr(out=st[:, 12:14], in_=st[:, 0:12]),
                  reads=[(ks, 0), (ks, 1)], writes=[(ks, 2)])
            S.add("dve", lambda e, st=st: e.tensor_scalar(out=st[:, 14:15], in0=st[:, 13:14], scalar1=float(LN_EPS), scalar2=None, op0=ALU.add),
                  reads=[(ks, 2)], writes=[(ks, 3)])
            S.add("act", lambda e, st=st: e.activation(out=st[:, 14:15], in_=st[:, 14:15], func=AF.Sqrt), reads=[(ks, 3)], writes=[(ks, 3)])
            S.add("dve", lambda e, st=st: e.reciprocal(out=st[:, 15:16], in_=st[:, 14:15]), reads=[(ks, 3)], writes=[(ks, 4)])
            S.add("dve", lambda e, st=st, xt=xt: e.tensor_scalar(out=xt, in0=xt, scalar1=st[:, 12:13], scalar2=st[:, 15:16], op0=ALU.subtract, op1=ALU.mult),
                  reads=[("X", t), (ks, 2), (ks, 4)], writes=[("X", t)])
            S.add("pool", lambda e, xt=xt: e.tensor_tensor(out=xt, in0=xt, in1=G, op=ALU.mult), reads=[("X", t), kG], writes=[("X", t)])
            S.add("pool", lambda e, xt=xt: e.tensor_tensor(out=xt, in0=xt, in1=B, op=ALU.add), reads=[("X", t), kB], writes=[("X", t)])

    def moe_bias(self, l, GT, BD, BGU):
        S, d, X, PS = self.S, self.d, self.X, self.PS
        kBD, kBGU = self.key("BD"), self.key("BGU")
        self.moe_keys = (kBD, kBGU)
        S.add("sp", lambda e: e.dma_start(out=BD, in_=d["b_down"][l]), writes=[kBD], dma="bd")
        S.add("sp", lambda e: e.dma_start(out=BGU, in_=d["bgu"][l]), writes=[kBGU], dma="bgu")
        for t in range(NT):
            for h in range(2):
                pb = PS[6 + h]
                S.add("pe", lambda e, pb=pb, t=t, h=h: e.matmul(pb[:], GT[0:NE, t * 128:(t + 1) * 128], BD[0:NE, h * 512:(h + 1) * 512], start=True, stop=True),
                      reads=[("GT", t), kBD], writes=[("ps", 6 + h)])
                S.add("dve", lambda e, pb=pb, t=t, h=h: e.tensor_tensor(out=X[:, t, h * 512:(h + 1) * 512], in0=pb[:], in1=X[:, t, h * 512:(h + 1) * 512], op=ALU.add),
                      reads=[("ps", 6 + h), ("X", t)], writes=[("X", t)])

    def moe(self, l):
        S, d, X, PS = self.S, self.d, self.X, self.PS
        PSB = [p[:].bitcast(BF16) for p in PS]
        NQ = GRP
        NST = NT // GRP
        NSL = NST * 128
        NSC = NSL // 512
        self.XB = self.XT[:].rearrange("p c s -> p (c s)").rearrange("p (t f) -> p t f", t=NT)
        XB = self.XB
        BGU = self.alloc([NE * 16])
        self.RK = self.alloc([NT, NE])
        self.VAL = self.alloc([NT, NE])
        self.MB = self.alloc([NT, NE], BF16)
        IOTA = self.alloc([128])
        kIO = self.key("iota")
        S.add("sp", lambda e: e.dma_start(out=IOTA, in_=d["iota"]), writes=[kIO], dma="iota")
        mark = self.aptr
        self.GT = self.alloc([S_TOK], parts=32)
        BD = self.alloc([D], parts=32)
        self.make_xt(l, scale=DN_ALPHA, router=True)
        self.moe_bias(l, self.GT, BD, BGU)
        S.barrier()
        self.aptr = mark
        kBD, kBGU = self.moe_keys
        RK, VAL = self.RK, self.VAL
        XG = self.alloc([NCH, NSL], BF16)
        ACTT = self.alloc([NCH, NSL], BF16)
        YS = self.alloc([NST, D], BF16)
        PA = self.alloc([NT, 128], BF16)
        PTAs = [self.alloc([NT, 128], BF16) for _ in range(2)]
        NSLOT = 6
        WGU = [self.alloc([NCH, 256], BF16) for _ in range(NSLOT)]
        WD = self.alloc([NCH, D], BF16)
        NTMP = 3
        TG = [self.alloc([512]) for _ in range(NTMP)]
        TU = [self.alloc([512]) for _ in range(NTMP)]
        TS = [self.alloc([512]) for _ in range(NTMP)]
        wgu_src = d["w_gate_up"]
        wd_src = d["w_down"]
        n_chunks = self.n_exp * 8

        def dma_wgu(g):
            if g >= n_chunks:
                return
            slot = g % NSLOT
            wsrc = wgu_src[l, g // 8].rearrange("(c p) f -> p c f", p=128)
            j = g % 8
            S.add("pool", lambda e, slot=slot, j=j, wsrc=wsrc: e.dma_start(out=WGU[slot], in_=wsrc[:, :, 256 * j:256 * (j + 1)]),
                  writes=[("WGU", slot)], dma=f"wgu{slot}")

        def dma_wd(ei):
            src = wd_src[l, ei].rearrange("(c p) f -> p c f", p=128)
            for q in range(2):
                S.add("pool", lambda e, q=q, src=src: e.dma_start(out=WD[:, 4 * q:4 * q + 4, :], in_=src[:, 4 * q:4 * q + 4, :]),
                      writes=[("WD", q)], dma=f"wd{q}")

        def build_sel(ei):
            for t in range(NT):
                S.add("dve", lambda e, t=t, ei=ei: e.tensor_scalar(out=PA[:, t, :], in0=IOTA, scalar1=RK[:, t, ei:ei + 1], scalar2=VAL[:, t, ei:ei + 1],
                                                                    op0=ALU.is_equal, op1=ALU.mult),
                      reads=[kIO], writes=[("PA", t)])

        for g in range(NSLOT - 1):
            dma_wgu(g)
        unit = 0
        gcnt = 0
        ycnt = 0

        def front(ei):
            nonlocal unit, gcnt, ycnt
            for g8 in range(NT // 8):
                ptb = PSB[6 + g8]
                for i in range(8):
                    t = 8 * g8 + i
                    S.add("pe", lambda e, ptb=ptb, i=i, t=t: e.transpose(ptb[:, i * 128:(i + 1) * 128], PA[:, t, :], self.IDB[:]),
                          reads=[("PA", t), "IDB"], writes=[("ps", 6 + g8)])
                S.add("act", lambda e, ptb=ptb, g8=g8: e.activation(out=PTAs[ei % 2][:, 8 * g8:8 * g8 + 8, :], in_=ptb[:].rearrange("p (a b) -> p a b", a=8), func=AF.Copy),
                      reads=[("ps", 6 + g8)], writes=[("PTA", ei % 2, g8)])
            for dch in range(NCH):
                for sc in range(NSC):
                    bi = gcnt % 6
                    gcnt += 1
                    pb = PS[bi]
                    for gl in range(4):
                        g_ = sc * 4 + gl
                        for i in range(GRP):
                            t = g_ * GRP + i
                            S.add("pe", lambda e, pb=pb, gl=gl, i=i, t=t, dch=dch: e.matmul(pb[:, gl * 128:(gl + 1) * 128], XB[:, t, dch * 128:(dch + 1) * 128], PA[:, t, :],
                                                                                         start=(i == 0), stop=(i == GRP - 1)),
                                  reads=[("PA", t)], writes=[("ps", bi)])
                    eng = "act" if (gcnt % 2 == 0) else "dve"
                    if eng == "act":
                        S.add("act", lambda e, pb=pb, dch=dch, sc=sc: e.activation(out=XG[:, dch, sc * 512:(sc + 1) * 512], in_=pb[:], func=AF.Copy), reads=[("ps", bi)], writes=[("XG", dch, sc)])
                    else:
                        S.add("dve", lambda e, pb=pb, dch=dch, sc=sc: e.tensor_copy(out=XG[:, dch, sc * 512:(sc + 1) * 512], in_=pb[:]), reads=[("ps", bi)], writes=[("XG", dch, sc)])

        def gateup(ei):
            nonlocal unit, gcnt, ycnt
            for j in range(8):
                dma_wgu(ei * 8 + j + NSLOT - 1)
                if j == 1:
                    dma_wd(ei)
                slot = (ei * 8 + j) % NSLOT
                W = WGU[slot]
                for c in range(NSC):
                    pr = unit % 3
                    pg_, pu_ = PS[2 * pr], PS[2 * pr + 1]
                    tb = unit % NTMP
                    unit += 1
                    tok = slice(c * 512, (c + 1) * 512)
                    for k in range(NCH):
                        S.add("pe", lambda e, pg_=pg_, W=W, k=k, tok=tok: e.matmul(pg_[:], W[:, k, 0:256:2], XG[:, k, tok], start=(k == 0), stop=(k == 7)),
                              reads=[("WGU", slot), ("XG", k, c)], writes=[("ps", 2 * pr)])
                    for k in range(NCH):
                        S.add("pe", lambda e, pu_=pu_, W=W, k=k, tok=tok: e.matmul(pu_[:], W[:, k, 1:256:2], XG[:, k, tok], start=(k == 0), stop=(k == 7)),
                              reads=[("WGU", slot), ("XG", k, c)], writes=[("ps", 2 * pr + 1)])
                    bg = BGU[:, ei * 16 + 2 * j:ei * 16 + 2 * j + 1]
                    bu = BGU[:, ei * 16 + 2 * j + 1:ei * 16 + 2 * j + 2]
                    tg, tu, ts = TG[tb], TU[tb], TS[tb]
                    S.add("dve", lambda e, tg=tg, pg_=pg_, bg=bg: e.tensor_scalar(out=tg, in0=pg_[:], scalar1=bg, scalar2=7.0, op0=ALU.add, op1=ALU.min),
                          reads=[("ps", 2 * pr), kBGU], writes=[("TG", tb)])
                    S.add("act", lambda e, tu=tu, pu_=pu_, bu=bu: e.activation(out=tu, in_=pu_[:], func=AF.Identity, bias=bu, scale=1.0),
                          reads=[("ps", 2 * pr + 1), kBGU], writes=[("TU", tb)])
                    S.add("act", lambda e, ts=ts, tg=tg: e.activation(out=ts, in_=tg, func=AF.Sigmoid, scale=1.702),
                          reads=[("TG", tb)], writes=[("TS", tb)])
                    S.add("pool", lambda e, tu=tu: e.tensor_scalar(out=tu, in0=tu, scalar1=7.0, scalar2=-7.0, op0=ALU.min, op1=ALU.max),
                          reads=[("TU", tb)], writes=[("TU", tb)])
                    S.add("pool", lambda e, tg=tg, ts=ts: e.tensor_tensor(out=tg, in0=tg, in1=ts, op=ALU.mult),
                          reads=[("TG", tb), ("TS", tb)], writes=[("TG", tb)])
                    S.add("dve", lambda e, tu=tu, tg=tg, j=j, tok=tok: e.scalar_tensor_tensor(out=ACTT[:, j, tok], in0=tu, scalar=1.0, in1=tg, op0=ALU.add, op1=ALU.mult),
                          reads=[("TU", tb), ("TG", tb)], writes=[("ACTT", j, c)])

        def down(ei):
            nonlocal unit, gcnt, ycnt
            for st in range(NST):
                for h in range(2):
                    pi = 6 + (ycnt % 2)
                    ycnt += 1
                    py = PS[pi]
                    for k in range(NCH):
                        S.add("pe", lambda e, py=py, k=k, st=st, h=h: e.matmul(py[:], ACTT[:, k, st * 128:(st + 1) * 128], WD[:, k, h * 512:(h + 1) * 512], start=(k == 0), stop=(k == 7)),
                              reads=[("ACTT", k, st // 4), ("WD", k // 4)], writes=[("ps", pi)])
                    S.add("act", lambda e, py=py, st=st, h=h: e.activation(out=YS[:, st, h * 512:(h + 1) * 512], in_=py[:], func=AF.Copy), reads=[("ps", pi)], writes=[("YS", st, h)])

        def scatter(ei):
            nonlocal unit, gcnt, ycnt
            for t in range(NT):
                for h in range(2):
                    pi = 6 + (ycnt % 2)
                    ycnt += 1
                    py = PS[pi]
                    S.add("pe", lambda e, py=py, t=t, h=h: e.matmul(py[:], PTAs[ei % 2][:, t, :], YS[:, t // NQ, h * 512:(h + 1) * 512], start=True, stop=True),
                          reads=[("PTA", ei % 2, t // 8), ("YS", t // NQ, h)], writes=[("ps", pi)])
                    S.add("dve", lambda e, py=py, t=t, h=h, ei=ei: e.scalar_tensor_tensor(out=X[:, t, h * 512:(h + 1) * 512], in0=py[:], scalar=self.GALL[:, t, ei:ei + 1],
                                                                                          in1=X[:, t, h * 512:(h + 1) * 512], op0=ALU.mult, op1=ALU.add),
                          reads=[("ps", pi), ("X", t)], writes=[("X", t)])

        build_sel(0)
        front(0)
        for ei in range(self.n_exp):
            if ei + 1 < self.n_exp:
                build_sel(ei + 1)
            gateup(ei)
            if ei + 1 < self.n_exp:
                front(ei + 1)
            down(ei)
            scatter(ei)
        S.barrier()
        self.arena_reset()

    def ple(self, l):
        S, d, X, XT, PS = self.S, self.d, self.X, self.XT, self.PS
        self.make_xt(l, scale=None, router=False)
        WG = self.alloc([NCH, D], BF16)
        WP = self.alloc([2, D], BF16)
        PT_ = self.alloc([2, S_TOK], BF16)
        BG = self.alloc([D], parts=1)
        SG = [self.alloc([D]) for _ in range(2)]
        kWG, kWP, kPT, kBG = self.key("WG"), self.key("WP"), self.key("PT"), self.key("BG")
        S.add("pool", lambda e: e.dma_start(out=WG, in_=d["ple_gate_w"][l].rearrange("(c p) f -> p c f", p=128)), writes=[kWG], dma="pwg")
        S.add("pool", lambda e: e.dma_start(out=WP, in_=d["ple_w"][l].rearrange("(c p) f -> p c f", p=128)), writes=[kWP], dma="pwp")
        S.add("pool", lambda e: e.dma_start(out=PT_, in_=d["pT"][l].rearrange("(c p) s -> p c s", p=128)), writes=[kPT], dma="ppt")
        S.add("sp", lambda e: e.dma_start(out=BG, in_=d["ple_gate_b"][l:l + 1, :]), writes=[kBG], dma="pbg")
        for t in range(NT):
            sg = SG[t % 2]
            for h in range(2):
                pg = PS[4 + h]
                pp = PS[6 + h]
                cols = slice(h * 512, (h + 1) * 512)
                for k in range(NCH):
                    S.add("pe", lambda e, pg=pg, k=k, t=t, cols=cols: e.matmul(pg[:], XT[:, k, t * 128:(t + 1) * 128], WG[:, k, cols], start=(k == 0), stop=False),
                          reads=[("XT", t), kWG], writes=[("ps", 4 + h)])
                S.add("pe", lambda e, pg=pg, cols=cols: e.matmul(pg[:], self.ONES[0:1, :], BG[0:1, cols], start=False, stop=True),
                      reads=["ONES", kBG], writes=[("ps", 4 + h)])
                for k in range(2):
                    S.add("pe", lambda e, pp=pp, k=k, t=t, cols=cols: e.matmul(pp[:], PT_[:, k, t * 128:(t + 1) * 128], WP[:, k, cols], start=(k == 0), stop=(k == 1)),
                          reads=[kPT, kWP], writes=[("ps", 6 + h)])
                S.add("act", lambda e, sg=sg, pg=pg, cols=cols: e.activation(out=sg[:, cols], in_=pg[:], func=AF.Sigmoid),
                      reads=[("ps", 4 + h)], writes=[("SG", t % 2, h)])
                S.add("dve", lambda e, sg=sg, pp=pp, cols=cols: e.tensor_tensor(out=sg[:, cols], in0=pp[:], in1=sg[:, cols], op=ALU.mult),
                      reads=[("ps", 6 + h), ("SG", t % 2, h)], writes=[("SG", t % 2, h)])
                S.add("pool", lambda e, sg=sg, t=t, cols=cols: e.tensor_tensor(out=X[:, t, cols], in0=X[:, t, cols], in1=sg[:, cols], op=ALU.add),
                      reads=[("SG", t % 2, h), ("X", t)], writes=[("X", t)])

    def retention(self, l):
        S, d, X, XT, PS = self.S, self.d, self.X, self.XT, self.PS
        jl = l // 2
        w_in = d["ret_w_in"][jl].rearrange("(c p) f -> p c f", p=128)
        w_out = d["ret_w_out"][jl]
        gam = self.consts["rgam"]
        COS = self.alloc([S_TOK])
        SIN = self.alloc([S_TOK])
        QT = self.alloc([2, S_TOK], BF16)
        KT = self.alloc([2, S_TOK], BF16)
        V = self.alloc([NT, 512], BF16)
        WC = self.alloc([NCH, 512], BF16)
        WO = self.alloc([4, D], BF16)
        MASK = self.alloc([4, 3, 256])
        kC, kSn, kM = self.key("cos"), self.key("sin"), self.key("rmask")
        S.add("sp", lambda e: e.dma_start(out=COS, in_=d["cos"]), writes=[kC], dma="cos")
        S.add("sp", lambda e: e.dma_start(out=SIN, in_=d["sin"]), writes=[kSn], dma="sin")
        S.add("sp", lambda e: e.dma_start(out=MASK, in_=d["rmask"]), writes=[kM], dma="rmask")
        mark = self.aptr
        PSB = [p[:].bitcast(BF16) for p in PS]
        for h in range(4):
            if h > 0:
                S.barrier()
            self.aptr = mark
            WA = self.alloc([NCH, 512], BF16)
            WB = self.alloc([NCH, 512], BF16)
            T1 = [self.alloc([512]) for _ in range(2)]
            T2 = [self.alloc([512]) for _ in range(2)]
            kWA, kWB, kWC, kWO = self.key("WA"), self.key("WB"), self.key("WC"), self.key("WO")
            S.add("pool", lambda e, h=h, WA=WA: e.dma_start(out=WA[:, :, 0:256], in_=w_in[:, :, h * 256:(h + 1) * 256]), writes=[(kWA, 0)], dma="rwa0")
            S.add("pool", lambda e, h=h, WA=WA: e.dma_start(out=WA[:, :, 256:512], in_=w_in[:, :, 1024 + h * 256:1024 + (h + 1) * 256]), writes=[(kWA, 1)], dma="rwa1")
            S.add("pool", lambda e, h=h, WB=WB: e.dma_start(out=WB, in_=w_in[:, :, 2048 + h * 512:2048 + (h + 1) * 512]), writes=[kWB], dma="rwb")
            S.add("pool", lambda e, h=h: e.dma_start(out=WC, in_=w_in[:, :, 4096 + h * 512:4096 + (h + 1) * 512]), writes=[kWC], dma="rwc")
            S.add("pool", lambda e, h=h: e.dma_start(out=WO, in_=w_out[h * 512:(h + 1) * 512, :].rearrange("(c p) f -> p c f", p=128)), writes=[kWO], dma="rwo")
            u = 0
            for qk in range(2):
                DST = QT if qk == 0 else KT
                for c in range(4):
                    tok = slice(c * 512, (c + 1) * 512)
                    p1, p2 = PS[2 * (u % 2)], PS[2 * (u % 2) + 1]
                    tb = u % 2
                    u += 1
                    for a, pp in ((0, p1), (1, p2)):
                        for kk in range(NCH):
                            S.add("pe", lambda e, pp=pp, kk=kk, a=a, qk=qk, tok=tok, WA=WA: e.matmul(pp[:], WA[:, kk, qk * 256 + a * 128:qk * 256 + (a + 1) * 128], XT[:, kk, tok],
                                                                                              start=(kk == 0), stop=(kk == 7)),
                                  reads=[(kWA, qk)] + [("XT", tt) for tt in range(4 * c, 4 * c + 4)], writes=[("ps", 2 * tb + a)])
                    t1, t2 = T1[tb], T2[tb]
                    k1, k2 = ("T1", tb), ("T2", tb)
                    S.add("dve", lambda e, t1=t1, p1=p1, tok=tok: e.tensor_tensor(out=t1, in0=p1[:], in1=COS[:, tok], op=ALU.mult), reads=[("ps", 2 * tb), kC], writes=[k1])
                    S.add("dve", lambda e, t2=t2, p2=p2, tok=tok: e.tensor_tensor(out=t2, in0=p2[:], in1=SIN[:, tok], op=ALU.mult), reads=[("ps", 2 * tb + 1), kSn], writes=[k2])
                    S.add("pool", lambda e, t1=t1, t2=t2, DST=DST, tok=tok: e.tensor_tensor(out=DST[:, 0, tok], in0=t1, in1=t2, op=ALU.subtract), reads=[k1, k2], writes=[("QK", qk, c, 0)])
                    S.add("dve", lambda e, t1=t1, p1=p1, tok=tok: e.tensor_tensor(out=t1, in0=p1[:], in1=SIN[:, tok], op=ALU.mult), reads=[("ps", 2 * tb), kSn], writes=[k1])
                    S.add("dve", lambda e, t2=t2, p2=p2, tok=tok: e.tensor_tensor(out=t2, in0=p2[:], in1=COS[:, tok], op=ALU.mult), reads=[("ps", 2 * tb + 1), kC], writes=[k2])
                    S.add("pool", lambda e, t1=t1, t2=t2, DST=DST, tok=tok: e.tensor_tensor(out=DST[:, 1, tok], in0=t1, in1=t2, op=ALU.add), reads=[k1, k2], writes=[("QK", qk, c, 1)])
            for t in range(NT):
                pv = PS[4 + t % 2]
                for kk in range(NCH):
                    S.add("pe", lambda e, pv=pv, kk=kk, t=t, WB=WB: e.matmul(pv[:], XT[:, kk, t * 128:(t + 1) * 128], WB[:, kk, :], start=(kk == 0), stop=(kk == 7)),
                          reads=[kWB, ("XT", t)], writes=[("ps", 4 + t % 2)])
                S.add("act", lambda e, pv=pv, t=t: e.activation(out=V[:, t, :], in_=pv[:], func=AF.Copy), reads=[("ps", 4 + t % 2)], writes=[("V", t)])
            S.barrier()
            self.aptr = mark
            PT = self.alloc([NT, 256], BF16)
            RB_ = [self.alloc([512]) for _ in range(2)]
            SG = [self.alloc([512]) for _ in range(2)]
            Y = [self.alloc([512], BF16) for _ in range(2)]
            YT = [self.alloc([4, 128], BF16) for _ in range(2)]
            ST = [self.alloc([16]) for _ in range(2)]
            sc = 0
            for c in range(8):
                q0 = 256 * c
                nk = 2 * c + 2
                for ks in range(nk):
                    pi = sc % 3
                    sc += 1
                    pss = PS[pi]
                    for a in range(2):
                        S.add("pe", lambda e, pss=pss, a=a, ks=ks, q0=q0: e.matmul(pss[:, 0:256], KT[:, a, ks * 128:(ks + 1) * 128], QT[:, a, q0:q0 + 256], start=(a == 0), stop=(a == 1)),
                              reads=[], writes=[("ps", pi)])
                    if ks >= 2 * c:
                        S.add("dve", lambda e, pss=pss, ks=ks, c=c, h=h: e.tensor_tensor(out=PT[:, ks, :], in0=pss[:, 0:256], in1=MASK[:, h, 1 + ks - 2 * c, :], op=ALU.mult),
                              reads=[("ps", pi), kM], writes=[("PT", ks)])
                    else:
                        off = q0 - 128 * ks
                        gv = float(gam[h] ** off)
                        S.add("dve", lambda e, pss=pss, ks=ks, gv=gv, h=h: e.scalar_tensor_tensor(out=PT[:, ks, :], in0=pss[:, 0:256], scalar=gv, in1=MASK[:, h, 0, :], op0=ALU.mult, op1=ALU.mult),
                              reads=[("ps", pi), kM], writes=[("PT", ks)])
                for qi in range(2):
                    qt = 2 * c + qi
                    b = qt % 2
                    po, pg, ptr_, px0, px1 = PS[3], PS[4], PSB[5], PS[6], PS[7]
                    for ks in range(qt + 1):
                        S.add("pe", lambda e, po=po, ks=ks, qi=qi, qt=qt: e.matmul(po[:], PT[:, ks, qi * 128:(qi + 1) * 128], V[:, ks, :], start=(ks == 0), stop=(ks == qt)),
                              reads=[("PT", ks), ("V", ks)], writes=[("ps", 3)])
                    for kk in range(NCH):
                        S.add("pe", lambda e, pg=pg, kk=kk, qt=qt: e.matmul(pg[:], XT[:, kk, qt * 128:(qt + 1) * 128], WC[:, kk, :], start=(kk == 0), stop=(kk == 7)),
                              reads=[kWC, ("XT", qt)], writes=[("ps", 4)])
                    st, rb, sg, y, yt = ST[b], RB_[b], SG[b], Y[b], YT[b]
                    ks_ = ("gn", b)
                    S.add("dve", lambda e, st=st, po=po: e.bn_stats(out=st[:, 0:6], in_=po[:]), reads=[("ps", 3)], writes=[(ks_, 0)])
                    S.add("dve", lambda e, st=st: e.bn_aggr(out=st[:, 6:8], in_=st[:, 0:6]), reads=[(ks_, 0)], writes=[(ks_, 1)])
                    S.add("dve", lambda e, st=st: e.tensor_scalar(out=st[:, 8:9], in0=st[:, 7:8], scalar1=float(GN_EPS), scalar2=None, op0=ALU.add), reads=[(ks_, 1)], writes=[(ks_, 2)])
                    S.add("act", lambda e, st=st: e.activation(out=st[:, 8:9], in_=st[:, 8:9], func=AF.Sqrt), reads=[(ks_, 2)], writes=[(ks_, 2)])
                    S.add("dve", lambda e, st=st: e.reciprocal(out=st[:, 9:10], in_=st[:, 8:9]), reads=[(ks_, 2)], writes=[(ks_, 3)])
                    S.add("dve", lambda e, st=st, po=po, rb=rb: e.tensor_scalar(out=rb, in0=po[:], scalar1=st[:, 6:7], scalar2=st[:, 9:10], op0=ALU.subtract, op1=ALU.mult),
                          reads=[("ps", 3), (ks_, 1), (ks_, 3)], writes=[("RB", b)])
                    S.add("act", lambda e, sg=sg, pg=pg: e.activation(out=sg, in_=pg[:], func=AF.Silu), reads=[("ps", 4)], writes=[("SGr", b)])
                    S.add("pool", lambda e, y=y, rb=rb, sg=sg: e.tensor_tensor(out=y, in0=rb, in1=sg, op=ALU.mult), reads=[("RB", b), ("SGr", b)], writes=[("Y", b)])
                    for a in range(4):
                        S.add("pe", lambda e, ptr_=ptr_, a=a, y=y: e.transpose(ptr_[:, a * 128:(a + 1) * 128], y[:, a * 128:(a + 1) * 128], self.IDB[:]),
                              reads=[("Y", b), "IDB"], writes=[("ps", 5)])
                    S.add("act", lambda e, ptr_=ptr_, yt=yt: e.activation(out=yt, in_=ptr_[:, 0:512].rearrange("p (a b) -> p a b", a=4), func=AF.Copy), reads=[("ps", 5)], writes=[("YT", b)])
                    for hh, px in ((0, px0), (1, px1)):
                        for a in range(4):
                            S.add("pe", lambda e, px=px, a=a, yt=yt, hh=hh: e.matmul(px[:], yt[:, a, :], WO[:, a, hh * 512:(hh + 1) * 512], start=(a == 0), stop=(a == 3)),
                                  reads=[("YT", b), kWO], writes=[("ps", 6 + hh)])
                        S.add("dve", lambda e, px=px, qt=qt, hh=hh: e.tensor_tensor(out=X[:, qt, hh * 512:(hh + 1) * 512], in0=px[:], in1=X[:, qt, hh * 512:(hh + 1) * 512], op=ALU.add),
                              reads=[("ps", 6 + hh), ("X", qt)], writes=[("X", qt)])
        S.barrier()
        self.arena_reset()

    def stickbreak(self, l):
        S, d, X, XT, PS = self.S, self.d, self.X, self.XT, self.PS
        jl = l // 2
        w_in = d["sb_w_in"][jl].rearrange("(c p) f -> p c f", p=128)
        w_out = d["sb_w_out"][jl]
        PSB = [p[:].bitcast(BF16) for p in PS]
        NSET = 3
        SBMf = self.alloc([128])
        SBM = self.alloc([128], BF16)
        kSBM = self.key("sbm")
        S.add("sp", lambda e: e.dma_start(out=SBMf, in_=d["sbmask"]), writes=[kSBM], dma="sbm")
        S.add("dve", lambda e: e.tensor_copy(out=SBM, in_=SBMf), reads=[kSBM], writes=[kSBM])
        WQ = self.alloc([NCH, 128], BF16)
        WK = self.alloc([NCH, 128], BF16)
        WV = self.alloc([NCH, 128], BF16)
        WOp = self.alloc([D], BF16)
        QT = self.alloc([S_TOK], BF16)
        KT = self.alloc([S_TOK], BF16)
        V = self.alloc([NT, 128], BF16)
        E1 = [self.alloc([S_TOK]) for _ in range(NSET)]
        SP = [self.alloc([S_TOK]) for _ in range(NSET)]
        EX = [self.alloc([S_TOK]) for _ in range(NSET)]
        AT = [self.alloc([NT, 128], BF16) for _ in range(NSET)]
        SM = [self.alloc([8]) for _ in range(NSET)]
        OS = [self.alloc([128], BF16) for _ in range(2)]
        OT = [self.alloc([128], BF16) for _ in range(2)]
        for s_ in range(NSET):
            S.add("pool", lambda e, s_=s_: e.memset(EX[s_][:, 0:1], 0.0), writes=[("EX", s_)])

        def load_w(m):
            S.add("pool", lambda e, m=m: e.dma_start(out=WQ, in_=w_in[:, :, m * 128:(m + 1) * 128]), writes=["WQ"], dma="swq")
            S.add("pool", lambda e, m=m: e.dma_start(out=WK, in_=w_in[:, :, 1024 + m * 128:1024 + (m + 1) * 128]), writes=["WK"], dma="swk")
            S.add("pool", lambda e, m=m: e.dma_start(out=WV, in_=w_in[:, :, 2048 + m * 128:2048 + (m + 1) * 128]), writes=["WV"], dma="swv")

        self._zc = 0

        def stA(u):
            m, qt, hh, sb_ = u
            n = 128 * (qt + 1)
            e1, sp, sm = E1[sb_], SP[sb_], SM[sb_]
            hp = slice(64 * hh, 64 * hh + 64)
            nkb = (n + 511) // 512
            for kb in range(nkb):
                w = min(512, n - 512 * kb)
                zb = self._zc % 2
                self._zc += 1
                pz = PS[zb]
                last = (kb == nkb - 1)
                S.add("pe", lambda e, pz=pz, w=w, kb=kb, hp=hp, qt=qt, last=last: e.matmul(pz[:, 0:w], QT[hp, qt * 128:(qt + 1) * 128], KT[hp, 512 * kb:512 * kb + w], start=True, stop=(not last)),
                      reads=[("QTKT", 0, qt // 4), ("QTKT", 1, kb)], writes=[("ps", zb)])
                if last:
                    S.add("pe", lambda e, pz=pz, w=w: e.matmul(pz[:, w - 128:w], self.IDB[:], SBM, start=False, stop=True),
                          reads=["IDB", kSBM], writes=[("ps", zb)])
                S.add("act", lambda e, pz=pz, e1=e1, kb=kb, w=w: e.activation(out=e1[:, 512 * kb:512 * kb + w], in_=pz[:, 0:w], func=AF.Exp),
                      reads=[("ps", zb)], writes=[("E1", sb_)])
            S.add("act", lambda e, e1=e1, sp=sp, n=n, sm=sm: e.activation(out=sp[:, 0:n], in_=e1[:, 0:n], func=AF.Ln, bias=1.0, scale=1.0, accum_out=sm[:, 0:1]),
                  reads=[("E1", sb_)], writes=[("SP", sb_), ("TT", sb_)])

        def stB1(u):
            m, qt, hh, sb_ = u
            n = 128 * (qt + 1)
            sp, ex, sm = SP[sb_], EX[sb_], SM[sb_]
            S.add("dve", lambda e, sm=sm: e.tensor_scalar(out=sm[:, 1:2], in0=sm[:, 0:1], scalar1=-1.0, scalar2=None, op0=ALU.mult), reads=[("TT", sb_)], writes=[("NTT", sb_)])
            S.add("dve", lambda e, sp=sp, ex=ex, n=n: e.tensor_tensor_scan(out=ex[:, 1:n], data0=sp[:, 0:n - 1], data1=sp[:, 0:n - 1], initial=0.0, op0=ALU.add, op1=ALU.max),
                  reads=[("SP", sb_)], writes=[("EX", sb_)])

        def stB2(u):
            m, qt, hh, sb_ = u
            n = 128 * (qt + 1)
            e1, sp, ex, sm = E1[sb_], SP[sb_], EX[sb_], SM[sb_]
            a_ = ex.bitcast(BF16)[:, S_TOK:2 * S_TOK]
            S.add("act", lambda e, ex=ex, sp=sp, n=n, sm=sm: e.activation(out=sp[:, 0:n], in_=ex[:, 0:n], func=AF.Exp, bias=sm[:, 1:2], scale=1.0),
                  reads=[("EX", sb_), ("NTT", sb_)], writes=[("SP", sb_)])
            S.add("dve", lambda e, e1=e1, sp=sp, a_=a_, n=n: e.tensor_tensor(out=a_[:, 0:n], in0=e1[:, 0:n], in1=sp[:, 0:n], op=ALU.mult),
                  reads=[("E1", sb_), ("SP", sb_)], writes=[("EX", sb_), ("A", sb_)])

        def stC1(u):
            m, qt, hh, sb_ = u
            ex, at = EX[sb_], AT[sb_]
            a_ = ex.bitcast(BF16)[:, S_TOK:2 * S_TOK]
            for g in range((qt + 8) // 8):
                nb = min(8, qt + 1 - 8 * g)
                ptb = PSB[2 + g % 2]
                for i in range(nb):
                    kb2 = 8 * g + i
                    S.add("pe", lambda e, ptb=ptb, i=i, kb2=kb2, a_=a_: e.transpose(ptb[:, i * 128:(i + 1) * 128], a_[:, kb2 * 128:(kb2 + 1) * 128], self.IDB[:]),
                          reads=[("A", sb_), "IDB"], writes=[("ps", 2 + g % 2)])

        def stC1b(u):
            m, qt, hh, sb_ = u
            at = AT[sb_]
            for g in range((qt + 8) // 8):
                nb = min(8, qt + 1 - 8 * g)
                ptb = PSB[2 + g % 2]
                S.add("dve", lambda e, ptb=ptb, at=at, g=g, nb=nb: e.tensor_copy(out=at[:, 8 * g:8 * g + nb, :], in_=ptb[:, 0:nb * 128].rearrange("p (a b) -> p a b", a=nb)),
                      reads=[("ps", 2 + g % 2)], writes=[("AT", sb_, g)])

        def stC2(u):
            m, qt, hh, sb_ = u
            at = AT[sb_]
            ob = qt % 2
            hp = slice(64 * hh, 64 * hh + 64)
            po = PS[4 + hh]
            for kb2 in range(qt + 1):
                S.add("pe", lambda e, po=po, kb2=kb2, at=at, hp=hp, qt=qt: e.matmul(po[:, 0:64], at[:, kb2, :], V[:, kb2, hp], start=(kb2 == 0), stop=(kb2 == qt)),
                      reads=[("AT", sb_, kb2 // 8), ("V", kb2)], writes=[("ps", 4 + hh)])
            S.add("dve", lambda e, po=po, hp=hp, ob=ob: e.tensor_copy(out=OS[ob][:, hp], in_=po[:, 0:64]), reads=[("ps", 4 + hh)], writes=[("OS", ob, hh)])
            if hh == 1:
                ptb = PSB[6]
                S.add("pe", lambda e, ptb=ptb, ob=ob: e.transpose(ptb[:, 0:128], OS[ob], self.IDB[:]), reads=[("OS", ob, 0), ("OS", ob, 1), "IDB"], writes=[("ps", 6)])
                S.add("dve", lambda e, ptb=ptb, ob=ob: e.tensor_copy(out=OT[ob], in_=ptb[:, 0:128]), reads=[("ps", 6)], writes=[("OT", ob)])
                for hf in range(2):
                    px = PS[6 + hf]
                    S.add("pe", lambda e, px=px, ob=ob, hf=hf: e.matmul(px[:], OT[ob], WOp[:, hf * 512:(hf + 1) * 512], start=True, stop=True),
                          reads=[("OT", ob), "WOp"], writes=[("ps", 6 + hf)])
                    S.add("dve", lambda e, px=px, qt=qt, hf=hf: e.tensor_tensor(out=X[:, qt, hf * 512:(hf + 1) * 512], in0=px[:], in1=X[:, qt, hf * 512:(hf + 1) * 512], op=ALU.add),
                          reads=[("ps", 6 + hf), ("X", qt)], writes=[("X", qt)])

        ucount = 0
        for m in range(8):
            load_w(m)
            for c in range(4):
                tok = slice(c * 512, (c + 1) * 512)
                for which, W_, DST, scl in ((0, WQ, QT, 0.125), (1, WK, KT, 1.0)):
                    pp = PS[which]
                    for kk in range(NCH):
                        S.add("pe", lambda e, pp=pp, kk=kk, W_=W_, tok=tok: e.matmul(pp[:], W_[:, kk, :], XT[:, kk, tok], start=(kk == 0), stop=(kk == 7)),
                              reads=["WQ" if which == 0 else "WK"] + [("XT", tt) for tt in range(4 * c, 4 * c + 4)], writes=[("ps", which)])
                    S.add("act", lambda e, pp=pp, DST=DST, tok=tok, scl=scl: e.activation(out=DST[:, tok], in_=pp[:], func=AF.Copy, scale=float(scl)),
                          reads=[("ps", which)], writes=[("QTKT", which, c)])
            for t in range(NT):
                pv = PS[2 + t % 2]
                for kk in range(NCH):
                    S.add("pe", lambda e, pv=pv, kk=kk, t=t: e.matmul(pv[:, 0:128], XT[:, kk, t * 128:(t + 1) * 128], WV[:, kk, :], start=(kk == 0), stop=(kk == 7)),
                          reads=["WV", ("XT", t)], writes=[("ps", 2 + t % 2)])
                S.add("dve", lambda e, pv=pv, t=t: e.tensor_copy(out=V[:, t, :], in_=pv[:, 0:128]), reads=[("ps", 2 + t % 2)], writes=[("V", t)])
            S.add("pool", lambda e, m=m: e.dma_start(out=WOp, in_=w_out[m * 128:(m + 1) * 128, :]), writes=["WOp"], dma="swo")
            units = []
            for qt in range(NT):
                for hh in range(2):
                    units.append((m, qt, hh, ucount % NSET))
                    ucount += 1
            nu = len(units)
            for step in range(nu + 2):
                if step < nu:
                    stA(units[step])
                if 0 <= step - 1 < nu:
                    stB1(units[step - 1])
                if 0 <= step - 2 < nu:
                    stC1(units[step - 2])
                    stC1b(units[step - 2])
                    stC2(units[step - 2])
                if 0 <= step - 1 < nu:
                    stB2(units[step - 1])
        S.barrier()
        self.arena_reset()


def _host_inputs(inp, b, consts, big=True):
    f = lambda a: np.ascontiguousarray(np.asarray(a, dtype=np.float32))
    m = {}
    m["x"] = f(inp["x"][b])
    m["pT"] = f(np.transpose(np.asarray(inp["p"])[:, b], (0, 2, 1)))
    for k in ("ret_w_in", "ret_w_out", "sb_w_in", "sb_w_out", "router_w", "router_b", "w_gate_up", "w_down",
              "b_down", "ple_w", "ple_gate_w", "ple_gate_b"):
        if not big and k in ("w_gate_up", "w_down"):
            m[k] = f(np.asarray(inp[k])[0:1, 0:1])
        else:
            m[k] = f(inp[k])
    m["lnp"] = f(np.stack([inp["ln1_g"], inp["ln1_b"], inp["ln2_g"], inp["ln2_b"]], axis=1))
    bgu = np.asarray(inp["b_gate_up"], dtype=np.float32).reshape(DEPTH, NE, 8, 128, 2)
    m["bgu"] = f(np.transpose(bgu, (0, 3, 1, 2, 4)).reshape(DEPTH, 128, NE * 16))
    for k in ("identf", "identb", "ones", "ut", "iota", "onesb", "cos", "sin", "rmask", "sbmask"):
        m[k] = consts[k]
    return m


def run_layers(inp, layers, n_exp=NE, stages=("mix", "moe", "ple"), cores=8):
    bld = Builder(layers, n_exp=n_exp, stages=stages)
    nc = bld.build()
    in_maps = [_host_inputs(inp, b, bld.consts, big=("moe" in stages)) for b in range(cores)]
    res = run_bass_kernel_spmd(nc, in_maps, core_ids=list(range(cores)))
    return np.stack([res.results[b]["y"] for b in range(cores)], axis=0)


def kernel(**inputs):
    out = run_layers(inputs, layers=range(DEPTH))
    return out.astype(np.float32)
```

```python
import math
from contextlib import ExitStack

import ml_dtypes
import numpy as np

import concourse.bass as bass
import concourse.mybir as mybir
from concourse.bass_utils import run_bass_kernel_spmd

F32 = mybir.dt.float32
BF16 = mybir.dt.bfloat16
ALU = mybir.AluOpType
AF = mybir.ActivationFunctionType
AX = mybir.AxisListType

S_TOK = 2048
D = 1024
NT = 16
NCH = 8
DEPTH = 4
NE = 32
DN_ALPHA = float((2 * DEPTH) ** 0.25)
LN_EPS = 1e-5
GN_EPS = 1e-6
SEG = 30000
GRP = 4


class _Op:
    __slots__ = ("eng", "fn", "deps", "dma", "sig", "sigval", "idx")


class Sched:
    ENGS = ("pe", "act", "dve", "pool", "sp")

    def __init__(self, nc):
        self.nc = nc
        self.streams = {e: [] for e in self.ENGS}
        self.last_w = {}
        self.readers = {}
        self.dma_cnt = {}
        self.dma_last = {}
        self.barrier_ops = []
        self.barrier_seen = {e: True for e in self.ENGS}

    def barrier(self):
        ops = []
        for e in self.ENGS:
            for op in reversed(self.streams[e]):
                if op.dma is None:
                    ops.append(op)
                    break
        ops.extend(self.dma_last.values())
        self.barrier_ops = ops
        self.barrier_seen = {e: False for e in self.ENGS}
        self.last_w = {}
        self.readers = {}

    def add(self, eng, fn, reads=(), writes=(), dma=None):
        op = _Op()
        op.eng, op.fn, op.dma = eng, fn, None
        op.sig = False
        op.sigval = None
        ps_r = [k for k in reads if isinstance(k, tuple) and k[0] == "ps"]
        if ps_r:
            reads = [k for k in reads if not (isinstance(k, tuple) and k[0] == "ps")]
            writes = list(writes) + ps_r
        deps = {}
        for k in reads:
            w = self.last_w.get(k)
            if w is not None:
                deps[w] = True
        for k in writes:
            w = self.last_w.get(k)
            if w is not None:
                deps[w] = True
            for r in self.readers.get(k, ()):
                if r not in deps:
                    deps[r] = False
        if not self.barrier_seen[eng]:
            self.barrier_seen[eng] = True
            for b in self.barrier_ops:
                deps[b] = True
        for k in reads:
            self.readers.setdefault(k, []).append(op)
        for k in writes:
            self.last_w[k] = op
            self.readers[k] = []
        pruned = []
        latest = {}
        for d, strong in deps.items():
            if d is op:
                continue
            if d.dma is not None:
                pruned.append(d)
                continue
            if d.eng == eng and (eng == "pe" or not strong):
                continue
            o = latest.get(d.eng)
            if o is None or o.idx < d.idx:
                latest[d.eng] = d
        pruned.extend(latest.values())
        op.deps = pruned
        op.idx = len(self.streams[eng])
        if dma is not None:
            c = self.dma_cnt.get(dma, 0) + 1
            self.dma_cnt[dma] = c
            op.dma = (dma, 16 * c)
            self.dma_last[dma] = op
        self.streams[eng].append(op)
        return op

    def emit(self):
        nc = self.nc
        for e in self.ENGS:
            for op in self.streams[e]:
                for d in op.deps:
                    if d.dma is None:
                        d.sig = True
        nsegs = {}
        for e in self.ENGS:
            c = 0
            for op in self.streams[e]:
                if op.sig and op.dma is None:
                    op.sigval = (e, c // SEG, c % SEG + 1)
                    c += 1
            nsegs[e] = (c + SEG - 1) // SEG
        with ExitStack() as es:
            sems = {}
            for e in self.ENGS:
                for s in range(nsegs[e]):
                    sems[(e, s)] = es.enter_context(nc.semaphore(f"s_{e}_{s}"))
            dsems = {}
            for g in self.dma_cnt:
                dsems[g] = es.enter_context(nc.semaphore(f"d_{g}"))
            self.n_sems = len(sems) + len(dsems)
            block = es.enter_context(nc.Block())

            def run(ename, eng):
                waited = {}
                for op in self.streams[ename]:
                    need = {}
                    for d in op.deps:
                        if d.dma is not None:
                            key, val = ("d", d.dma[0]), d.dma[1]
                        else:
                            key, val = (d.sigval[0], d.sigval[1]), d.sigval[2]
                        if need.get(key, 0) < val:
                            need[key] = val
                    for key, val in need.items():
                        if waited.get(key, 0) >= val:
                            continue
                        waited[key] = val
                        sem = dsems[key[1]] if key[0] == "d" else sems[key]
                        eng.wait_ge(sem, val)
                    ins = op.fn(eng)
                    if op.dma is not None:
                        ins.then_inc(dsems[op.dma[0]], 16)
                    elif op.sig:
                        ins.then_inc(sems[(op.sigval[0], op.sigval[1])], 1)

            block.tensor(lambda eng: run("pe", eng))
            block.scalar(lambda eng: run("act", eng))
            block.vector(lambda eng: run("dve", eng))
            block.gpsimd(lambda eng: run("pool", eng))
            block.sync(lambda eng: run("sp", eng))


def _consts():
    c = {}
    c["identf"] = np.eye(128, dtype=np.float32)
    c["identb"] = np.eye(128, dtype=np.float32).astype(ml_dtypes.bfloat16)
    c["ones"] = np.ones((1, 128), dtype=np.float32)
    ii = np.arange(128)
    c["ut"] = (ii[:, None] < ii[None, :]).astype(np.float32).astype(ml_dtypes.bfloat16)
    c["iota"] = np.ascontiguousarray(np.broadcast_to(ii[None, :], (128, 128))).astype(np.float32)
    c["onesb"] = np.ones((128, 128), dtype=np.float32).astype(ml_dtypes.bfloat16)
    half = 128
    inv_freq = (1.0 / (10000.0 ** (np.arange(half, dtype=np.float32) / np.float32(half)))).astype(np.float32)
    ang = (np.arange(S_TOK, dtype=np.float32)[None, :] * inv_freq[:, None]).astype(np.float32)
    c["cos"] = np.cos(ang).astype(np.float32)
    c["sin"] = np.sin(ang).astype(np.float32)
    H = 4
    lg = np.log(1.0 - 2.0 ** (-5.0 - np.arange(H, dtype=np.float64)))
    sl = np.arange(128)[:, None].astype(np.float64)
    tl = np.arange(256)[None, :].astype(np.float64)
    masks = np.zeros((H, 3, 128, 256), dtype=np.float32)
    for h in range(H):
        masks[h, 0] = np.exp(lg[h] * (tl - sl)) / 16.0
        for m in range(2):
            s_abs = 128 * m + sl
            dec = np.exp(lg[h] * np.abs(tl - s_abs))
            ok = (np.floor(s_abs / 64) <= np.floor(tl / 64))
            masks[h, 1 + m] = dec * ok / 16.0
    c["rmask"] = np.ascontiguousarray(masks.transpose(2, 0, 1, 3))
    c["rgam"] = np.exp(lg)
    t = np.arange(128)[:, None]
    s = np.arange(128)[None, :]
    c["sbmask"] = np.where(s < t, 0.0, -30000.0).astype(np.float32)
    return c


class Builder:
    def __init__(self, layers, n_exp=NE, stages=("mix", "moe", "ple")):
        self.layers = list(layers)
        self.n_exp = n_exp
        self.stages = stages
        self.nc = bass.Bass("TRN2", target_bir_lowering=False)
        self.consts = _consts()

    def dram_in(self, name, shape, dt=F32):
        return self.nc.dram_tensor(name, list(shape), dt, kind="ExternalInput").ap()

    def build(self):
        nc = self.nc
        L = len(self.layers)
        d = {}
        d["x"] = self.dram_in("x", [S_TOK, D])
        d["pT"] = self.dram_in("pT", [DEPTH, 256, S_TOK])
        d["ret_w_in"] = self.dram_in("ret_w_in", [2, D, 6144])
        d["ret_w_out"] = self.dram_in("ret_w_out", [2, 2048, D])
        d["sb_w_in"] = self.dram_in("sb_w_in", [2, D, 3 * D])
        d["sb_w_out"] = self.dram_in("sb_w_out", [2, D, D])
        d["lnp"] = self.dram_in("lnp", [DEPTH, 4, D])
        d["router_w"] = self.dram_in("router_w", [DEPTH, D, NE])
        d["router_b"] = self.dram_in("router_b", [DEPTH, NE])
        big = "moe" in self.stages
        d["w_gate_up"] = self.dram_in("w_gate_up", [DEPTH, NE, D, 2 * D] if big else [1, 1, D, 2 * D])
        d["bgu"] = self.dram_in("bgu", [DEPTH, 128, NE * 16])
        d["w_down"] = self.dram_in("w_down", [DEPTH, NE, D, D] if big else [1, 1, D, D])
        d["b_down"] = self.dram_in("b_down", [DEPTH, NE, D])
        d["ple_w"] = self.dram_in("ple_w", [DEPTH, 256, D])
        d["ple_gate_w"] = self.dram_in("ple_gate_w", [DEPTH, D, D])
        d["ple_gate_b"] = self.dram_in("ple_gate_b", [DEPTH, D])
        d["identf"] = self.dram_in("identf", [128, 128])
        d["identb"] = self.dram_in("identb", [128, 128], BF16)
        d["ones"] = self.dram_in("ones", [1, 128])
        d["ut"] = self.dram_in("ut", [128, 128], BF16)
        d["iota"] = self.dram_in("iota", [128, 128])
        d["onesb"] = self.dram_in("onesb", [128, 128], BF16)
        d["cos"] = self.dram_in("cos", [128, S_TOK])
        d["sin"] = self.dram_in("sin", [128, S_TOK])
        d["rmask"] = self.dram_in("rmask", [128, 4, 3, 256])
        d["sbmask"] = self.dram_in("sbmask", [128, 128])
        d["y"] = nc.dram_tensor("y", [S_TOK, D], F32, kind="ExternalOutput").ap()
        self.d = d

        AW = 27648
        with ExitStack() as es:
            sb = lambda n, s, dt: es.enter_context(nc.sbuf_tensor(n, s, dt))
            self.X = sb("X", [128, NT, D], F32)
            self.XT = sb("XT", [128, NCH, S_TOK], BF16)
            self.IDF = sb("IDF", [128, 128], F32)
            self.IDB = sb("IDB", [128, 128], BF16)
            self.ONES = sb("ONES", [1, 128], F32)
            self.GALL = sb("GALL", [128, NT, NE], F32)
            self.ARENA = sb("ARENA", [128, AW], F32)
            self.AW = AW
            self.PS = [es.enter_context(nc.psum_tensor(f"ps{i}", [128, 512], F32)) for i in range(8)]
            self.S = Sched(nc)
            self.uid = 0
            self.program()
            self.S.emit()
        return nc

    def arena_reset(self):
        self.aptr = 0

    def alloc(self, shape, dt=F32, parts=128):
        n = int(np.prod(shape))
        words = n if dt == F32 else (n + 1) // 2
        words = (words + 7) // 8 * 8
        a = self.ARENA[0:parts, self.aptr:self.aptr + words]
        self.aptr += words
        assert self.aptr <= self.AW, f"arena overflow {self.aptr} > {self.AW}"
        if dt != F32:
            a = a.bitcast(dt)[:, 0:n]
        else:
            a = a[:, 0:n]
        if len(shape) == 2:
            a = a.rearrange("p (a b) -> p a b", a=shape[0])
        elif len(shape) == 3:
            a = a.rearrange("p (a b c) -> p a b c", a=shape[0], b=shape[1])
        return a

    def key(self, base):
        self.uid += 1
        return (base, self.uid)

    def program(self):
        S, d = self.S, self.d
        X = self.X
        S.add("sp", lambda e: e.dma_start(out=self.IDF[:], in_=d["identf"]), writes=["IDF"], dma="c0")
        S.add("sp", lambda e: e.dma_start(out=self.IDB[:], in_=d["identb"]), writes=["IDB"], dma="c1")
        S.add("sp", lambda e: e.dma_start(out=self.ONES[:], in_=d["ones"]), writes=["ONES"], dma="c2")
        xin = d["x"].rearrange("(t p) f -> p t f", p=128)
        for q in range(4):
            S.add("sp", lambda e, q=q: e.dma_start(out=X[:, 4 * q:4 * q + 4, :], in_=xin[:, 4 * q:4 * q + 4, :]),
                  writes=[("X", t) for t in range(4 * q, 4 * q + 4)], dma=f"xin{q}")
        for l in self.layers:
            if "mix" in self.stages:
                self.arena_reset()
                S.barrier()
                self.make_xt(l, scale=DN_ALPHA, router=False)
                if l % 2 == 0:
                    self.retention(l)
                else:
                    self.stickbreak(l)
                self.layernorm(l, 0)
            if "moe" in self.stages:
                self.arena_reset()
                S.barrier()
                self.moe(l)
                self.layernorm(l, 1)
            if "ple" in self.stages:
                self.arena_reset()
                S.barrier()
                self.ple(l)
        yout = d["y"].rearrange("(t p) f -> p t f", p=128)
        for q in range(4):
            S.add("sp", lambda e, q=q: e.dma_start(out=yout[:, 4 * q:4 * q + 4, :], in_=X[:, 4 * q:4 * q + 4, :]),
                  reads=[("X", t) for t in range(4 * q, 4 * q + 4)], writes=[("yout", q)], dma=f"yout{q}")
        S.add("sp", lambda e: e.nop(), reads=[("yout", q) for q in range(4)])

    def make_xt(self, l, scale=None, router=False):
        S, d, X, XT, PS = self.S, self.d, self.X, self.XT, self.PS
        if router:
            RW = self.alloc([NCH, NE])
            RB = self.alloc([NE], parts=1)
            XT32 = [self.alloc([NCH, 128]) for _ in range(2)]
            LG = [self.alloc([NE]) for _ in range(2)]
            EXm = [self.alloc([NE]) for _ in range(2)]
            MSK = [self.alloc([NE]) for _ in range(2)]
            SM = [self.alloc([16]) for _ in range(2)]
            UT = self.alloc([128], BF16)
            kUT = self.key("UT")
            S.add("sp", lambda e: e.dma_start(out=UT, in_=d["ut"]), writes=[kUT], dma="ut")
            ONB = self.alloc([128], BF16)
            kONB = self.key("ONB")
            S.add("sp", lambda e: e.dma_start(out=ONB, in_=d["onesb"]), writes=[kONB], dma="onb")
            XB = self.XB
            kRW, kRB = self.key("RW"), self.key("RB")
            S.add("sp", lambda e: e.dma_start(out=RW, in_=d["router_w"][l].rearrange("(c p) n -> p c n", p=128)),
                  writes=[kRW], dma="rw")
            S.add("sp", lambda e: e.dma_start(out=RB, in_=d["router_b"][l:l + 1, :]), writes=[kRB], dma="rb")
        for t in range(NT):
            b = t % 2
            for h in range(2):
                pb = PS[2 * b + h]
                for j in range(4):
                    c = 4 * h + j
                    S.add("pe", lambda e, pb=pb, j=j, c=c, t=t: e.transpose(pb[:, j * 128:(j + 1) * 128], X[:, t, c * 128:(c + 1) * 128], self.IDF[:]),
                          reads=[("X", t), "IDF"], writes=[("ps", 2 * b + h)])
                if not router:
                    S.add("act", lambda e, pb=pb, h=h, t=t: e.activation(out=XT[:, 4 * h:4 * h + 4, t * 128:(t + 1) * 128],
                                                                          in_=pb[:].rearrange("p (a b) -> p a b", a=4), func=AF.Copy),
                          reads=[("ps", 2 * b + h)], writes=[("XT", t)])
                if router:
                    S.add("dve", lambda e, pb=pb, h=h, b=b: e.tensor_copy(out=XT32[b][:, 4 * h:4 * h + 4, :],
                                                                           in_=pb[:].rearrange("p (a b) -> p a b", a=4)),
                          reads=[("ps", 2 * b + h)], writes=[("XT32", b, h)])
            if router:
                S.add("act", lambda e, t=t: e.activation(out=XB[:, t, :], in_=X[:, t, :], func=AF.Copy), reads=[("X", t)], writes=[("XB", t)])
            if scale is not None:
                S.add("pool", lambda e, t=t: e.tensor_scalar(out=X[:, t, :], in0=X[:, t, :], scalar1=float(scale), scalar2=None, op0=ALU.mult),
                      reads=[("X", t)], writes=[("X", t)])
            if router:
                pl = PS[4 + b]
                for c in range(NCH):
                    S.add("pe", lambda e, pl=pl, c=c, b=b: e.matmul(pl[:, 0:NE], XT32[b][:, c, :], RW[:, c, :], start=(c == 0), stop=False),
                          reads=[("XT32", b, c // 4), kRW], writes=[("ps", 4 + b)])
                S.add("pe", lambda e, pl=pl: e.matmul(pl[:, 0:NE], self.ONES[0:1, :], RB[0:1, :], start=False, stop=True),
                      reads=["ONES", kRB], writes=[("ps", 4 + b)])
                lg, ex, mk, sm = LG[b], EXm[b], MSK[b], SM[b]
                kl = ("rt", b)
                S.add("dve", lambda e, pl=pl, lg=lg: e.tensor_copy(out=lg, in_=pl[:, 0:NE]), reads=[("ps", 4 + b)], writes=[(kl, "lg")])
                S.add("dve", lambda e, lg=lg, sm=sm: e.max(out=sm[:, 0:8], in_=lg), reads=[(kl, "lg")], writes=[(kl, "top")])
                S.add("dve", lambda e, lg=lg, sm=sm, mk=mk: e.tensor_scalar(out=mk, in0=lg, scalar1=sm[:, 3:4], scalar2=None, op0=ALU.is_ge),
                      reads=[(kl, "lg"), (kl, "top")], writes=[(kl, "mk")])
                S.add("dve", lambda e, sm=sm: e.tensor_scalar(out=sm[:, 8:9], in0=sm[:, 0:1], scalar1=-1.0, scalar2=None, op0=ALU.mult),
                      reads=[(kl, "top")], writes=[(kl, "nm")])
                S.add("dve", lambda e, mk=mk, t=t: e.tensor_copy(out=self.MB[:, t, :], in_=mk), reads=[(kl, "mk")], writes=[("MB", t)])
                gi = t % GRP
                S.add("pe", lambda e, pl=pl, t=t, gi=gi: e.matmul(pl[:, 64:64 + NE], UT, self.MB[:, t, :], start=True, stop=(gi == 0)),
                      reads=[("MB", t), kUT], writes=[("ps", 4 + b)])
                for tp in range(t - gi, t):
                    S.add("pe", lambda e, pl=pl, tp=tp, t=t: e.matmul(pl[:, 64:64 + NE], ONB, self.MB[:, tp, :], start=False, stop=(tp == t - 1)),
                          reads=[("MB", tp), kONB], writes=[("ps", 4 + b)])
                S.add("dve", lambda e, pl=pl, mk=mk, t=t: e.scalar_tensor_tensor(out=self.VAL[:, t, :], in0=pl[:, 64:64 + NE], scalar=127.5, in1=mk, op0=ALU.is_lt, op1=ALU.mult),
                      reads=[("ps", 4 + b), (kl, "mk")], writes=[("VAL", t)])
                S.add("dve", lambda e, pl=pl, t=t: e.tensor_copy(out=self.RK[:, t, :], in_=pl[:, 64:64 + NE]), reads=[("ps", 4 + b)], writes=[("RK", t)])
                S.add("act", lambda e, lg=lg, ex=ex, sm=sm: e.activation(out=ex, in_=lg, func=AF.Exp, bias=sm[:, 8:9], scale=1.0),
                      reads=[(kl, "lg"), (kl, "nm")], writes=[(kl, "ex")])
                S.add("dve", lambda e, ex=ex, mk=mk: e.tensor_tensor(out=ex, in0=ex, in1=mk, op=ALU.mult),
                      reads=[(kl, "ex"), (kl, "mk")], writes=[(kl, "ex")])
                S.add("dve", lambda e, ex=ex, sm=sm: e.reduce_sum(out=sm[:, 9:10], in_=ex, axis=AX.X),
                      reads=[(kl, "ex")], writes=[(kl, "ss")])
                S.add("dve", lambda e, sm=sm: e.reciprocal(out=sm[:, 10:11], in_=sm[:, 9:10]), reads=[(kl, "ss")], writes=[(kl, "rs")])
                S.add("dve", lambda e, ex=ex, sm=sm, t=t: e.tensor_scalar(out=self.GALL[:, t, :], in0=ex, scalar1=sm[:, 10:11], scalar2=None, op0=ALU.mult),
                      reads=[(kl, "ex"), (kl, "rs")], writes=[("G", t)])
                pg = PS[6 + b]
                S.add("pe", lambda e, pg=pg, t=t: e.transpose(pg[0:NE, 0:128], self.GALL[:, t, :], self.IDF[:]),
                      reads=[("G", t), "IDF"], writes=[("ps", 6 + b)])
                S.add("act", lambda e, pg=pg, t=t: e.activation(out=self.GT[0:NE, t * 128:(t + 1) * 128], in_=pg[0:NE, 0:128], func=AF.Copy),
                      reads=[("ps", 6 + b)], writes=[("GT", t)])

    def ln_alloc(self):
        return (self.alloc([D]), self.alloc([D]), [self.alloc([16]) for _ in range(2)])

    def layernorm(self, l, which, bufs=None):
        S, d, X = self.S, self.d, self.X
        G, B, ST = bufs if bufs is not None else self.ln_alloc()
        kG, kB = self.key("lng"), self.key("lnb")
        S.add("sp", lambda e: e.dma_start(out=G, in_=d["lnp"][l, 2 * which:2 * which + 1, :].to_broadcast([128, D])), writes=[kG], dma="lng")
        S.add("sp", lambda e: e.dma_start(out=B, in_=d["lnp"][l, 2 * which + 1:2 * which + 2, :].to_broadcast([128, D])), writes=[kB], dma="lnb")
        for t in range(NT):
            st = ST[t % 2]
            ks = ("lnst", t % 2)
            xt = X[:, t, :]
            S.add("dve", lambda e, st=st, xt=xt: e.bn_stats(out=st[:, 0:6], in_=xt[:, 0:512]), reads=[("X", t)], writes=[(ks, 0)])
            S.add("dve", lambda e, st=st, xt=xt: e.bn_stats(out=st[:, 6:12], in_=xt[:, 512:1024]), reads=[("X", t)], writes=[(ks, 1)])
            S.add("dve", lambda e, st=st: e.bn_aggr(out=st[:, 12:14], in_=st[:, 0:12]),
                  reads=[(ks, 0), (ks, 1)], writes=[(ks, 2)])
            S.add("dve", lambda e, st=st: e.tensor_scalar(out=st[:, 14:15], in0=st[:, 13:14], scalar1=float(LN_EPS), scalar2=None, op0=ALU.add),
                  reads=[(ks, 2)], writes=[(ks, 3)])
            S.add("act", lambda e, st=st: e.activation(out=st[:, 14:15], in_=st[:, 14:15], func=AF.Sqrt), reads=[(ks, 3)], writes=[(ks, 3)])
            S.add("dve", lambda e, st=st: e.reciprocal(out=st[:, 15:16], in_=st[:, 14:15]), reads=[(ks, 3)], writes=[(ks, 4)])
            S.add("dve", lambda e, st=st, xt=xt: e.tensor_scalar(out=xt, in0=xt, scalar1=st[:, 12:13], scalar2=st[:, 15:16], op0=ALU.subtract, op1=ALU.mult),
                  reads=[("X", t), (ks, 2), (ks, 4)], writes=[("X", t)])
            S.add("pool", lambda e, xt=xt: e.tensor_tensor(out=xt, in0=xt, in1=G, op=ALU.mult), reads=[("X", t), kG], writes=[("X", t)])
            S.add("pool", lambda e, xt=xt: e.tensor_tensor(out=xt, in0=xt, in1=B, op=ALU.add), reads=[("X", t), kB], writes=[("X", t)])

    def moe_bias(self, l, GT, BD, BGU):
        S, d, X, PS = self.S, self.d, self.X, self.PS
        kBD, kBGU = self.key("BD"), self.key("BGU")
        self.moe_keys = (kBD, kBGU)
        S.add("sp", lambda e: e.dma_start(out=BD, in_=d["b_down"][l]), writes=[kBD], dma="bd")
        S.add("sp", lambda e: e.dma_start(out=BGU, in_=d["bgu"][l]), writes=[kBGU], dma="bgu")
        for t in range(NT):
            for h in range(2):
                pb = PS[6 + h]
                S.add("pe", lambda e, pb=pb, t=t, h=h: e.matmul(pb[:], GT[0:NE, t * 128:(t + 1) * 128], BD[0:NE, h * 512:(h + 1) * 512], start=True, stop=True),
                      reads=[("GT", t), kBD], writes=[("ps", 6 + h)])
                S.add("dve", lambda e, pb=pb, t=t, h=h: e.tensor_tensor(out=X[:, t, h * 512:(h + 1) * 512], in0=pb[:], in1=X[:, t, h * 512:(h + 1) * 512], op=ALU.add),
                      reads=[("ps", 6 + h), ("X", t)], writes=[("X", t)])

    def moe(self, l):
        S, d, X, PS = self.S, self.d, self.X, self.PS
        PSB = [p[:].bitcast(BF16) for p in PS]
        NQ = GRP
        NST = NT // GRP
        NSL = NST * 128
        NSC = NSL // 512
        self.XB = self.XT[:].rearrange("p c s -> p (c s)").rearrange("p (t f) -> p t f", t=NT)
        XB = self.XB
        BGU = self.alloc([NE * 16])
        self.RK = self.alloc([NT, NE])
        self.VAL = self.alloc([NT, NE])
        self.MB = self.alloc([NT, NE], BF16)
        IOTA = self.alloc([128])
        kIO = self.key("iota")
        S.add("sp", lambda e: e.dma_start(out=IOTA, in_=d["iota"]), writes=[kIO], dma="iota")
        mark = self.aptr
        self.GT = self.alloc([S_TOK], parts=32)
        BD = self.alloc([D], parts=32)
        self.make_xt(l, scale=DN_ALPHA, router=True)
        self.moe_bias(l, self.GT, BD, BGU)
        S.barrier()
        self.aptr = mark
        kBD, kBGU = self.moe_keys
        RK, VAL = self.RK, self.VAL
        XG = self.alloc([NCH, NSL], BF16)
        ACTT = self.alloc([NCH, NSL], BF16)
        YS = self.alloc([NST, D], BF16)
        PA = self.alloc([NT, 128], BF16)
        PTAs = [self.alloc([NT, 128], BF16) for _ in range(2)]
        NSLOT = 6
        WGU = [self.alloc([NCH, 256], BF16) for _ in range(NSLOT)]
        WD = self.alloc([NCH, D], BF16)
        NTMP = 3
        TG = [self.alloc([512]) for _ in range(NTMP)]
        TU = [self.alloc([512]) for _ in range(NTMP)]
        TS = [self.alloc([512]) for _ in range(NTMP)]
        wgu_src = d["w_gate_up"]
        wd_src = d["w_down"]
        n_chunks = self.n_exp * 8

        def dma_wgu(g):
            if g >= n_chunks:
                return
            slot = g % NSLOT
            wsrc = wgu_src[l, g // 8].rearrange("(c p) f -> p c f", p=128)
            j = g % 8
            S.add("pool", lambda e, slot=slot, j=j, wsrc=wsrc: e.dma_start(out=WGU[slot], in_=wsrc[:, :, 256 * j:256 * (j + 1)]),
                  writes=[("WGU", slot)], dma=f"wgu{slot}")

        def dma_wd(ei):
            src = wd_src[l, ei].rearrange("(c p) f -> p c f", p=128)
            for q in range(2):
                S.add("pool", lambda e, q=q, src=src: e.dma_start(out=WD[:, 4 * q:4 * q + 4, :], in_=src[:, 4 * q:4 * q + 4, :]),
                      writes=[("WD", q)], dma=f"wd{q}")

        def build_sel(ei):
            for t in range(NT):
                S.add("dve", lambda e, t=t, ei=ei: e.tensor_scalar(out=PA[:, t, :], in0=IOTA, scalar1=RK[:, t, ei:ei + 1], scalar2=VAL[:, t, ei:ei + 1],
                                                                    op0=ALU.is_equal, op1=ALU.mult),
                      reads=[kIO], writes=[("PA", t)])

        for g in range(NSLOT - 1):
            dma_wgu(g)
        unit = 0
        gcnt = 0
        ycnt = 0

        def front(ei):
            nonlocal unit, gcnt, ycnt
            for g8 in range(NT // 8):
                ptb = PSB[6 + g8]
                for i in range(8):
                    t = 8 * g8 + i
                    S.add("pe", lambda e, ptb=ptb, i=i, t=t: e.transpose(ptb[:, i * 128:(i + 1) * 128], PA[:, t, :], self.IDB[:]),
                          reads=[("PA", t), "IDB"], writes=[("ps", 6 + g8)])
                S.add("act", lambda e, ptb=ptb, g8=g8: e.activation(out=PTAs[ei % 2][:, 8 * g8:8 * g8 + 8, :], in_=ptb[:].rearrange("p (a b) -> p a b", a=8), func=AF.Copy),
                      reads=[("ps", 6 + g8)], writes=[("PTA", ei % 2, g8)])
            for dch in range(NCH):
                for sc in range(NSC):
                    bi = gcnt % 6
                    gcnt += 1
                    pb = PS[bi]
                    for gl in range(4):
                        g_ = sc * 4 + gl
                        for i in range(GRP):
                            t = g_ * GRP + i
                            S.add("pe", lambda e, pb=pb, gl=gl, i=i, t=t, dch=dch: e.matmul(pb[:, gl * 128:(gl + 1) * 128], XB[:, t, dch * 128:(dch + 1) * 128], PA[:, t, :],
                                                                                         start=(i == 0), stop=(i == GRP - 1)),
                                  reads=[("PA", t)], writes=[("ps", bi)])
                    eng = "act" if (gcnt % 2 == 0) else "dve"
                    if eng == "act":
                        S.add("act", lambda e, pb=pb, dch=dch, sc=sc: e.activation(out=XG[:, dch, sc * 512:(sc + 1) * 512], in_=pb[:], func=AF.Copy), reads=[("ps", bi)], writes=[("XG", dch, sc)])
                    else:
                        S.add("dve", lambda e, pb=pb, dch=dch, sc=sc: e.tensor_copy(out=XG[:, dch, sc * 512:(sc + 1) * 512], in_=pb[:]), reads=[("ps", bi)], writes=[("XG", dch, sc)])

        def gateup(ei):
            nonlocal unit, gcnt, ycnt
            for j in range(8):
                dma_wgu(ei * 8 + j + NSLOT - 1)
                if j == 1:
                    dma_wd(ei)
                slot = (ei * 8 + j) % NSLOT
                W = WGU[slot]
                for c in range(NSC):
                    pr = unit % 3
                    pg_, pu_ = PS[2 * pr], PS[2 * pr + 1]
                    tb = unit % NTMP
                    unit += 1
                    tok = slice(c * 512, (c + 1) * 512)
                    for k in range(NCH):
                        S.add("pe", lambda e, pg_=pg_, W=W, k=k, tok=tok: e.matmul(pg_[:], W[:, k, 0:256:2], XG[:, k, tok], start=(k == 0), stop=(k == 7)),
                              reads=[("WGU", slot), ("XG", k, c)], writes=[("ps", 2 * pr)])
                    for k in range(NCH):
                        S.add("pe", lambda e, pu_=pu_, W=W, k=k, tok=tok: e.matmul(pu_[:], W[:, k, 1:256:2], XG[:, k, tok], start=(k == 0), stop=(k == 7)),
                              reads=[("WGU", slot), ("XG", k, c)], writes=[("ps", 2 * pr + 1)])
                    bg = BGU[:, ei * 16 + 2 * j:ei * 16 + 2 * j + 1]
                    bu = BGU[:, ei * 16 + 2 * j + 1:ei * 16 + 2 * j + 2]
                    tg, tu, ts = TG[tb], TU[tb], TS[tb]
                    S.add("dve", lambda e, tg=tg, pg_=pg_, bg=bg: e.tensor_scalar(out=tg, in0=pg_[:], scalar1=bg, scalar2=7.0, op0=ALU.add, op1=ALU.min),
                          reads=[("ps", 2 * pr), kBGU], writes=[("TG", tb)])
                    S.add("act", lambda e, tu=tu, pu_=pu_, bu=bu: e.activation(out=tu, in_=pu_[:], func=AF.Identity, bias=bu, scale=1.0),
                          reads=[("ps", 2 * pr + 1), kBGU], writes=[("TU", tb)])
                    S.add("act", lambda e, ts=ts, tg=tg: e.activation(out=ts, in_=tg, func=AF.Sigmoid, scale=1.702),
                          reads=[("TG", tb)], writes=[("TS", tb)])
                    S.add("pool", lambda e, tu=tu: e.tensor_scalar(out=tu, in0=tu, scalar1=7.0, scalar2=-7.0, op0=ALU.min, op1=ALU.max),
                          reads=[("TU", tb)], writes=[("TU", tb)])
                    S.add("pool", lambda e, tg=tg, ts=ts: e.tensor_tensor(out=tg, in0=tg, in1=ts, op=ALU.mult),
                          reads=[("TG", tb), ("TS", tb)], writes=[("TG", tb)])
                    S.add("dve", lambda e, tu=tu, tg=tg, j=j, tok=tok: e.scalar_tensor_tensor(out=ACTT[:, j, tok], in0=tu, scalar=1.0, in1=tg, op0=ALU.add, op1=ALU.mult),
                          reads=[("TU", tb), ("TG", tb)], writes=[("ACTT", j, c)])

        def down(ei):
            nonlocal unit, gcnt, ycnt
            for st in range(NST):
                for h in range(2):
                    pi = 6 + (ycnt % 2)
                    ycnt += 1
                    py = PS[pi]
                    for k in range(NCH):
                        S.add("pe", lambda e, py=py, k=k, st=st, h=h: e.matmul(py[:], ACTT[:, k, st * 128:(st + 1) * 128], WD[:, k, h * 512:(h + 1) * 512], start=(k == 0), stop=(k == 7)),
                              reads=[("ACTT", k, st // 4), ("WD", k // 4)], writes=[("ps", pi)])
                    S.add("act", lambda e, py=py, st=st, h=h: e.activation(out=YS[:, st, h * 512:(h + 1) * 512], in_=py[:], func=AF.Copy), reads=[("ps", pi)], writes=[("YS", st, h)])

        def scatter(ei):
            nonlocal unit, gcnt, ycnt
            for t in range(NT):
                for h in range(2):
                    pi = 6 + (ycnt % 2)
                    ycnt += 1
                    py = PS[pi]
                    S.add("pe", lambda e, py=py, t=t, h=h: e.matmul(py[:], PTAs[ei % 2][:, t, :], YS[:, t // NQ, h * 512:(h + 1) * 512], start=True, stop=True),
                          reads=[("PTA", ei % 2, t // 8), ("YS", t // NQ, h)], writes=[("ps", pi)])
                    S.add("dve", lambda e, py=py, t=t, h=h, ei=ei: e.scalar_tensor_tensor(out=X[:, t, h * 512:(h + 1) * 512], in0=py[:], scalar=self.GALL[:, t, ei:ei + 1],
                                                                                          in1=X[:, t, h * 512:(h + 1) * 512], op0=ALU.mult, op1=ALU.add),
                          reads=[("ps", pi), ("X", t)], writes=[("X", t)])

        build_sel(0)
        front(0)
        for ei in range(self.n_exp):
            if ei + 1 < self.n_exp:
                build_sel(ei + 1)
            gateup(ei)
            if ei + 1 < self.n_exp:
                front(ei + 1)
            down(ei)
            scatter(ei)
        S.barrier()
        self.arena_reset()

    def ple(self, l):
        S, d, X, XT, PS = self.S, self.d, self.X, self.XT, self.PS
        self.make_xt(l, scale=None, router=False)
        WG = self.alloc([NCH, D], BF16)
        WP = self.alloc([2, D], BF16)
        PT_ = self.alloc([2, S_TOK], BF16)
        BG = self.alloc([D], parts=1)
        SG = [self.alloc([D]) for _ in range(2)]
        kWG, kWP, kPT, kBG = self.key("WG"), self.key("WP"), self.key("PT"), self.key("BG")
        S.add("pool", lambda e: e.dma_start(out=WG, in_=d["ple_gate_w"][l].rearrange("(c p) f -> p c f", p=128)), writes=[kWG], dma="pwg")
        S.add("pool", lambda e: e.dma_start(out=WP, in_=d["ple_w"][l].rearrange("(c p) f -> p c f", p=128)), writes=[kWP], dma="pwp")
        S.add("pool", lambda e: e.dma_start(out=PT_, in_=d["pT"][l].rearrange("(c p) s -> p c s", p=128)), writes=[kPT], dma="ppt")
        S.add("sp", lambda e: e.dma_start(out=BG, in_=d["ple_gate_b"][l:l + 1, :]), writes=[kBG], dma="pbg")
        for t in range(NT):
            sg = SG[t % 2]
            for h in range(2):
                pg = PS[4 + h]
                pp = PS[6 + h]
                cols = slice(h * 512, (h + 1) * 512)
                for k in range(NCH):
                    S.add("pe", lambda e, pg=pg, k=k, t=t, cols=cols: e.matmul(pg[:], XT[:, k, t * 128:(t + 1) * 128], WG[:, k, cols], start=(k == 0), stop=False),
                          reads=[("XT", t), kWG], writes=[("ps", 4 + h)])
                S.add("pe", lambda e, pg=pg, cols=cols: e.matmul(pg[:], self.ONES[0:1, :], BG[0:1, cols], start=False, stop=True),
                      reads=["ONES", kBG], writes=[("ps", 4 + h)])
                for k in range(2):
                    S.add("pe", lambda e, pp=pp, k=k, t=t, cols=cols: e.matmul(pp[:], PT_[:, k, t * 128:(t + 1) * 128], WP[:, k, cols], start=(k == 0), stop=(k == 1)),
                          reads=[kPT, kWP], writes=[("ps", 6 + h)])
                S.add("act", lambda e, sg=sg, pg=pg, cols=cols: e.activation(out=sg[:, cols], in_=pg[:], func=AF.Sigmoid),
                      reads=[("ps", 4 + h)], writes=[("SG", t % 2, h)])
                S.add("dve", lambda e, sg=sg, pp=pp, cols=cols: e.tensor_tensor(out=sg[:, cols], in0=pp[:], in1=sg[:, cols], op=ALU.mult),
                      reads=[("ps", 6 + h), ("SG", t % 2, h)], writes=[("SG", t % 2, h)])
                S.add("pool", lambda e, sg=sg, t=t, cols=cols: e.tensor_tensor(out=X[:, t, cols], in0=X[:, t, cols], in1=sg[:, cols], op=ALU.add),
                      reads=[("SG", t % 2, h), ("X", t)], writes=[("X", t)])

    def retention(self, l):
        S, d, X, XT, PS = self.S, self.d, self.X, self.XT, self.PS
        jl = l // 2
        w_in = d["ret_w_in"][jl].rearrange("(c p) f -> p c f", p=128)
        w_out = d["ret_w_out"][jl]
        gam = self.consts["rgam"]
        COS = self.alloc([S_TOK])
        SIN = self.alloc([S_TOK])
        QT = self.alloc([2, S_TOK], BF16)
        KT = self.alloc([2, S_TOK], BF16)
        V = self.alloc([NT, 512], BF16)
        WC = self.alloc([NCH, 512], BF16)
        WO = self.alloc([4, D], BF16)
        MASK = self.alloc([4, 3, 256])
        kC, kSn, kM = self.key("cos"), self.key("sin"), self.key("rmask")
        S.add("sp", lambda e: e.dma_start(out=COS, in_=d["cos"]), writes=[kC], dma="cos")
        S.add("sp", lambda e: e.dma_start(out=SIN, in_=d["sin"]), writes=[kSn], dma="sin")
        S.add("sp", lambda e: e.dma_start(out=MASK, in_=d["rmask"]), writes=[kM], dma="rmask")
        mark = self.aptr
        PSB = [p[:].bitcast(BF16) for p in PS]
        for h in range(4):
            if h > 0:
                S.barrier()
            self.aptr = mark
            WA = self.alloc([NCH, 512], BF16)
            WB = self.alloc([NCH, 512], BF16)
            T1 = [self.alloc([512]) for _ in range(2)]
            T2 = [self.alloc([512]) for _ in range(2)]
            kWA, kWB, kWC, kWO = self.key("WA"), self.key("WB"), self.key("WC"), self.key("WO")
            S.add("pool", lambda e, h=h, WA=WA: e.dma_start(out=WA[:, :, 0:256], in_=w_in[:, :, h * 256:(h + 1) * 256]), writes=[(kWA, 0)], dma="rwa0")
            S.add("pool", lambda e, h=h, WA=WA: e.dma_start(out=WA[:, :, 256:512], in_=w_in[:, :, 1024 + h * 256:1024 + (h + 1) * 256]), writes=[(kWA, 1)], dma="rwa1")
            S.add("pool", lambda e, h=h, WB=WB: e.dma_start(out=WB, in_=w_in[:, :, 2048 + h * 512:2048 + (h + 1) * 512]), writes=[kWB], dma="rwb")
            S.add("pool", lambda e, h=h: e.dma_start(out=WC, in_=w_in[:, :, 4096 + h * 512:4096 + (h + 1) * 512]), writes=[kWC], dma="rwc")
            S.add("pool", lambda e, h=h: e.dma_start(out=WO, in_=w_out[h * 512:(h + 1) * 512, :].rearrange("(c p) f -> p c f", p=128)), writes=[kWO], dma="rwo")
            u = 0
            for qk in range(2):
                DST = QT if qk == 0 else KT
                for c in range(4):
                    tok = slice(c * 512, (c + 1) * 512)
                    p1, p2 = PS[2 * (u % 2)], PS[2 * (u % 2) + 1]
                    tb = u % 2
                    u += 1
                    for a, pp in ((0, p1), (1, p2)):
                        for kk in range(NCH):
                            S.add("pe", lambda e, pp=pp, kk=kk, a=a, qk=qk, tok=tok, WA=WA: e.matmul(pp[:], WA[:, kk, qk * 256 + a * 128:qk * 256 + (a + 1) * 128], XT[:, kk, tok],
                                                                                              start=(kk == 0), stop=(kk == 7)),
                                  reads=[(kWA, qk)] + [("XT", tt) for tt in range(4 * c, 4 * c + 4)], writes=[("ps", 2 * tb + a)])
                    t1, t2 = T1[tb], T2[tb]
                    k1, k2 = ("T1", tb), ("T2", tb)
                    S.add("dve", lambda e, t1=t1, p1=p1, tok=tok: e.tensor_tensor(out=t1, in0=p1[:], in1=COS[:, tok], op=ALU.mult), reads=[("ps", 2 * tb), kC], writes=[k1])
                    S.add("dve", lambda e, t2=t2, p2=p2, tok=tok: e.tensor_tensor(out=t2, in0=p2[:], in1=SIN[:, tok], op=ALU.mult), reads=[("ps", 2 * tb + 1), kSn], writes=[k2])
                    S.add("pool", lambda e, t1=t1, t2=t2, DST=DST, tok=tok: e.tensor_tensor(out=DST[:, 0, tok], in0=t1, in1=t2, op=ALU.subtract), reads=[k1, k2], writes=[("QK", qk, c, 0)])
                    S.add("dve", lambda e, t1=t1, p1=p1, tok=tok: e.tensor_tensor(out=t1, in0=p1[:], in1=SIN[:, tok], op=ALU.mult), reads=[("ps", 2 * tb), kSn], writes=[k1])
                    S.add("dve", lambda e, t2=t2, p2=p2, tok=tok: e.tensor_tensor(out=t2, in0=p2[:], in1=COS[:, tok], op=ALU.mult), reads=[("ps", 2 * tb + 1), kC], writes=[k2])
                    S.add("pool", lambda e, t1=t1, t2=t2, DST=DST, tok=tok: e.tensor_tensor(out=DST[:, 1, tok], in0=t1, in1=t2, op=ALU.add), reads=[k1, k2], writes=[("QK", qk, c, 1)])
            for t in range(NT):
                pv = PS[4 + t % 2]
                for kk in range(NCH):
                    S.add("pe", lambda e, pv=pv, kk=kk, t=t, WB=WB: e.matmul(pv[:], XT[:, kk, t * 128:(t + 1) * 128], WB[:, kk, :], start=(kk == 0), stop=(kk == 7)),
                          reads=[kWB, ("XT", t)], writes=[("ps", 4 + t % 2)])
                S.add("act", lambda e, pv=pv, t=t: e.activation(out=V[:, t, :], in_=pv[:], func=AF.Copy), reads=[("ps", 4 + t % 2)], writes=[("V", t)])
            S.barrier()
            self.aptr = mark
            PT = self.alloc([NT, 256], BF16)
            RB_ = [self.alloc([512]) for _ in range(2)]
            SG = [self.alloc([512]) for _ in range(2)]
            Y = [self.alloc([512], BF16) for _ in range(2)]
            YT = [self.alloc([4, 128], BF16) for _ in range(2)]
            ST = [self.alloc([16]) for _ in range(2)]
            scn = [0]

            def scores(c, h=h, PT=PT):
                q0 = 256 * c
                nk = 2 * c + 2
                for ks in range(nk):
                    pi = scn[0] % 2
                    scn[0] += 1
                    pss = PS[pi]
                    for a in range(2):
                        S.add("pe", lambda e, pss=pss, a=a, ks=ks, q0=q0: e.matmul(pss[:, 0:256], KT[:, a, ks * 128:(ks + 1) * 128], QT[:, a, q0:q0 + 256], start=(a == 0), stop=(a == 1)),
                              reads=[], writes=[("ps", pi)])
                    if ks >= 2 * c:
                        S.add("dve", lambda e, pss=pss, ks=ks, c=c, h=h: e.tensor_tensor(out=PT[:, ks, :], in0=pss[:, 0:256], in1=MASK[:, h, 1 + ks - 2 * c, :], op=ALU.mult),
                              reads=[("ps", pi), kM], writes=[("PT", ks)])
                    else:
                        off = q0 - 128 * ks
                        gv = float(gam[h] ** off)
                        S.add("dve", lambda e, pss=pss, ks=ks, gv=gv, h=h: e.scalar_tensor_tensor(out=PT[:, ks, :], in0=pss[:, 0:256], scalar=gv, in1=MASK[:, h, 0, :], op0=ALU.mult, op1=ALU.mult),
                              reads=[("ps", pi), kM], writes=[("PT", ks)])

            def st1(qt, PT=PT):
                b = qt % 2
                qi = qt % 2
                po, pg = PS[2 + b], PS[4 + b]
                for ks in range(qt + 1):
                    S.add("pe", lambda e, po=po, ks=ks, qi=qi, qt=qt: e.matmul(po[:], PT[:, ks, qi * 128:(qi + 1) * 128], V[:, ks, :], start=(ks == 0), stop=(ks == qt)),
                          reads=[("PT", ks), ("V", ks)], writes=[("ps", 2 + b)])
                for kk in range(NCH):
                    S.add("pe", lambda e, pg=pg, kk=kk, qt=qt: e.matmul(pg[:], XT[:, kk, qt * 128:(qt + 1) * 128], WC[:, kk, :], start=(kk == 0), stop=(kk == 7)),
                          reads=[kWC, ("XT", qt)], writes=[("ps", 4 + b)])

            def st2(qt, ST=ST, RB_=RB_, SG=SG, Y=Y):
                b = qt % 2
                po, pg = PS[2 + b], PS[4 + b]
                st, rb, sg, y = ST[b], RB_[b], SG[b], Y[b]
                ks_ = ("gn", b)
                S.add("dve", lambda e, st=st, po=po: e.bn_stats(out=st[:, 0:6], in_=po[:]), reads=[("ps", 2 + b)], writes=[(ks_, 0)])
                S.add("dve", lambda e, st=st: e.bn_aggr(out=st[:, 6:8], in_=st[:, 0:6]), reads=[(ks_, 0)], writes=[(ks_, 1)])
                S.add("dve", lambda e, st=st: e.tensor_scalar(out=st[:, 8:9], in0=st[:, 7:8], scalar1=float(GN_EPS), scalar2=None, op0=ALU.add), reads=[(ks_, 1)], writes=[(ks_, 2)])
                S.add("act", lambda e, st=st: e.activation(out=st[:, 8:9], in_=st[:, 8:9], func=AF.Sqrt), reads=[(ks_, 2)], writes=[(ks_, 2)])
                S.add("act", lambda e, sg=sg, pg=pg: e.activation(out=sg, in_=pg[:], func=AF.Silu), reads=[("ps", 4 + b)], writes=[("SGr", b)])
                S.add("dve", lambda e, st=st: e.reciprocal(out=st[:, 9:10], in_=st[:, 8:9]), reads=[(ks_, 2)], writes=[(ks_, 3)])
                S.add("dve", lambda e, st=st, po=po, rb=rb: e.tensor_scalar(out=rb, in0=po[:], scalar1=st[:, 6:7], scalar2=st[:, 9:10], op0=ALU.subtract, op1=ALU.mult),
                      reads=[("ps", 2 + b), (ks_, 1), (ks_, 3)], writes=[("RB", b)])
                S.add("pool", lambda e, y=y, rb=rb, sg=sg: e.tensor_tensor(out=y, in0=rb, in1=sg, op=ALU.mult), reads=[("RB", b), ("SGr", b)], writes=[("Y", b)])

            def st3(qt, Y=Y, YT=YT):
                b = qt % 2
                y, yt = Y[b], YT[b]
                ptr_ = PSB[6]
                for a in range(4):
                    S.add("pe", lambda e, ptr_=ptr_, a=a, y=y: e.transpose(ptr_[:, a * 128:(a + 1) * 128], y[:, a * 128:(a + 1) * 128], self.IDB[:]),
                          reads=[("Y", b), "IDB"], writes=[("ps", 6)])
                S.add("act", lambda e, ptr_=ptr_, yt=yt: e.activation(out=yt, in_=ptr_[:, 0:512].rearrange("p (a b) -> p a b", a=4), func=AF.Copy), reads=[("ps", 6)], writes=[("YT", b)])
                for hh, pxi in ((0, 7), (1, 6)):
                    px = PS[pxi]
                    for a in range(4):
                        S.add("pe", lambda e, px=px, a=a, yt=yt, hh=hh: e.matmul(px[:], yt[:, a, :], WO[:, a, hh * 512:(hh + 1) * 512], start=(a == 0), stop=(a == 3)),
                              reads=[("YT", b), kWO], writes=[("ps", pxi)])
                    S.add("dve", lambda e, px=px, qt=qt, hh=hh: e.tensor_tensor(out=X[:, qt, hh * 512:(hh + 1) * 512], in0=px[:], in1=X[:, qt, hh * 512:(hh + 1) * 512], op=ALU.add),
                          reads=[("ps", pxi), ("X", qt)], writes=[("X", qt)])

            for step in range(NT + 2):
                if step < NT and step % 2 == 0:
                    scores(step // 2)
                if step < NT:
                    st1(step)
                if 0 <= step - 1 < NT:
                    st2(step - 1)
                if 0 <= step - 2 < NT:
                    st3(step - 2)
        S.barrier()
        self.arena_reset()

    def stickbreak(self, l):
        S, d, X, XT, PS = self.S, self.d, self.X, self.XT, self.PS
        jl = l // 2
        w_in = d["sb_w_in"][jl].rearrange("(c p) f -> p c f", p=128)
        w_out = d["sb_w_out"][jl]
        PSB = [p[:].bitcast(BF16) for p in PS]
        NSET = 3
        SBMf = self.alloc([128])
        SBM = self.alloc([128], BF16)
        kSBM = self.key("sbm")
        S.add("sp", lambda e: e.dma_start(out=SBMf, in_=d["sbmask"]), writes=[kSBM], dma="sbm")
        S.add("dve", lambda e: e.tensor_copy(out=SBM, in_=SBMf), reads=[kSBM], writes=[kSBM])
        WQ = self.alloc([NCH, 128], BF16)
        WK = self.alloc([NCH, 128], BF16)
        WV = self.alloc([NCH, 128], BF16)
        WOp = self.alloc([D], BF16)
        QT = self.alloc([S_TOK], BF16)
        KT = self.alloc([S_TOK], BF16)
        V = self.alloc([NT, 128], BF16)
        E1 = [self.alloc([S_TOK]) for _ in range(NSET)]
        SP = [self.alloc([S_TOK]) for _ in range(NSET)]
        EX = [self.alloc([S_TOK]) for _ in range(NSET)]
        AT = [self.alloc([NT, 128], BF16) for _ in range(NSET)]
        SM = [self.alloc([8]) for _ in range(NSET)]
        OS = [self.alloc([128], BF16) for _ in range(2)]
        OT = [self.alloc([128], BF16) for _ in range(2)]
        for s_ in range(NSET):
            S.add("pool", lambda e, s_=s_: e.memset(EX[s_][:, 0:1], 0.0), writes=[("EX", s_)])

        def load_w(m):
            S.add("pool", lambda e, m=m: e.dma_start(out=WQ, in_=w_in[:, :, m * 128:(m + 1) * 128]), writes=["WQ"], dma="swq")
            S.add("pool", lambda e, m=m: e.dma_start(out=WK, in_=w_in[:, :, 1024 + m * 128:1024 + (m + 1) * 128]), writes=["WK"], dma="swk")
            S.add("pool", lambda e, m=m: e.dma_start(out=WV, in_=w_in[:, :, 2048 + m * 128:2048 + (m + 1) * 128]), writes=["WV"], dma="swv")

        self._zc = 0

        def stA(u):
            m, qt, hh, sb_ = u
            n = 128 * (qt + 1)
            e1, sp, sm = E1[sb_], SP[sb_], SM[sb_]
            hp = slice(64 * hh, 64 * hh + 64)
            nkb = (n + 511) // 512
            for kb in range(nkb):
                w = min(512, n - 512 * kb)
                zb = self._zc % 2
                self._zc += 1
                pz = PS[zb]
                last = (kb == nkb - 1)
                S.add("pe", lambda e, pz=pz, w=w, kb=kb, hp=hp, qt=qt, last=last: e.matmul(pz[:, 0:w], QT[hp, qt * 128:(qt + 1) * 128], KT[hp, 512 * kb:512 * kb + w], start=True, stop=(not last)),
                      reads=[("QTKT", 0, qt // 4), ("QTKT", 1, kb)], writes=[("ps", zb)])
                if last:
                    S.add("pe", lambda e, pz=pz, w=w: e.matmul(pz[:, w - 128:w], self.IDB[:], SBM, start=False, stop=True),
                          reads=["IDB", kSBM], writes=[("ps", zb)])
                S.add("act", lambda e, pz=pz, e1=e1, kb=kb, w=w: e.activation(out=e1[:, 512 * kb:512 * kb + w], in_=pz[:, 0:w], func=AF.Exp),
                      reads=[("ps", zb)], writes=[("E1", sb_)])
            S.add("act", lambda e, e1=e1, sp=sp, n=n, sm=sm: e.activation(out=sp[:, 0:n], in_=e1[:, 0:n], func=AF.Ln, bias=1.0, scale=1.0, accum_out=sm[:, 0:1]),
                  reads=[("E1", sb_)], writes=[("SP", sb_), ("TT", sb_)])

        def stB1(u):
            m, qt, hh, sb_ = u
            n = 128 * (qt + 1)
            sp, ex, sm = SP[sb_], EX[sb_], SM[sb_]
            S.add("dve", lambda e, sm=sm: e.tensor_scalar(out=sm[:, 1:2], in0=sm[:, 0:1], scalar1=-1.0, scalar2=None, op0=ALU.mult), reads=[("TT", sb_)], writes=[("NTT", sb_)])
            S.add("dve", lambda e, sp=sp, ex=ex, n=n: e.tensor_tensor_scan(out=ex[:, 1:n], data0=sp[:, 0:n - 1], data1=sp[:, 0:n - 1], initial=0.0, op0=ALU.add, op1=ALU.max),
                  reads=[("SP", sb_)], writes=[("EX", sb_)])

        def stB2(u):
            m, qt, hh, sb_ = u
            n = 128 * (qt + 1)
            e1, sp, ex, sm = E1[sb_], SP[sb_], EX[sb_], SM[sb_]
            a_ = ex.bitcast(BF16)[:, S_TOK:2 * S_TOK]
            S.add("act", lambda e, ex=ex, sp=sp, n=n, sm=sm: e.activation(out=sp[:, 0:n], in_=ex[:, 0:n], func=AF.Exp, bias=sm[:, 1:2], scale=1.0),
                  reads=[("EX", sb_), ("NTT", sb_)], writes=[("SP", sb_)])
            S.add("dve", lambda e, e1=e1, sp=sp, a_=a_, n=n: e.tensor_tensor(out=a_[:, 0:n], in0=e1[:, 0:n], in1=sp[:, 0:n], op=ALU.mult),
                  reads=[("E1", sb_), ("SP", sb_)], writes=[("EX", sb_), ("A", sb_)])

        def stC1(u):
            m, qt, hh, sb_ = u
            ex, at = EX[sb_], AT[sb_]
            a_ = ex.bitcast(BF16)[:, S_TOK:2 * S_TOK]
            for g in range((qt + 8) // 8):
                nb = min(8, qt + 1 - 8 * g)
                ptb = PSB[2 + g % 2]
                for i in range(nb):
                    kb2 = 8 * g + i
                    S.add("pe", lambda e, ptb=ptb, i=i, kb2=kb2, a_=a_: e.transpose(ptb[:, i * 128:(i + 1) * 128], a_[:, kb2 * 128:(kb2 + 1) * 128], self.IDB[:]),
                          reads=[("A", sb_), "IDB"], writes=[("ps", 2 + g % 2)])

        def stC1b(u):
            m, qt, hh, sb_ = u
            at = AT[sb_]
            for g in range((qt + 8) // 8):
                nb = min(8, qt + 1 - 8 * g)
                ptb = PSB[2 + g % 2]
                S.add("dve", lambda e, ptb=ptb, at=at, g=g, nb=nb: e.tensor_copy(out=at[:, 8 * g:8 * g + nb, :], in_=ptb[:, 0:nb * 128].rearrange("p (a b) -> p a b", a=nb)),
                      reads=[("ps", 2 + g % 2)], writes=[("AT", sb_, g)])

        def stC2(u):
            m, qt, hh, sb_ = u
            at = AT[sb_]
            ob = qt % 2
            hp = slice(64 * hh, 64 * hh + 64)
            po = PS[4 + hh]
            for kb2 in range(qt + 1):
                S.add("pe", lambda e, po=po, kb2=kb2, at=at, hp=hp, qt=qt: e.matmul(po[:, 0:64], at[:, kb2, :], V[:, kb2, hp], start=(kb2 == 0), stop=(kb2 == qt)),
                      reads=[("AT", sb_, kb2 // 8), ("V", kb2)], writes=[("ps", 4 + hh)])
            S.add("dve", lambda e, po=po, hp=hp, ob=ob: e.tensor_copy(out=OS[ob][:, hp], in_=po[:, 0:64]), reads=[("ps", 4 + hh)], writes=[("OS", ob, hh)])
            if hh == 1:
                ptb = PSB[6]
                S.add("pe", lambda e, ptb=ptb, ob=ob: e.transpose(ptb[:, 0:128], OS[ob], self.IDB[:]), reads=[("OS", ob, 0), ("OS", ob, 1), "IDB"], writes=[("ps", 6)])
                S.add("dve", lambda e, ptb=ptb, ob=ob: e.tensor_copy(out=OT[ob], in_=ptb[:, 0:128]), reads=[("ps", 6)], writes=[("OT", ob)])
                for hf in range(2):
                    px = PS[6 + hf]
                    S.add("pe", lambda e, px=px, ob=ob, hf=hf: e.matmul(px[:], OT[ob], WOp[:, hf * 512:(hf + 1) * 512], start=True, stop=True),
                          reads=[("OT", ob), "WOp"], writes=[("ps", 6 + hf)])
                    S.add("dve", lambda e, px=px, qt=qt, hf=hf: e.tensor_tensor(out=X[:, qt, hf * 512:(hf + 1) * 512], in0=px[:], in1=X[:, qt, hf * 512:(hf + 1) * 512], op=ALU.add),
                          reads=[("ps", 6 + hf), ("X", qt)], writes=[("X", qt)])

        ucount = 0
        for m in range(8):
            load_w(m)
            for c in range(4):
                tok = slice(c * 512, (c + 1) * 512)
                for which, W_, DST, scl in ((0, WQ, QT, 0.125), (1, WK, KT, 1.0)):
                    pp = PS[which]
                    for kk in range(NCH):
                        S.add("pe", lambda e, pp=pp, kk=kk, W_=W_, tok=tok: e.matmul(pp[:], W_[:, kk, :], XT[:, kk, tok], start=(kk == 0), stop=(kk == 7)),
                              reads=["WQ" if which == 0 else "WK"] + [("XT", tt) for tt in range(4 * c, 4 * c + 4)], writes=[("ps", which)])
                    S.add("act", lambda e, pp=pp, DST=DST, tok=tok, scl=scl: e.activation(out=DST[:, tok], in_=pp[:], func=AF.Copy, scale=float(scl)),
                          reads=[("ps", which)], writes=[("QTKT", which, c)])
            for t in range(NT):
                pv = PS[2 + t % 2]
                for kk in range(NCH):
                    S.add("pe", lambda e, pv=pv, kk=kk, t=t: e.matmul(pv[:, 0:128], XT[:, kk, t * 128:(t + 1) * 128], WV[:, kk, :], start=(kk == 0), stop=(kk == 7)),
                          reads=["WV", ("XT", t)], writes=[("ps", 2 + t % 2)])
                S.add("dve", lambda e, pv=pv, t=t: e.tensor_copy(out=V[:, t, :], in_=pv[:, 0:128]), reads=[("ps", 2 + t % 2)], writes=[("V", t)])
            S.add("pool", lambda e, m=m: e.dma_start(out=WOp, in_=w_out[m * 128:(m + 1) * 128, :]), writes=["WOp"], dma="swo")
            units = []
            for qt in range(NT):
                for hh in range(2):
                    units.append((m, qt, hh, ucount % NSET))
                    ucount += 1
            nu = len(units)
            for step in range(nu + 2):
                if step < nu:
                    stA(units[step])
                if 0 <= step - 1 < nu:
                    stB1(units[step - 1])
                if 0 <= step - 2 < nu:
                    stC1(units[step - 2])
                    stC1b(units[step - 2])
                    stC2(units[step - 2])
                if 0 <= step - 1 < nu:
                    stB2(units[step - 1])
        S.barrier()
        self.arena_reset()


def _host_inputs(inp, b, consts, big=True):
    f = lambda a: np.ascontiguousarray(np.asarray(a, dtype=np.float32))
    m = {}
    m["x"] = f(inp["x"][b])
    m["pT"] = f(np.transpose(np.asarray(inp["p"])[:, b], (0, 2, 1)))
    for k in ("ret_w_in", "ret_w_out", "sb_w_in", "sb_w_out", "router_w", "router_b", "w_gate_up", "w_down",
              "b_down", "ple_w", "ple_gate_w", "ple_gate_b"):
        if not big and k in ("w_gate_up", "w_down"):
            m[k] = f(np.asarray(inp[k])[0:1, 0:1])
        else:
            m[k] = f(inp[k])
    m["lnp"] = f(np.stack([inp["ln1_g"], inp["ln1_b"], inp["ln2_g"], inp["ln2_b"]], axis=1))
    bgu = np.asarray(inp["b_gate_up"], dtype=np.float32).reshape(DEPTH, NE, 8, 128, 2)
    m["bgu"] = f(np.transpose(bgu, (0, 3, 1, 2, 4)).reshape(DEPTH, 128, NE * 16))
    for k in ("identf", "identb", "ones", "ut", "iota", "onesb", "cos", "sin", "rmask", "sbmask"):
        m[k] = consts[k]
    return m


def run_layers(inp, layers, n_exp=NE, stages=("mix", "moe", "ple"), cores=8):
    bld = Builder(layers, n_exp=n_exp, stages=stages)
    nc = bld.build()
    in_maps = [_host_inputs(inp, b, bld.consts, big=("moe" in stages)) for b in range(cores)]
    res = run_bass_kernel_spmd(nc, in_maps, core_ids=list(range(cores)))
    return np.stack([res.results[b]["y"] for b in range(cores)], axis=0)


def kernel(**inputs):
    out = run_layers(inputs, layers=range(DEPTH))
    return out.astype(np.float32)
```
